# Optimizing a Trainium2 kernel written in Bass

```python
import jax, jax.numpy as jnp
from jax import lax
import numpy as np

D_MODEL = 2048
BATCH = 1
SEQ = 16384
DEPTH = 2

GRID_W = 64
CTX_LEN = 256
A_HEADS = D_MODEL // 256
A_HEAD_DIM = 128
A_WIDTH = A_HEADS * A_HEAD_DIM
HGRN_CHUNK = 64
B_Q_HEADS = D_MODEL // 256
B_KV_HEADS = 2
B_HEAD_DIM = 128
B_WIDTH = B_Q_HEADS * B_HEAD_DIM
B_KV_WIDTH = B_KV_HEADS * B_HEAD_DIM
EVEN_IN = 5 * A_WIDTH + B_WIDTH + 2 * B_KV_WIDTH
MLA_HEADS = D_MODEL // 128
MLA_Q_LORA = 512
MLA_KV_LORA = 512
MLA_NOPE = 128
MLA_ROPE = 64
MLA_V = 128
MLA_DOWN = MLA_Q_LORA + MLA_KV_LORA + MLA_ROPE
N_EXPERTS = 16
EXPERT_FF = D_MODEL // 2
EC_CAPACITY_FACTOR = 2
Q_BLOCK = 128
ROPE_THETA = 10000.0
NORM_EPS = 1e-6
N_EVEN = (DEPTH + 1) // 2
N_ODD = DEPTH // 2
DEEPNORM_ALPHA = (2.0 * DEPTH) ** 0.25
DEEPNORM_BETA = (8.0 * DEPTH) ** -0.25

kernel_name = 'hybrid_hgrn2_gqa_mla_ecmoe_dit_trunk'


def layer_norm(x):
    xf = x.astype(jnp.float32)
    mu = jnp.mean(xf, axis=-1, keepdims=True)
    var = jnp.mean(jnp.square(xf - mu), axis=-1, keepdims=True)
    return (xf - mu) * lax.rsqrt(var + NORM_EPS)


def rms_norm(x, g):
    xf = x.astype(jnp.float32)
    y = xf * lax.rsqrt(jnp.mean(jnp.square(xf), axis=-1, keepdims=True) + NORM_EPS) * g
    return y.astype(x.dtype)


def modulate(x, shift, scale):
    return (layer_norm(x) * (1.0 + scale) + shift).astype(x.dtype)


def post_norm(x, y, gate, g, b):
    return (layer_norm(DEEPNORM_ALPHA * x + gate * y) * g + b).astype(x.dtype)


def axial_rope_tables(n_tokens, rot_dim):
    rows = n_tokens // GRID_W
    row = jnp.repeat(jnp.arange(rows, dtype=jnp.float32), GRID_W)
    col = jnp.tile(jnp.arange(GRID_W, dtype=jnp.float32), rows)
    n_freq = rot_dim // 4
    inv = ROPE_THETA ** (-jnp.arange(n_freq, dtype=jnp.float32) / n_freq)
    ang = jnp.concatenate([row[:, None] * inv, col[:, None] * inv], axis=-1)
    return jnp.cos(ang), jnp.sin(ang)


def apply_rope(x, cos, sin):
    half = x.shape[-1] // 2
    xf = x.astype(jnp.float32)
    x1, x2 = xf[..., :half], xf[..., half:]
    c = cos[None, :, None, :]
    s = sin[None, :, None, :]
    return jnp.concatenate([x1 * c - x2 * s, x2 * c + x1 * s], axis=-1).astype(x.dtype)


def block_attention(q, k, v):
    b, n, hk, g, d = q.shape
    scale = d ** -0.5
    nblk = n // Q_BLOCK
    qb = q.reshape(b, nblk, Q_BLOCK, hk, g, d).swapaxes(0, 1)

    def one_block(qi):
        s = jnp.einsum('bqhgd,bkhd->bhgqk', qi, k, preferred_element_type=jnp.float32) * scale
        p = jax.nn.softmax(s, axis=-1)
        return jnp.einsum('bhgqk,bkhe->bqhge', p.astype(v.dtype), v)

    o = lax.map(one_block, qb)
    return o.swapaxes(0, 1).reshape(b, n, hk * g * v.shape[-1])


def hgrn2_chunked(q, k, logf, v, s0):
    b, L, h, dk = q.shape
    dv = v.shape[-1]
    n = L // HGRN_CHUNK

    def to_chunks(t):
        return t.reshape(b, n, HGRN_CHUNK, h, t.shape[-1]).transpose(1, 0, 3, 2, 4)

    mask = jnp.tril(jnp.ones((HGRN_CHUNK, HGRN_CHUNK), dtype=bool))[:, :, None]

    def step(S, inp):
        qc, kc, gc, vc = inp
        cum = jnp.cumsum(gc, axis=-2)
        rel = jnp.where(mask, cum[:, :, :, None, :] - cum[:, :, None, :, :], -jnp.inf)
        decay = jnp.exp(rel)
        scores = jnp.einsum('bhtc,bhsc,bhtsc->bhts', qc, kc, decay)
        o = (jnp.einsum('bhts,bhsv->bhtv', scores, vc)
             + jnp.einsum('bhtc,bhcv->bhtv', qc * jnp.exp(cum), S))
        last = cum[:, :, -1:, :]
        S_new = (jnp.exp(last)[:, :, 0, :, None] * S
                 + jnp.einsum('bhsc,bhsv->bhcv', kc * jnp.exp(last - cum), vc))
        return S_new, o

    s_fin, o = lax.scan(step, s0, (to_chunks(q), to_chunks(k), to_chunks(logf), to_chunks(v)))
    o = o.transpose(1, 0, 3, 2, 4).reshape(b, L, h, dv)
    return o, s_fin


def forget_gate(f_logit, lb):
    f = lb + (1.0 - lb) * jax.nn.sigmoid(f_logit.astype(jnp.float32))
    return jnp.log(f), (1.0 - f).astype(f_logit.dtype)


def heads(t, hd):
    return t.reshape(t.shape[0], t.shape[1], -1, hd)


def even_mixer(h_ctx, h_lat, w_in, w_out, lb_dir, a_norm_g, q_norm_g, k_norm_g, rope_b, need_ctx):
    splits = np.cumsum([A_WIDTH] * 5 + [B_WIDTH, B_KV_WIDTH]).tolist()

    def project(h):
        qa, fa_f, fa_b, ia, ga, qb, kb, vb = jnp.split(h @ w_in, splits, axis=-1)
        gf, kf = forget_gate(fa_f, lb_dir[0])
        gb, kbk = forget_gate(fa_b, lb_dir[1])
        hgrn = (heads(qa, A_HEAD_DIM), heads(ia, A_HEAD_DIM),
                heads(gf, A_HEAD_DIM), heads(kf, A_HEAD_DIM),
                heads(gb, A_HEAD_DIM), heads(kbk, A_HEAD_DIM), ga)
        gqa = (rms_norm(heads(qb, B_HEAD_DIM), q_norm_g),
               rms_norm(heads(kb, B_HEAD_DIM), k_norm_g),
               heads(vb, B_HEAD_DIM))
        return hgrn, gqa

    (qa_c, i_c, gf_c, kf_c, gb_c, kb_c, ga_c), (qb_c, kb_cx, vb_c) = project(h_ctx)
    (qa_l, i_l, gf_l, kf_l, gb_l, kb_l, ga_l), (qb_l, kb_lt, vb_l) = project(h_lat)

    flip = lambda t: t[:, ::-1]
    z0 = jnp.zeros((h_lat.shape[0], A_HEADS, A_HEAD_DIM, A_HEAD_DIM), jnp.float32)
    o_cf, s_f = hgrn2_chunked(qa_c, kf_c, gf_c, i_c, z0)
    o_lf, _ = hgrn2_chunked(qa_l, kf_l, gf_l, i_l, s_f)
    o_cb, s_b = hgrn2_chunked(flip(qa_c), flip(kb_c), flip(gb_c), flip(i_c), z0)
    o_lb, _ = hgrn2_chunked(flip(qa_l), flip(kb_l), flip(gb_l), flip(i_l), s_b)

    def hgrn_out(o, g):
        o = rms_norm(o, a_norm_g).reshape(g.shape[0], g.shape[1], A_WIDTH)
        return (o * jax.nn.silu(g)).astype(g.dtype)

    a_lat = hgrn_out(o_lf + flip(o_lb), ga_l)

    grp = B_Q_HEADS // B_KV_HEADS
    q_l = apply_rope(qb_l, *rope_b)
    k_l = apply_rope(kb_lt, *rope_b)
    k_all = jnp.concatenate([kb_cx, k_l], axis=1)
    v_all = jnp.concatenate([vb_c, vb_l], axis=1)
    b_lat = block_attention(q_l.reshape(q_l.shape[0], q_l.shape[1], B_KV_HEADS, grp, B_HEAD_DIM), k_all, v_all)
    y_lat = jnp.concatenate([a_lat, b_lat], axis=-1) @ w_out

    y_ctx = None
    if need_ctx:
        a_ctx = hgrn_out(o_cf + flip(o_cb), ga_c)
        b_ctx = block_attention(qb_c.reshape(qb_c.shape[0], qb_c.shape[1], B_KV_HEADS, grp, B_HEAD_DIM), kb_cx, vb_c)
        y_ctx = jnp.concatenate([a_ctx, b_ctx], axis=-1) @ w_out
    return y_ctx, y_lat


def mla_mixer(h_ctx, h_lat, w_down, q_norm_g, kv_norm_g, w_uq, w_ukv, w_o, rope_c, need_ctx):
    def project(h, rope, want_q):
        b, n, _ = h.shape
        cq, ckv, kr = jnp.split(h @ w_down, [MLA_Q_LORA, MLA_Q_LORA + MLA_KV_LORA], axis=-1)
        kv = (rms_norm(ckv, kv_norm_g) @ w_ukv).reshape(b, n, MLA_HEADS, MLA_NOPE + MLA_V)
        k_nope, v = kv[..., :MLA_NOPE], kv[..., MLA_NOPE:]
        kr = kr[:, :, None, :]
        if rope is not None:
            kr = apply_rope(kr, *rope)
        k = jnp.concatenate([k_nope, jnp.broadcast_to(kr, (b, n, MLA_HEADS, MLA_ROPE))], axis=-1)
        q = None
        if want_q:
            q = (rms_norm(cq, q_norm_g) @ w_uq).reshape(b, n, MLA_HEADS, MLA_NOPE + MLA_ROPE)
            if rope is not None:
                q = jnp.concatenate([q[..., :MLA_NOPE], apply_rope(q[..., MLA_NOPE:], *rope)], axis=-1)
        return q, k, v

    q_c, k_c, v_c = project(h_ctx, None, need_ctx)
    q_l, k_l, v_l = project(h_lat, rope_c, True)
    k_all = jnp.concatenate([k_c, k_l], axis=1)
    v_all = jnp.concatenate([v_c, v_l], axis=1)
    y_lat = block_attention(q_l[:, :, :, None, :], k_all, v_all) @ w_o
    y_ctx = None
    if need_ctx:
        y_ctx = block_attention(q_c[:, :, :, None, :], k_c, v_c) @ w_o
    return y_ctx, y_lat


def ec_moe(h, w_router, w_gate, w_up, w_down):
    b, n, d = h.shape
    cap = max(1, EC_CAPACITY_FACTOR * n // N_EXPERTS)
    aff = jax.nn.softmax(jnp.einsum('bnd,de->bne', h, w_router, preferred_element_type=jnp.float32), axis=-1)
    weight, idx = lax.top_k(aff.transpose(0, 2, 1), cap)
    xg = jax.vmap(lambda hb, ib: hb[ib])(h, idx)
    hid = jax.nn.silu(jnp.einsum('becd,edf->becf', xg, w_gate)) * jnp.einsum('becd,edf->becf', xg, w_up)
    y = jnp.einsum('becf,efd->becd', hid, w_down) * weight[..., None].astype(h.dtype)
    return jax.vmap(lambda yb, ib: jnp.zeros((n, d), y.dtype).at[ib.reshape(-1)].add(yb.reshape(-1, d)))(y, idx)


def setup_inputs(seed: int = 0) -> dict:
    key = jax.random.key(seed)
    ks = jax.random.split(key, 24)
    D = D_MODEL
    nrm = lambda k, shape, scale: jax.random.normal(k, shape, jnp.float32) * scale
    return {
        'x': nrm(ks[0], (BATCH, SEQ, D), 1.0),
        'c': nrm(ks[1], (BATCH, D), 1.0),
        'ctx': nrm(ks[2], (BATCH, CTX_LEN, D), 1.0),
        'c_ctx': nrm(ks[3], (D,), 1.0),
        'ada_w': nrm(ks[4], (DEPTH, D, 6 * D), D ** -0.5),
        'ada_b': nrm(ks[5], (DEPTH, 6 * D), 0.01),
        'ln_g': 1.0 + nrm(ks[6], (DEPTH, 2, D), 0.02),
        'ln_b': nrm(ks[7], (DEPTH, 2, D), 0.01),
        'ev_w_in': nrm(ks[8], (N_EVEN, D, EVEN_IN), D ** -0.5),
        'ev_w_out': nrm(ks[9], (N_EVEN, A_WIDTH + B_WIDTH, D), DEEPNORM_BETA * (A_WIDTH + B_WIDTH) ** -0.5),
        'hgrn_lb': nrm(ks[10], (2, DEPTH + 1, A_WIDTH), 0.1),
        'hgrn_norm_g': 1.0 + nrm(ks[11], (N_EVEN, A_HEAD_DIM), 0.02),
        'gqa_q_norm_g': 1.0 + nrm(ks[12], (N_EVEN, B_HEAD_DIM), 0.02),
        'gqa_k_norm_g': 1.0 + nrm(ks[13], (N_EVEN, B_HEAD_DIM), 0.02),
        'mla_w_down': nrm(ks[14], (N_ODD, D, MLA_DOWN), D ** -0.5),
        'mla_q_norm_g': 1.0 + nrm(ks[15], (N_ODD, MLA_Q_LORA), 0.02),
        'mla_kv_norm_g': 1.0 + nrm(ks[16], (N_ODD, MLA_KV_LORA), 0.02),
        'mla_w_uq': nrm(ks[17], (N_ODD, MLA_Q_LORA, MLA_HEADS * (MLA_NOPE + MLA_ROPE)), MLA_Q_LORA ** -0.5),
        'mla_w_ukv': nrm(ks[18], (N_ODD, MLA_KV_LORA, MLA_HEADS * (MLA_NOPE + MLA_V)), MLA_KV_LORA ** -0.5),
        'mla_w_o': nrm(ks[19], (N_ODD, MLA_HEADS * MLA_V, D), DEEPNORM_BETA * (MLA_HEADS * MLA_V) ** -0.5),
        'moe_router': nrm(ks[20], (DEPTH, D, N_EXPERTS), D ** -0.5),
        'moe_w_gate': nrm(ks[21], (DEPTH, N_EXPERTS, D, EXPERT_FF), D ** -0.5),
        'moe_w_up': nrm(ks[22], (DEPTH, N_EXPERTS, D, EXPERT_FF), D ** -0.5),
        'moe_w_down': nrm(ks[23], (DEPTH, N_EXPERTS, EXPERT_FF, D), DEEPNORM_BETA * EXPERT_FF ** -0.5),
    }


def reference(x, c, ctx, c_ctx, ada_w, ada_b, ln_g, ln_b, ev_w_in, ev_w_out, hgrn_lb, hgrn_norm_g,
              gqa_q_norm_g, gqa_k_norm_g, mla_w_down, mla_q_norm_g, mla_kv_norm_g, mla_w_uq, mla_w_ukv,
              mla_w_o, moe_router, moe_w_gate, moe_w_up, moe_w_down):
    n_tok = x.shape[1]
    rope_b = axial_rope_tables(n_tok, B_HEAD_DIM)
    rope_c = axial_rope_tables(n_tok, MLA_ROPE)
    lb_all = jnp.cumsum(jax.nn.softmax(hgrn_lb.astype(jnp.float32), axis=1), axis=1)
    for l in range(DEPTH):
        last = l == DEPTH - 1
        mod_l = (jax.nn.silu(c) @ ada_w[l] + ada_b[l])[:, None, :]
        mod_c = jax.nn.silu(c_ctx) @ ada_w[l] + ada_b[l]
        sh1, sc1, g1, sh2, sc2, g2 = jnp.split(mod_l, 6, axis=-1)
        csh1, csc1, cg1, csh2, csc2, cg2 = jnp.split(mod_c, 6, axis=-1)

        h_l = modulate(x, sh1, sc1)
        h_c = modulate(ctx, csh1, csc1)
        i = l // 2
        if l % 2 == 0:
            y_c, y_l = even_mixer(h_c, h_l, ev_w_in[i], ev_w_out[i], lb_all[:, l], hgrn_norm_g[i],
                                  gqa_q_norm_g[i], gqa_k_norm_g[i], rope_b, not last)
        else:
            y_c, y_l = mla_mixer(h_c, h_l, mla_w_down[i], mla_q_norm_g[i], mla_kv_norm_g[i],
                                 mla_w_uq[i], mla_w_ukv[i], mla_w_o[i], rope_c, not last)
        x = post_norm(x, y_l, g1, ln_g[l, 0], ln_b[l, 0])
        if not last:
            ctx = post_norm(ctx, y_c, cg1, ln_g[l, 0], ln_b[l, 0])

        y_l = ec_moe(modulate(x, sh2, sc2), moe_router[l], moe_w_gate[l], moe_w_up[l], moe_w_down[l])
        x = post_norm(x, y_l, g2, ln_g[l, 1], ln_b[l, 1])
        if not last:
            y_c = ec_moe(modulate(ctx, csh2, csc2), moe_router[l], moe_w_gate[l], moe_w_up[l], moe_w_down[l])
            ctx = post_norm(ctx, y_c, cg2, ln_g[l, 1], ln_b[l, 1])
    return x
```

```python
import contextlib
import numpy as np
import concourse.bass as bass
import concourse.mybir as mybir

F32 = mybir.dt.float32
BF16 = mybir.dt.bfloat16
I32 = mybir.dt.int32
ALU = mybir.AluOpType
ACTF = mybir.ActivationFunctionType
AX = mybir.AxisListType

COMPUTE = ("pe", "act", "dve", "pool")
EPOCH = 4000
NSEM_ENG = 14
NSEM_DMA = 20


class Buf:
    __slots__ = ("name", "w", "rs", "excl")

    def __init__(self, name="", excl=False):
        self.name = name
        self.w = None
        self.rs = []
        self.excl = excl


class Op:
    __slots__ = ("eng", "idx", "fn", "waits", "is_dma", "dma_id", "sig", "clock", "q")

    def __init__(self, eng, idx, fn, is_dma=False):
        self.eng = eng
        self.idx = idx
        self.fn = fn
        self.waits = []
        self.is_dma = is_dma
        self.dma_id = None
        self.sig = None
        self.clock = None
        self.q = None


class Prog:
    def __init__(self, nc, same_engine_sync=True):
        self.nc = nc
        self.es = contextlib.ExitStack()
        self.streams = {e: [] for e in ("pe", "act", "dve", "pool", "sp")}
        self.known = {e: {c: -1 for c in COMPUTE} for e in self.streams}
        self.known_dma = {e: set() for e in self.streams}
        self.dma_count = {"sp": 0, "pool": 0, "act": 0}
        self.dma_ops = {"sp": [], "pool": [], "act": []}
        self.same_engine_sync = same_engine_sync
        self.nbuf = 0
        self.pending = {}

    def barrier(self):
        deps = []
        for f in COMPUTE:
            for o in reversed(self.streams[f]):
                if not o.is_dma:
                    deps.append(o)
                    break
        for q in self.dma_ops:
            deps.extend(self.dma_ops[q][-NSEM_DMA:])
        for e in self.streams:
            self.pending[e] = list(deps)

    def _pend(self, o):
        for d in self.pending.pop(o.eng, []):
            self._need(o, d)

    def sb(self, name, shape, dtype):
        t = self.es.enter_context(self.nc.sbuf_tensor(name, list(shape), dtype))
        return t

    def ps(self, name, shape, dtype=F32):
        t = self.es.enter_context(self.nc.psum_tensor(name, list(shape), dtype))
        return t

    def buf(self, name="", excl=False):
        self.nbuf += 1
        return Buf(name or f"b{self.nbuf}", excl)

    def bufs(self, n, name="", excl=False):
        return [self.buf(f"{name}{i}", excl) for i in range(n)]

    def _need(self, op, dep):
        if dep is None or dep is op:
            return
        e = op.eng
        if dep.is_dma:
            if dep.dma_id in self.known_dma[e]:
                return
            self.known_dma[e].add(dep.dma_id)
            op.waits.append(dep)
            for c, v in dep.clock.items():
                if v > self.known[e][c]:
                    self.known[e][c] = v
            return
        f = dep.eng
        if f == e and (f == "pe" or not self.same_engine_sync):
            return
        if self.known[e][f] >= dep.idx:
            return
        op.waits.append(dep)
        for c, v in dep.clock.items():
            if v > self.known[e][c]:
                self.known[e][c] = v
        if dep.idx > self.known[e][f]:
            self.known[e][f] = dep.idx

    def _track(self, op, reads, writes):
        ex = [r for r in reads if r.excl]
        if ex:
            reads = [r for r in reads if not r.excl]
            writes = list(writes) + [r for r in ex if r not in writes]
        for r in reads:
            self._need(op, r.w)
        for w in writes:
            self._need(op, w.w)
            for rd in w.rs:
                self._need(op, rd)
        for w in writes:
            w.w = op
            w.rs = []
        for r in reads:
            if r.w is not op:
                r.rs.append(op)

    def op(self, eng, fn, reads=(), writes=()):
        st = self.streams[eng]
        o = Op(eng, len(st), fn)
        self._pend(o)
        self._track(o, reads, writes)
        o.clock = dict(self.known[eng])
        if eng in COMPUTE:
            o.clock[eng] = o.idx
        st.append(o)
        return o

    def dma(self, q, out, in_, reads=(), writes=(), **kw):
        st = self.streams[q]
        o = Op(q, len(st), lambda e: e.dma_start(out=out, in_=in_, **kw), is_dma=True)
        o.q = q
        n = self.dma_count[q]
        o.dma_id = (q, n)
        self.dma_count[q] = n + 1
        if n >= NSEM_DMA:
            self._need(o, self.dma_ops[q][n - NSEM_DMA])
        self.dma_ops[q].append(o)
        self._pend(o)
        self._track(o, reads, writes)
        o.clock = dict(self.known[q])
        st.append(o)
        return o

    def emit(self, final_waits=()):
        nc = self.nc
        es = self.es
        needed = set()
        for e, st in self.streams.items():
            for o in st:
                for d in o.waits:
                    if not d.is_dma:
                        needed.add((d.eng, d.idx))
        nsig = {}
        for e in COMPUTE:
            k = 0
            for o in self.streams[e]:
                if (e, o.idx) in needed:
                    o.sig = k
                    k += 1
            nsig[e] = k
        sems = {}
        for e in COMPUTE:
            ne = max(1, (nsig[e] + EPOCH - 1) // EPOCH)
            assert ne <= NSEM_ENG, (e, nsig[e])
            sems[e] = [es.enter_context(nc.semaphore(f"s_{e}{i}")) for i in range(ne)]
        dsems = {}
        for q in ("sp", "pool", "act"):
            if self.dma_count[q]:
                dsems[q] = [es.enter_context(nc.semaphore(f"d_{q}{i}")) for i in range(NSEM_DMA)]
        engobj = {"pe": "tensor", "act": "scalar", "dve": "vector", "pool": "gpsimd", "sp": "sync"}

        def wait_for(eobj, d):
            if d.is_dma:
                q, n = d.dma_id
                eobj.wait_ge(dsems[q][n % NSEM_DMA], 16 * (n // NSEM_DMA + 1))
            else:
                eobj.wait_ge(sems[d.eng][d.sig // EPOCH], d.sig % EPOCH + 1)

        stats = {}
        with nc.Block() as block:
            for e in ("sp", "act", "pe", "dve", "pool"):
                st = self.streams[e]
                if not st and e != "sp":
                    continue

                def body(eobj, st=st, e=e):
                    nw = 0
                    for o in st:
                        for d in o.waits:
                            wait_for(eobj, d)
                            nw += 1
                        ins = o.fn(eobj)
                        if o.is_dma:
                            q, n = o.dma_id
                            ins.then_inc(dsems[q][n % NSEM_DMA], 16)
                        elif o.sig is not None:
                            ins.then_inc(sems[e][o.sig // EPOCH], 1)
                    if e == "sp":
                        for d in final_waits:
                            wait_for(eobj, d)
                    stats[e] = (len(st), nw)

                getattr(block, engobj[e])(body)
        self.stats = stats
        self.nsig = nsig
        print("signals", nsig, flush=True)
        return stats


class Arena:
    def __init__(self, P, name, nwords):
        self.t = P.sb(name, [128, nwords], F32)
        self.n = nwords
        self.off = 0
        self.floor = 0

    def mark(self):
        self.floor = self.off

    def reset(self):
        self.off = self.floor

    def alloc(self, shape, dtype, parts=128):
        per = 1
        for s_ in shape[1:]:
            per *= s_
        words = per if dtype in (F32, I32) else (per + 1) // 2
        words = (words + 7) // 8 * 8
        assert self.off + words <= self.n, ("arena overflow", self.off, words, self.n)
        v = self.t[0:shape[0], self.off:self.off + words]
        self.off += words
        if dtype not in (F32,):
            v = v.bitcast(dtype)
        v = v[:, 0:per]
        if len(shape) == 3:
            v = v.rearrange("p (a b) -> p a b", a=shape[1])
        elif len(shape) == 4:
            v = v.rearrange("p (a b c) -> p a b c", a=shape[1], b=shape[2])
        return v

import numpy as np
import ml_dtypes
from concourse.bass_utils import run_bass_kernel_spmd

D = 2048
EPS = 1e-6
NCORE = 8
ALPHA = (2.0 * 2) ** 0.25
BF = ml_dtypes.bfloat16


def new_nc():
    return bass.Bass("TRN2", target_bir_lowering=False)


def din(nc, name, shape, dt=F32):
    return nc.dram_tensor(name, list(shape), dt, kind="ExternalInput").ap()


def dout(nc, name, shape, dt=F32):
    return nc.dram_tensor(name, list(shape), dt, kind="ExternalOutput").ap()


def dscr(nc, name, shape, dt=F32):
    return nc.dram_tensor(name, list(shape), dt, kind="Internal").ap()


def run(nc, P, in_maps):
    with P.es:
        res = run_bass_kernel_spmd(nc, in_maps, core_ids=list(range(NCORE)))
    return res.results


class LN:
    def __init__(self, P, name, nb=2):
        self.P = P
        self.nb = nb
        self.st = [P.sb(f"{name}_st{i}", [128, 4, 6], F32) for i in range(nb)]
        self.mv = [P.sb(f"{name}_mv{i}", [128, 4], F32) for i in range(nb)]
        self.b = P.bufs(nb, f"{name}_st")
        self.k = 0

    def norm(self, out_ap, in_ap, b_in, b_out, rows=128):
        P = self.P
        i = self.k % self.nb
        self.k += 1
        st, mv, b = self.st[i], self.mv[i], self.b[i]
        for c in range(4):
            P.op("dve", lambda e, c=c: e.bn_stats(out=st[:rows, c, :], in_=in_ap[:, c * 512:(c + 1) * 512]),
                 reads=[b_in], writes=[b])
        P.op("dve", lambda e: e.bn_aggr(out=mv[:rows, 0:2], in_=st[:rows].rearrange("p a b -> p (a b)")), reads=[b], writes=[b])
        P.op("act", lambda e: e.activation(out=mv[:rows, 2:3], in_=mv[:rows, 1:2], func=ACTF.Sqrt, bias=EPS, scale=1.0),
             reads=[b], writes=[b])
        P.op("dve", lambda e: e.reciprocal(out=mv[:rows, 2:3], in_=mv[:rows, 2:3]), reads=[b], writes=[b])
        P.op("dve", lambda e: e.tensor_scalar(out=mv[:rows, 3:4], in0=mv[:rows, 0:1], scalar1=mv[:rows, 2:3], scalar2=-1.0,
                                              op0=ALU.mult, op1=ALU.mult), reads=[b], writes=[b])
        P.op("act", lambda e: e.activation(out=out_ap, in_=in_ap, func=ACTF.Identity, bias=mv[:rows, 3:4], scale=mv[:rows, 2:3]),
             reads=[b_in, b], writes=[b_out])


def load_bcast(P, name, src_row_ap, width, q="sp"):
    t = P.sb(name, [128, width], F32)
    b = P.buf(name)
    P.dma(q, t[:], src_row_ap.partition_broadcast(128), writes=[b])
    return t, b


def build_l0():
    nc = new_nc()
    CW = 12288 // NCORE
    cv = din(nc, "cv", [128, 16, 2])
    adaw = din(nc, "adaw", [2, D, CW])
    adab = din(nc, "adab", [2, CW])
    modv = dout(nc, "modv", [2, 2, CW])
    P = Prog(nc)
    cvs = P.sb("cvs", [128, 16, 2], F32)
    b_cv = P.buf()
    P.dma("sp", cvs[:], cv, writes=[b_cv])
    scv = P.sb("scv", [128, 16, 2], F32)
    P.op("act", lambda e: e.activation(out=scv[:], in_=cvs[:], func=ACTF.Silu), reads=[b_cv], writes=[b_cv])
    bias = P.sb("bias", [2, 2, CW], F32)
    b_bias = P.buf()
    for l in range(2):
        P.dma("sp", bias[:, l, :], adab[l:l + 1, :].partition_broadcast(2), writes=[b_bias])
    wt = [P.sb(f"wt{i}", [128, 16, 512], F32) for i in range(2)]
    b_wt = P.bufs(2)
    pm = [P.ps(f"pm{i}", [2, 512]) for i in range(2)]
    b_pm = P.bufs(2)
    res = P.sb("res", [2, 2, CW], F32)
    b_res = P.buf()
    k = 0
    for l in range(2):
        for cc in range(CW // 512):
            i = k % 2
            k += 1
            P.dma("sp", wt[i][:], adaw[l].rearrange("(kc p) n -> p kc n", p=128)[:, :, cc * 512:(cc + 1) * 512], writes=[b_wt[i]])
            for kc in range(16):
                P.op("pe", lambda e, i=i, kc=kc: e.matmul(pm[i][:], lhsT=scv[:, kc, :], rhs=wt[i][:, kc, :], start=(kc == 0), stop=(kc == 15)),
                     reads=[b_cv, b_wt[i]], writes=[b_pm[i]])
            P.op("dve", lambda e, i=i, l=l, cc=cc: e.tensor_tensor(out=res[:, l, cc * 512:(cc + 1) * 512], in0=pm[i][:],
                                                                  in1=bias[:, l, cc * 512:(cc + 1) * 512], op=ALU.add),
                 reads=[b_pm[i], b_bias], writes=[b_res])
    o = P.dma("sp", modv.rearrange("l r n -> r l n"), res[:], reads=[b_res])
    P.emit(final_waits=[o])
    return nc, P


def l0_inputs(inp):
    c = np.asarray(inp["c"], np.float32).reshape(D)
    cc = np.asarray(inp["c_ctx"], np.float32).reshape(D)
    cv = np.stack([c, cc], -1).reshape(16, 128, 2).transpose(1, 0, 2).copy()
    CW = 12288 // NCORE
    maps = []
    for j in range(NCORE):
        maps.append({"cv": cv, "adaw": np.ascontiguousarray(inp["ada_w"][:, :, j * CW:(j + 1) * CW]),
                     "adab": np.ascontiguousarray(inp["ada_b"][:, j * CW:(j + 1) * CW])})
    return maps


def l0_gather(results):
    return np.concatenate([np.asarray(r["modv"]) for r in results], axis=-1)


def load_mods(P, modl, modc, slots):
    out = {}
    for which, m in (("l", modl), ("c", modc)):
        for s in slots:
            t, b = load_bcast(P, f"mod_{which}{s}", m[0:1, s * D:(s + 1) * D], D)
            if s in (1, 4):
                P.op("pool", lambda e, t=t: e.tensor_scalar(out=t[:], in0=t[:], scalar1=1.0, scalar2=None, op0=ALU.add),
                     reads=[b], writes=[b])
            out[(s, which)] = (t, b)
    return out


def build_la(ntile=18, nctx_tile=2):
    nc = new_nc()
    NT = ntile * 128
    xin = din(nc, "xin", [NT, D])
    modl = din(nc, "modl", [1, 12288])
    modc = din(nc, "modc", [1, 12288])
    ident_d = din(nc, "ident", [128, 128])
    hT = dout(nc, "hT", [D, NT], BF16)
    P = Prog(nc)
    ident = P.sb("ident_s", [128, 128], F32)
    b_ident = P.buf("ident")
    P.dma("sp", ident[:], ident_d, writes=[b_ident])
    mods = load_mods(P, modl, modc, (0, 1))
    ln = LN(P, "ln")
    NB = 2
    xt = [P.sb(f"xt{i}", [128, D], F32) for i in range(NB)]
    b_xt = P.bufs(NB, "xt")
    ht = [P.sb(f"ht{i}", [128, D], F32) for i in range(NB)]
    b_ht = P.bufs(NB, "ht")
    pT = [P.ps(f"pT{i}", [128, 1024], F32) for i in range(2)]
    b_pT = P.bufs(2, "pT")
    hTs = [P.sb(f"hTs{i}", [128, 16, 512], BF16) for i in range(2)]
    b_hTs = P.bufs(2, "hTs")
    outs = []
    blocks = [(0, nctx_tile)] + [(t, min(4, ntile - t)) for t in range(nctx_tile, ntile, 4)]
    for bi, (t0, nt) in enumerate(blocks):
        hb = bi % 2
        for tb in range(nt):
            t = t0 + tb
            i = t % NB
            which = "c" if t < nctx_tile else "l"
            P.dma("sp", xt[i][:], xin[t * 128:(t + 1) * 128, :], writes=[b_xt[i]])
            ln.norm(ht[i][:], xt[i][:], b_xt[i], b_ht[i])
            sc, bsc = mods[(1, which)]
            sh, bsh = mods[(0, which)]
            P.op("dve", lambda e, i=i, sc=sc: e.tensor_tensor(out=ht[i][:], in0=ht[i][:], in1=sc[:], op=ALU.mult),
                 reads=[b_ht[i], bsc], writes=[b_ht[i]])
            P.op("pool", lambda e, i=i, sh=sh: e.tensor_tensor(out=ht[i][:], in0=ht[i][:], in1=sh[:], op=ALU.add),
                 reads=[b_ht[i], bsh], writes=[b_ht[i]])
            for half in range(2):
                for k in range(8):
                    kc = half * 8 + k
                    P.op("pe", lambda e, i=i, kc=kc, k=k, half=half: e.transpose(out=pT[half][:, k * 128:(k + 1) * 128],
                                                                                in_=ht[i][:, kc * 128:(kc + 1) * 128], identity=ident[:]),
                         reads=[b_ht[i], b_ident], writes=[b_pT[half]])
                P.op("act", lambda e, half=half, hb=hb, tb=tb: e.activation(
                    out=hTs[hb][:, half * 8:(half + 1) * 8, tb * 128:(tb + 1) * 128],
                    in_=pT[half][:].rearrange("p (k t) -> p k t", k=8), func=ACTF.Copy),
                    reads=[b_pT[half]], writes=[b_hTs[hb]])
        w = nt * 128
        o = P.dma("sp", hT.rearrange("(k p) n -> p k n", p=128)[:, :, t0 * 128:t0 * 128 + w], hTs[hb][:, :, 0:w],
                  reads=[b_hTs[hb]], writes=[])
        outs.append(o)
    P.emit(final_waits=outs)
    return nc, P


NCTX = 256


def lb_consts():
    ident_bf = np.eye(128, dtype=np.float32).astype(BF)
    rotT = np.zeros((128, 128), np.float32)
    for m in range(64):
        rotT[m + 64, m] = -1.0
    for m in range(64, 128):
        rotT[m - 64, m] = 1.0
    ones = np.ones((128, 128), np.float32)
    s = np.arange(64)[:, None]
    t = np.arange(64)[None, :]
    masks = np.stack([(s <= t), (s >= t)]).astype(np.float32)
    reset01 = np.ones((128, 512), np.float32)
    reset01[:, ::64] = 0.0
    return {"ident_bf": ident_bf, "rotT": rotT, "ones": ones, "ones_bf": ones.astype(BF), "masks": masks, "reset01": reset01}


def build_lb(nblk=32, upto=3, debug=False):
    nc = new_nc()
    NLAT = nblk * 512
    NTOK = NCTX + NLAT
    NT128 = NTOK // 128
    hT = din(nc, "hT", [D, NTOK], BF16)
    wfm = din(nc, "wfm", [6, D, 128])
    wtm = din(nc, "wtm", [D, 256])
    lbv = din(nc, "lbv", [128, 2, 3])
    gvec_d = din(nc, "gvec", [128, 3])
    cosT = din(nc, "cosT", [128, NLAT])
    sinT = din(nc, "sinT", [128, NLAT])
    c_ident = din(nc, "ident_bf", [128, 128], BF16)
    c_rotT = din(nc, "rotT", [128, 128])
    c_ones = din(nc, "ones", [128, 128])
    c_ones_bf = din(nc, "ones_bf", [128, 128], BF16)
    c_masks = din(nc, "masks", [2, 64, 64])
    c_reset = din(nc, "reset01", [128, 512])
    aT = dout(nc, "aT", [128, NTOK], BF16)
    bT = dout(nc, "bT", [128, NTOK], BF16)
    dscr_ = dout if debug else dscr
    s_q = dscr_(nc, "s_q", [128, NTOK], BF16)
    s_g = dscr_(nc, "s_g", [128, NTOK], BF16)
    s_lf = [dscr_(nc, f"s_lf{d}", [128, NTOK], F32) for d in range(2)]
    s_v = dscr_(nc, "s_v", [NTOK, 128], BF16)
    s_qb = dscr_(nc, "s_qb", [128, NTOK], BF16)
    s_of = dscr_(nc, "s_of", [128, NTOK], F32)
    P = Prog(nc)
    blocks = [(0, NCTX)] + [(NCTX + 512 * i, 512) for i in range(nblk)]
    NBLK = len(blocks)
    db = {n: P.bufs(NBLK, n) for n in ("q", "g", "lf0", "lf1", "v", "qb", "of")}

    def cload(name, src, shape, dt):
        t = P.sb(name, shape, dt)
        b = P.buf(name)
        P.dma("sp", t[:], src, writes=[b])
        return t, b

    ident, b_ident = cload("ident", c_ident, [128, 128], BF16)
    rotT, b_rot = cload("rotT_s", c_rotT, [128, 128], F32)
    ones, b_ones = cload("ones_s", c_ones, [128, 128], F32)
    ones_bf, b_onesbf = cload("onesbf_s", c_ones_bf, [128, 128], BF16)
    masks = P.sb("masks_s", [64, 2, 64], F32)
    b_masks = P.buf()
    P.dma("sp", masks[:], c_masks.rearrange("d s t -> s d t"), writes=[b_masks])
    reset01, b_reset = cload("reset_s", c_reset, [128, 512], F32)
    gvec, b_gvec = cload("gvec_s", gvec_d, [128, 3], F32)
    lbr = P.sb("lbr", [128, 2, 3], F32)
    b_lb = P.buf()
    P.dma("sp", lbr[:], lbv, writes=[b_lb])
    lbt = P.sb("lbt", [128, 8], F32)
    P.op("act", lambda e: e.activation(out=lbr[:], in_=lbr[:], func=ACTF.Exp), reads=[b_lb], writes=[b_lb])
    P.op("dve", lambda e: e.tensor_reduce(out=lbt[:, 0:2], in_=lbr[:], axis=AX.X, op=ALU.add), reads=[b_lb], writes=[b_lb])
    P.op("dve", lambda e: e.reciprocal(out=lbt[:, 0:2], in_=lbt[:, 0:2]), reads=[b_lb], writes=[b_lb])
    P.op("dve", lambda e: e.tensor_tensor(out=lbt[:, 2:4], in0=lbr[:, :, 0], in1=lbt[:, 0:2], op=ALU.mult), reads=[b_lb], writes=[b_lb])
    P.op("dve", lambda e: e.tensor_scalar(out=lbt[:, 4:6], in0=lbt[:, 2:4], scalar1=-1.0, scalar2=1.0, op0=ALU.mult, op1=ALU.add),
         reads=[b_lb], writes=[b_lb])

    KT = P.sb("KT", [128, NTOK], BF16)
    Vres = P.sb("Vres", [128, NT128, 128], BF16)
    b_KT = P.bufs(NBLK, "KT")
    b_V = P.bufs(NBLK, "V")
    pb = [P.ps(f"pb{i}", [128, 512], F32) for i in range(8)]
    b_pb = P.bufs(8, "pb", excl=True)
    pbf = [pb[6][:].bitcast(BF16), pb[7][:].bitcast(BF16)]
    b_pbf = [b_pb[6], b_pb[7]]
    A = Arena(P, "arena", 27 * 1024)

    wfm_bf = A.alloc([128, 6, 16, 128], BF16)
    wtm_bf = A.alloc([128, 16, 256], BF16)
    b_w = P.buf("w")
    wst = [A.alloc([128, 16, 128], F32) for _ in range(2)]
    b_wst = P.bufs(2)
    for idx in range(8):
        i = idx % 2
        if idx < 6:
            src = wfm[idx].rearrange("(kc p) n -> p kc n", p=128)
            dst = wfm_bf[:, idx]
        else:
            h = idx - 6
            src = wtm.rearrange("(kc p) n -> p kc n", p=128)[:, :, h * 128:(h + 1) * 128]
            dst = wtm_bf[:, :, h * 128:(h + 1) * 128]
        P.dma("sp", wst[i][:], src, writes=[b_wst[i]])
        if idx % 2:
            P.op("act", lambda e, i=i, dst=dst: e.activation(out=dst, in_=wst[i][:], func=ACTF.Copy), reads=[b_wst[i]], writes=[b_w])
        else:
            P.op("dve", lambda e, i=i, dst=dst: e.tensor_copy(out=dst, in_=wst[i][:]), reads=[b_wst[i]], writes=[b_w])
    hb = [A.alloc([128, 16, 512], BF16) for _ in range(2)]
    b_hb = P.bufs(2, "hb")
    NTMP = 2
    tmpA = [A.alloc([128, 512], F32) for _ in range(NTMP)]
    tmpB = [A.alloc([128, 512], F32) for _ in range(NTMP)]
    tmpC = [A.alloc([128, 512], F32) for _ in range(NTMP)]
    b_tA, b_tB, b_tC = P.bufs(NTMP), P.bufs(NTMP), P.bufs(NTMP)
    obf = [A.alloc([128, 512], BF16) for _ in range(4)]
    b_obf = P.bufs(4)
    cs = [A.alloc([128, 2, 512], F32) for _ in range(2)]
    b_cs = P.bufs(2)
    vo = [A.alloc([128, 128], BF16) for _ in range(2)]
    b_vo = P.bufs(2)
    kk = [0, 0, 0, 0]

    for bi, (s0, w) in enumerate(blocks):
        lat = bi > 0
        i = bi % 2
        P.dma("sp", hb[i][:, :, 0:w], hT.rearrange("(kc p) n -> p kc n", p=128)[:, :, s0:s0 + w], writes=[b_hb[i]])
        if lat:
            l0 = s0 - NCTX
            P.dma("sp", cs[i][:, 0, 0:w], cosT[:, l0:l0 + w], writes=[b_cs[i]])
            P.dma("sp", cs[i][:, 1, 0:w], sinT[:, l0:l0 + w], writes=[b_cs[i]])
        for idx in range(6):
            pf_i = kk[2] % 3
            kk[2] += 1
            pf, bpf = pb[pf_i], b_pb[pf_i]
            for kc in range(16):
                P.op("pe", lambda e, pf=pf, idx=idx, kc=kc, i=i, w=w: e.matmul(pf[:, 0:w], lhsT=wfm_bf[:, idx, kc, :], rhs=hb[i][:, kc, 0:w],
                                                                             start=(kc == 0), stop=(kc == 15)),
                     reads=[b_w, b_hb[i]], writes=[bpf])
            if idx == 0 or idx == 3:
                oi = kk[1] % 4
                kk[1] += 1
                fn = ACTF.Copy if idx == 0 else ACTF.Silu
                P.op("act", lambda e, oi=oi, pf=pf, w=w, fn=fn: e.activation(out=obf[oi][:, 0:w], in_=pf[:, 0:w], func=fn),
                     reads=[bpf], writes=[b_obf[oi]])
                dst, dbuf = (s_q, db["q"]) if idx == 0 else (s_g, db["g"])
                P.dma("sp", dst[:, s0:s0 + w], obf[oi][:, 0:w], reads=[b_obf[oi]], writes=[dbuf[bi]])
            elif idx in (1, 2):
                d = idx - 1
                ti = kk[0] % NTMP
                kk[0] += 1
                tA, bA = tmpA[ti], b_tA[ti]
                P.op("act", lambda e, tA=tA, pf=pf, w=w: e.activation(out=tA[:, 0:w], in_=pf[:, 0:w], func=ACTF.Sigmoid), reads=[bpf], writes=[bA])
                P.op("dve", lambda e, tA=tA, w=w, d=d: e.tensor_scalar(out=tA[:, 0:w], in0=tA[:, 0:w], scalar1=lbt[:, 4 + d:5 + d], scalar2=lbt[:, 2 + d:3 + d],
                                                                      op0=ALU.mult, op1=ALU.add), reads=[bA, b_lb], writes=[bA])
                P.op("act", lambda e, tA=tA, w=w: e.activation(out=tA[:, 0:w], in_=tA[:, 0:w], func=ACTF.Ln), reads=[bA], writes=[bA])
                P.dma("sp", s_lf[d][:, s0:s0 + w], tA[:, 0:w], reads=[bA], writes=[db[f"lf{d}"][bi]])
            else:
                ti = kk[0] % NTMP
                kk[0] += 1
                tA, bA, tB, bB, tC, bC = tmpA[ti], b_tA[ti], tmpB[ti], b_tB[ti], tmpC[ti], b_tC[ti]
                pn_i = 3 + kk[3] % 2
                kk[3] += 1
                pn, bpn = pb[pn_i], b_pb[pn_i]
                gcol = 1 if idx == 4 else 2
                P.op("act", lambda e, tA=tA, pf=pf, w=w: e.activation(out=tA[:, 0:w], in_=pf[:, 0:w], func=ACTF.Square), reads=[bpf], writes=[bA])
                P.op("pe", lambda e, pn=pn, tA=tA, w=w: e.matmul(pn[:, 0:w], lhsT=ones[:], rhs=tA[:, 0:w], start=True, stop=True),
                     reads=[b_ones, bA], writes=[bpn])
                P.op("act", lambda e, tB=tB, pn=pn, w=w: e.activation(out=tB[:, 0:w], in_=pn[:, 0:w], func=ACTF.Sqrt, bias=EPS, scale=1.0 / 128),
                     reads=[bpn], writes=[bB])
                P.op("dve", lambda e, tB=tB, w=w: e.reciprocal(out=tB[:, 0:w], in_=tB[:, 0:w]), reads=[bB], writes=[bB])
                P.op("dve", lambda e, tA=tA, tB=tB, pf=pf, w=w, gcol=gcol: e.scalar_tensor_tensor(out=tA[:, 0:w], in0=pf[:, 0:w], scalar=gvec[:, gcol:gcol + 1],
                                                                                                 in1=tB[:, 0:w], op0=ALU.mult, op1=ALU.mult),
                     reads=[bpf, bB, b_gvec], writes=[bA])
                if idx == 4:
                    oi = kk[1] % 4
                    kk[1] += 1
                    dest, bdest = obf[oi][:, 0:w], b_obf[oi]
                else:
                    dest, bdest = KT[:, s0:s0 + w], b_KT[bi]
                if lat:
                    pn2_i = 3 + kk[3] % 2
                    kk[3] += 1
                    pr, bpr = pb[pn2_i], b_pb[pn2_i]
                    P.op("pe", lambda e, pr=pr, tA=tA, w=w: e.matmul(pr[:, 0:w], lhsT=rotT[:], rhs=tA[:, 0:w], start=True, stop=True),
                         reads=[b_rot, bA], writes=[bpr])
                    P.op("dve", lambda e, tB=tB, pr=pr, w=w, i=i: e.tensor_tensor(out=tB[:, 0:w], in0=pr[:, 0:w], in1=cs[i][:, 1, 0:w], op=ALU.mult),
                         reads=[bpr, b_cs[i]], writes=[bB])
                    P.op("pool", lambda e, tC=tC, tA=tA, w=w, i=i: e.tensor_tensor(out=tC[:, 0:w], in0=tA[:, 0:w], in1=cs[i][:, 0, 0:w], op=ALU.mult),
                         reads=[bA, b_cs[i]], writes=[bC])
                    P.op("dve", lambda e, dest=dest, tB=tB, tC=tC, w=w: e.tensor_tensor(out=dest, in0=tB[:, 0:w], in1=tC[:, 0:w], op=ALU.add),
                         reads=[bB, bC], writes=[bdest])
                else:
                    P.op("act", lambda e, dest=dest, tA=tA, w=w: e.activation(out=dest, in_=tA[:, 0:w], func=ACTF.Copy), reads=[bA], writes=[bdest])
                if idx == 4:
                    P.dma("sp", s_qb[:, s0:s0 + w], dest, reads=[bdest], writes=[db["qb"][bi]])
        for tt in range(w // 128):
            pt_i = 5 + tt % 2
            pt, bpt = pb[pt_i], b_pb[pt_i]
            for kc in range(16):
                P.op("pe", lambda e, pt=pt, kc=kc, i=i, tt=tt: e.matmul(pt[:, 0:256], lhsT=hb[i][:, kc, tt * 128:(tt + 1) * 128], rhs=wtm_bf[:, kc, :],
                                                                       start=(kc == 0), stop=(kc == 15)),
                     reads=[b_w, b_hb[i]], writes=[bpt])
            vi = tt % 2
            P.op("act", lambda e, pt=pt, vi=vi: e.activation(out=vo[vi][:], in_=pt[:, 0:128], func=ACTF.Copy), reads=[bpt], writes=[b_vo[vi]])
            tok = s0 + tt * 128
            P.dma("sp", s_v[tok:tok + 128, :], vo[vi][:], reads=[b_vo[vi]], writes=[db["v"][bi]])
            P.op("dve", lambda e, pt=pt, tok=tok: e.tensor_copy(out=Vres[:, tok // 128, :], in_=pt[:, 128:256]), reads=[bpt], writes=[b_V[bi]])

    if upto == 1:
        kd_ = dout(nc, "KTo", [128, NTOK], BF16)
        vd_ = dout(nc, "Vo", [128, NT128, 128], BF16)
        o1 = P.dma("sp", kd_, KT[:], reads=b_KT)
        o2 = P.dma("sp", vd_, Vres[:], reads=b_V)
        P.barrier()
        o3 = P.dma("sp", kd_[:, 0:8], KT[:, 0:8])
        P.emit(final_waits=[o1, o2, o3])
        return nc, P
    P.barrier()
    A.reset()
    S_f = A.alloc([128, 128], F32)
    S_bf = [A.alloc([128, 128], BF16) for _ in range(2)]
    b_S = P.buf("S")
    b_Sbf = P.bufs(2, "Sbf")
    NS = 2
    qt = [A.alloc([128, 512], BF16) for _ in range(NS)]
    lf = [A.alloc([128, 512], F32) for _ in range(NS)]
    vch = [A.alloc([64, 8, 128], BF16) for _ in range(NS)]
    b_qt, b_lf, b_vch = P.bufs(NS), P.bufs(NS), P.bufs(NS)
    cum = [A.alloc([128, 512], F32) for _ in range(NS)]
    G = [A.alloc([128, 512], F32) for _ in range(NS)]
    Gr = [A.alloc([128, 512], F32) for _ in range(NS)]
    EE = [A.alloc([128, 512], F32) for _ in range(NS)]
    kkt = [A.alloc([128, 512], F32) for _ in range(NS)]
    tot = [A.alloc([128, 8], F32) for _ in range(NS)]
    etot = [A.alloc([128, 8], F32) for _ in range(NS)]
    qd = [A.alloc([128, 512], BF16) for _ in range(NS)]
    kd = [A.alloc([128, 512], BF16) for _ in range(NS)]
    qe = [A.alloc([128, 512], BF16) for _ in range(NS)]
    klT = [A.alloc([128, 512], BF16) for _ in range(NS)]
    b_el = P.bufs(NS, "el")
    b_qd, b_kd, b_qe, b_klT = P.bufs(NS), P.bufs(NS), P.bufs(NS), P.bufs(NS)
    kl_sb = [A.alloc([64, 128], BF16) for _ in range(2)]
    b_kl = P.bufs(2)
    sT_sb = [A.alloc([64, 64], BF16) for _ in range(2)]
    b_sT = P.bufs(2)
    ofl = [A.alloc([128, 512], F32) for _ in range(2)]
    b_ofl = P.bufs(2)
    gsl = [A.alloc([128, 512], BF16) for _ in range(2)]
    b_gsl = P.bufs(2)
    osb = [A.alloc([128, 512], F32) for _ in range(2)]
    b_osb = P.bufs(2)
    tsq = [A.alloc([128, 512], F32) for _ in range(2)]
    b_tsq = P.bufs(2)
    abf = [A.alloc([128, 512], BF16) for _ in range(2)]
    b_abf = P.bufs(2)
    out_dmas = []
    cnt = 0
    for d in range(2):
        P.op("dve", lambda e: e.memset(S_f[:], 0.0), writes=[b_S])
        P.op("dve", lambda e: e.memset(S_bf[0][:], 0.0), writes=[b_Sbf[0]])
        sbi = 0
        order = list(range(NBLK)) if d == 0 else [0] + list(range(NBLK - 1, 0, -1))
        for bi in order:
            s0, w = blocks[bi]
            nch = w // 64
            i = cnt % NS
            cnt += 1
            P.dma("sp", qt[i][:, 0:w], s_q[:, s0:s0 + w], reads=[db["q"][bi]], writes=[b_qt[i]])
            P.dma("sp", lf[i][:, 0:w], s_lf[d][:, s0:s0 + w], reads=[db[f"lf{d}"][bi]], writes=[b_lf[i]])
            P.dma("sp", vch[i][:, 0:nch, :], s_v[s0:s0 + w, :].rearrange("(n p) v -> p n v", p=64), reads=[db["v"][bi]], writes=[b_vch[i]])
            be = b_el[i]
            c3 = cum[i][:, 0:w].rearrange("p (n t) -> p n t", t=64)
            G3 = G[i][:, 0:w].rearrange("p (n t) -> p n t", t=64)
            P.op("dve", lambda e, i=i, w=w: e.tensor_tensor_scan(out=cum[i][:, 0:w], data0=reset01[:, 0:w], data1=lf[i][:, 0:w], initial=0.0,
                                                                op0=ALU.mult, op1=ALU.add), reads=[b_reset, b_lf[i]], writes=[be])
            P.op("dve", lambda e, i=i, c3=c3, nch=nch: e.tensor_copy(out=tot[i][:, 0:nch], in_=c3[:, :, 63]), reads=[be], writes=[be])
            tot_b = tot[i][:, 0:nch].unsqueeze(2).to_broadcast([128, nch, 64])
            if d == 0:
                P.op("pool", lambda e, i=i, w=w: e.tensor_copy(out=G[i][:, 0:w], in_=cum[i][:, 0:w]), reads=[be], writes=[be])
                ref = G3[:, :, 31:32]
            else:
                P.op("dve", lambda e, i=i, w=w: e.tensor_tensor(out=G[i][:, 0:w], in0=lf[i][:, 0:w], in1=cum[i][:, 0:w], op=ALU.subtract),
                     reads=[be, b_lf[i]], writes=[be])
                P.op("dve", lambda e, G3=G3, tot_b=tot_b: e.tensor_tensor(out=G3, in0=G3, in1=tot_b, op=ALU.add), reads=[be], writes=[be])
                ref = G3[:, :, 32:33]
            ref_b = ref.to_broadcast([128, nch, 64])
            Gr3 = Gr[i][:, 0:w].rearrange("p (n t) -> p n t", t=64)
            P.op("dve", lambda e, Gr3=Gr3, G3=G3, ref_b=ref_b: e.tensor_tensor(out=Gr3, in0=G3, in1=ref_b, op=ALU.subtract), reads=[be], writes=[be])
            P.op("act", lambda e, i=i, w=w: e.activation(out=kkt[i][:, 0:w], in_=lf[i][:, 0:w], func=ACTF.Exp), reads=[b_lf[i]], writes=[be])
            P.op("pool", lambda e, i=i, w=w: e.tensor_scalar(out=kkt[i][:, 0:w], in0=kkt[i][:, 0:w], scalar1=-1.0, scalar2=1.0, op0=ALU.mult, op1=ALU.add),
                 reads=[be], writes=[be])
            P.op("act", lambda e, i=i, w=w: e.activation(out=EE[i][:, 0:w], in_=Gr[i][:, 0:w], func=ACTF.Exp), reads=[be], writes=[be])
            P.op("dve", lambda e, i=i, w=w: e.tensor_tensor(out=qd[i][:, 0:w], in0=qt[i][:, 0:w], in1=EE[i][:, 0:w], op=ALU.mult),
                 reads=[be, b_qt[i]], writes=[b_qd[i]])
            P.op("act", lambda e, i=i, w=w: e.activation(out=EE[i][:, 0:w], in_=Gr[i][:, 0:w], func=ACTF.Exp, scale=-1.0), reads=[be], writes=[be])
            P.op("dve", lambda e, i=i, w=w: e.tensor_tensor(out=kd[i][:, 0:w], in0=kkt[i][:, 0:w], in1=EE[i][:, 0:w], op=ALU.mult),
                 reads=[be], writes=[b_kd[i]])
            P.op("act", lambda e, i=i, w=w: e.activation(out=EE[i][:, 0:w], in_=G[i][:, 0:w], func=ACTF.Exp), reads=[be], writes=[be])
            P.op("dve", lambda e, i=i, w=w: e.tensor_tensor(out=qe[i][:, 0:w], in0=qt[i][:, 0:w], in1=EE[i][:, 0:w], op=ALU.mult),
                 reads=[be, b_qt[i]], writes=[b_qe[i]])
            P.op("dve", lambda e, Gr3=Gr3, G3=G3, tot_b=tot_b: e.tensor_tensor(out=Gr3, in0=tot_b, in1=G3, op=ALU.subtract), reads=[be], writes=[be])
            P.op("act", lambda e, i=i, w=w: e.activation(out=EE[i][:, 0:w], in_=Gr[i][:, 0:w], func=ACTF.Exp), reads=[be], writes=[be])
            P.op("dve", lambda e, i=i, w=w: e.tensor_tensor(out=klT[i][:, 0:w], in0=kkt[i][:, 0:w], in1=EE[i][:, 0:w], op=ALU.mult),
                 reads=[be], writes=[b_klT[i]])
            P.op("act", lambda e, i=i, nch=nch: e.activation(out=etot[i][:, 0:nch], in_=tot[i][:, 0:nch], func=ACTF.Exp), reads=[be], writes=[be])
            po_i = cnt % 2
            po, bpo = pb[po_i], b_pb[po_i]
            chunks = list(range(nch)) if d == 0 else list(range(nch - 1, -1, -1))
            for ci, n in enumerate(chunks):
                c0, c1 = n * 64, (n + 1) * 64
                j = ci % 2
                P.op("pe", lambda e, j=j, i=i, c0=c0, c1=c1: e.transpose(out=pbf[j][0:64, 0:128], in_=klT[i][:, c0:c1], identity=ident[:]),
                     reads=[b_klT[i], b_ident], writes=[b_pbf[j]])
                P.op("act", lambda e, j=j: e.activation(out=kl_sb[j][:], in_=pbf[j][0:64, 0:128], func=ACTF.Copy), reads=[b_pbf[j]], writes=[b_kl[j]])
                ps_s, bps = pb[2 + j], b_pb[2 + j]
                P.op("pe", lambda e, ps_s=ps_s, i=i, c0=c0, c1=c1: e.matmul(ps_s[0:64, 0:64], lhsT=kd[i][:, c0:c1], rhs=qd[i][:, c0:c1], start=True, stop=True),
                     reads=[b_kd[i], b_qd[i]], writes=[bps])
                P.op("dve", lambda e, ps_s=ps_s, j=j, d=d: e.tensor_tensor(out=sT_sb[j][:], in0=ps_s[0:64, 0:64], in1=masks[:, d, :], op=ALU.mult),
                     reads=[bps, b_masks], writes=[b_sT[j]])
                P.op("pe", lambda e, po=po, i=i, n=n, j=j, c0=c0, c1=c1: e.matmul(po[:, c0:c1], lhsT=vch[i][:, n, :], rhs=sT_sb[j][:], start=True, stop=False),
                     reads=[b_vch[i], b_sT[j]], writes=[bpo])
                P.op("pe", lambda e, po=po, i=i, sbi=sbi, c0=c0, c1=c1: e.matmul(po[:, c0:c1], lhsT=S_bf[sbi][:], rhs=qe[i][:, c0:c1], start=False, stop=True),
                     reads=[b_Sbf[sbi], b_qe[i]], writes=[bpo])
                pst, bpst = pb[4], b_pb[4]
                P.op("pe", lambda e, pst=pst, j=j, i=i, n=n: e.matmul(pst[:, 0:128], lhsT=kl_sb[j][:], rhs=vch[i][:, n, :], start=True, stop=True),
                     reads=[b_kl[j], b_vch[i]], writes=[bpst])
                P.op("dve", lambda e, pst=pst, i=i, n=n: e.scalar_tensor_tensor(out=S_f[:], in0=S_f[:], scalar=etot[i][:, n:n + 1], in1=pst[:, 0:128],
                                                                               op0=ALU.mult, op1=ALU.add), reads=[b_S, be, bpst], writes=[b_S])
                sbi = 1 - sbi
                P.op("act", lambda e, sbi=sbi: e.activation(out=S_bf[sbi][:], in_=S_f[:], func=ACTF.Copy), reads=[b_S], writes=[b_Sbf[sbi]])
            if d == 0:
                oi = cnt % 2
                P.op("act", lambda e, oi=oi, po=po, w=w: e.activation(out=osb[oi][:, 0:w], in_=po[:, 0:w], func=ACTF.Copy), reads=[bpo], writes=[b_osb[oi]])
                P.dma("sp", s_of[:, s0:s0 + w], osb[oi][:, 0:w], reads=[b_osb[oi]], writes=[db["of"][bi]])
            else:
                oi = cnt % 2
                P.dma("sp", ofl[oi][:, 0:w], s_of[:, s0:s0 + w], reads=[db["of"][bi]], writes=[b_ofl[oi]])
                P.dma("sp", gsl[oi][:, 0:w], s_g[:, s0:s0 + w], reads=[db["g"][bi]], writes=[b_gsl[oi]])
                P.op("dve", lambda e, oi=oi, po=po, w=w: e.tensor_tensor(out=osb[oi][:, 0:w], in0=po[:, 0:w], in1=ofl[oi][:, 0:w], op=ALU.add),
                     reads=[bpo, b_ofl[oi]], writes=[b_osb[oi]])
                P.op("act", lambda e, oi=oi, w=w: e.activation(out=tsq[oi][:, 0:w], in_=osb[oi][:, 0:w], func=ACTF.Square), reads=[b_osb[oi]], writes=[b_tsq[oi]])
                pn, bpn = pb[5], b_pb[5]
                P.op("pe", lambda e, pn=pn, oi=oi, w=w: e.matmul(pn[:, 0:w], lhsT=ones[:], rhs=tsq[oi][:, 0:w], start=True, stop=True),
                     reads=[b_ones, b_tsq[oi]], writes=[bpn])
                P.op("act", lambda e, pn=pn, oi=oi, w=w: e.activation(out=tsq[oi][:, 0:w], in_=pn[:, 0:w], func=ACTF.Sqrt, bias=EPS, scale=1.0 / 128),
                     reads=[bpn], writes=[b_tsq[oi]])
                P.op("dve", lambda e, oi=oi, w=w: e.reciprocal(out=tsq[oi][:, 0:w], in_=tsq[oi][:, 0:w]), reads=[b_tsq[oi]], writes=[b_tsq[oi]])
                P.op("dve", lambda e, oi=oi, w=w: e.tensor_tensor(out=osb[oi][:, 0:w], in0=osb[oi][:, 0:w], in1=tsq[oi][:, 0:w], op=ALU.mult),
                     reads=[b_osb[oi], b_tsq[oi]], writes=[b_osb[oi]])
                P.op("dve", lambda e, oi=oi, w=w: e.scalar_tensor_tensor(out=abf[oi][:, 0:w], in0=osb[oi][:, 0:w], scalar=gvec[:, 0:1], in1=gsl[oi][:, 0:w],
                                                                        op0=ALU.mult, op1=ALU.mult), reads=[b_osb[oi], b_gsl[oi], b_gvec], writes=[b_abf[oi]])
                out_dmas.append(P.dma("sp", aT[:, s0:s0 + w], abf[oi][:, 0:w], reads=[b_abf[oi]]))

    if upto == 2:
        P.barrier()
        o3 = P.dma("sp", bT[:, 0:8], KT[:, 0:8])
        P.emit(final_waits=out_dmas + [o3])
        return nc, P
    P.barrier()
    A.reset()
    SCALE = 128 ** -0.5
    qb_s = [A.alloc([128, 512], BF16) for _ in range(2)]
    b_qbs = P.bufs(2)
    NPT = 4
    pT_sb = [A.alloc([128, 512], BF16) for _ in range(NPT)]
    b_pTs = P.bufs(NPT)
    rz = [A.alloc([128, 512], F32) for _ in range(2)]
    b_rz = P.bufs(2)
    ob = [A.alloc([128, 512], BF16) for _ in range(2)]
    b_ob = P.bufs(2)
    allKT = b_KT
    allV = b_V
    kcount = 0
    for bi, (s0, w) in enumerate(blocks):
        i = bi % 2
        P.dma("sp", qb_s[i][:, 0:w], s_qb[:, s0:s0 + w], reads=[db["qb"][bi]], writes=[b_qbs[i]])
        nkc = (NCTX // 128) if bi == 0 else NT128
        pO, bpO = pb[3 + i], b_pb[3 + i]
        pZ, bpZ = pb[5 + i], b_pb[5 + i]
        for kc in range(nkc):
            kblk = 0 if kc < 2 else 1 + (kc * 128 - NCTX) // 512
            si = kcount % 3
            ti = kcount % NPT
            kcount += 1
            pS, bpS = pb[si], b_pb[si]
            P.op("pe", lambda e, pS=pS, kc=kc, i=i, w=w: e.matmul(pS[:, 0:w], lhsT=KT[:, kc * 128:(kc + 1) * 128], rhs=qb_s[i][:, 0:w], start=True, stop=True),
                 reads=[allKT[kblk], b_qbs[i]], writes=[bpS])
            P.op("act", lambda e, pS=pS, ti=ti, w=w: e.activation(out=pT_sb[ti][:, 0:w], in_=pS[:, 0:w], func=ACTF.Exp, scale=SCALE),
                 reads=[bpS], writes=[b_pTs[ti]])
            P.op("pe", lambda e, pO=pO, kc=kc, ti=ti, w=w, nkc=nkc: e.matmul(pO[:, 0:w], lhsT=Vres[:, kc, :], rhs=pT_sb[ti][:, 0:w], start=(kc == 0), stop=(kc == nkc - 1)),
                 reads=[allV[kblk], b_pTs[ti]], writes=[bpO])
            P.op("pe", lambda e, pZ=pZ, kc=kc, ti=ti, w=w, nkc=nkc: e.matmul(pZ[:, 0:w], lhsT=ones_bf[:], rhs=pT_sb[ti][:, 0:w], start=(kc == 0), stop=(kc == nkc - 1)),
                 reads=[b_onesbf, b_pTs[ti]], writes=[bpZ])
        P.op("dve", lambda e, i=i, pZ=pZ, w=w: e.reciprocal(out=rz[i][:, 0:w], in_=pZ[:, 0:w]), reads=[bpZ], writes=[b_rz[i]])
        P.op("dve", lambda e, i=i, pO=pO, w=w: e.tensor_tensor(out=ob[i][:, 0:w], in0=pO[:, 0:w], in1=rz[i][:, 0:w], op=ALU.mult),
             reads=[bpO, b_rz[i]], writes=[b_ob[i]])
        out_dmas.append(P.dma("sp", bT[:, s0:s0 + w], ob[i][:, 0:w], reads=[b_ob[i]]))
    st = P.emit(final_waits=out_dmas)
    print("LB stats", st, flush=True)
    return nc, P


def lb_inputs(inp, hT_all, nblk=32):
    NLAT = nblk * 512
    w_in = np.asarray(inp["ev_w_in"][0])
    consts = lb_consts()
    n_tok = 16384
    rows = n_tok // 64
    row = np.repeat(np.arange(rows, dtype=np.float32), 64)
    col = np.tile(np.arange(64, dtype=np.float32), rows)
    n_freq = 32
    inv = (10000.0 ** (-np.arange(n_freq, dtype=np.float32) / n_freq)).astype(np.float32)
    ang = np.concatenate([row[:, None] * inv, col[:, None] * inv], -1)
    cos = np.cos(ang).astype(np.float32)
    sin = np.sin(ang).astype(np.float32)
    cosT = np.ascontiguousarray(np.concatenate([cos, cos], -1).T[:, :NLAT])
    sinT = np.ascontiguousarray(np.concatenate([sin, sin], -1).T[:, :NLAT])
    hTs = np.ascontiguousarray(hT_all[:, :NCTX + NLAT])
    maps = []
    for j in range(NCORE):
        c = lambda base, wd=128: w_in[:, base + j * wd: base + (j + 1) * wd]
        kvh = j // 4
        wq, wff, wfb, wi, wg = c(0), c(1024), c(2048), c(3072), c(4096)
        wqb = c(5120)
        wkb = w_in[:, 6144 + kvh * 128: 6144 + (kvh + 1) * 128]
        wvb = w_in[:, 6400 + kvh * 128: 6400 + (kvh + 1) * 128]
        wfm = np.ascontiguousarray(np.stack([wq, wff, wfb, wg, wqb, wkb]))
        wtm = np.ascontiguousarray(np.concatenate([wi, wvb], 1))
        lbv = np.ascontiguousarray(np.asarray(inp["hgrn_lb"])[:, :, j * 128:(j + 1) * 128].transpose(2, 0, 1))
        gvec = np.ascontiguousarray(np.stack([inp["hgrn_norm_g"][0], inp["gqa_q_norm_g"][0], inp["gqa_k_norm_g"][0]], -1))
        m = {"hT": hTs, "wfm": wfm, "wtm": wtm, "lbv": lbv, "gvec": gvec, "cosT": cosT, "sinT": sinT}
        m.update(consts)
        maps.append(m)
    return maps


def build_lc(ntile=18, nctx_tile=2):
    nc = new_nc()
    NT = ntile * 128
    oT = din(nc, "oT", [D, NT], BF16)
    wo = din(nc, "wo", [D, D])
    xin = din(nc, "xin", [NT, D])
    modl = din(nc, "modl", [1, 12288])
    modc = din(nc, "modc", [1, 12288])
    lng = din(nc, "lng", [1, D])
    lnb = din(nc, "lnb", [1, D])
    wr = din(nc, "wr", [D, 16])
    ident_d = din(nc, "ident", [128, 128])
    xmid = dout(nc, "xmid", [NT, D])
    h2o = dout(nc, "h2", [NT, D], BF16)
    affo = dout(nc, "aff", [NT, 16])
    P = Prog(nc)
    ident = P.sb("ident_s", [128, 128], F32)
    b_ident = P.buf()
    P.dma("sp", ident[:], ident_d, writes=[b_ident])
    wr_s = P.sb("wr_s", [128, 16, 16], F32)
    b_wr = P.buf()
    P.dma("sp", wr_s[:], wr.rearrange("(kc p) n -> p kc n", p=128), writes=[b_wr])
    lng_s, b_lng = load_bcast(P, "lng_s", lng, D)
    lnb_s, b_lnb = load_bcast(P, "lnb_s", lnb, D)
    wo_bf = P.sb("wo_bf", [128, 16, D], BF16)
    b_wo = P.buf()
    pb = [P.ps(f"pb{i}", [128, 512], F32) for i in range(8)]
    b_pb = P.bufs(8, "pb", excl=True)
    A = Arena(P, "arena", 22 * 1024)
    wst = [A.alloc([128, 16, 256], F32) for _ in range(2)]
    b_wst = P.bufs(2)
    for c in range(8):
        i = c % 2
        P.dma("sp", wst[i][:], wo.rearrange("(kc p) n -> p kc n", p=128)[:, :, c * 256:(c + 1) * 256], writes=[b_wst[i]])
        if c % 2:
            P.op("act", lambda e, i=i, c=c: e.activation(out=wo_bf[:, :, c * 256:(c + 1) * 256], in_=wst[i][:], func=ACTF.Copy), reads=[b_wst[i]], writes=[b_wo])
        else:
            P.op("dve", lambda e, i=i, c=c: e.tensor_copy(out=wo_bf[:, :, c * 256:(c + 1) * 256], in_=wst[i][:]), reads=[b_wst[i]], writes=[b_wo])
    P.barrier()
    A.reset()
    g1 = A.alloc([128, D], F32)
    sh2 = A.alloc([128, D], F32)
    sc2 = A.alloc([128, D], F32)
    b_g1, b_sh2, b_sc2 = P.buf(), P.buf(), P.buf()
    ob = A.alloc([128, 16, 512], BF16)
    b_ob = P.buf()
    xt = A.alloc([128, D], F32)
    zt = A.alloc([128, D], F32)
    xm = A.alloc([128, D], F32)
    h2 = A.alloc([128, D], F32)
    h2T = A.alloc([128, 16, 128], F32)
    h2b = A.alloc([128, D], BF16)
    b_xt, b_zt, b_xm, b_h2, b_h2T, b_h2b = (P.buf() for _ in range(6))
    sm = A.alloc([128, 64], F32)
    b_sm = P.buf()
    ln = LN(P, "ln")
    outs = []

    def load_modset(m):
        P.dma("sp", g1[:], m[0:1, 2 * D:3 * D].partition_broadcast(128), writes=[b_g1])
        P.dma("sp", sh2[:], m[0:1, 3 * D:4 * D].partition_broadcast(128), writes=[b_sh2])
        P.dma("sp", sc2[:], m[0:1, 4 * D:5 * D].partition_broadcast(128), writes=[b_sc2])
        P.op("pool", lambda e: e.tensor_scalar(out=sc2[:], in0=sc2[:], scalar1=1.0, scalar2=None, op0=ALU.add), reads=[b_sc2], writes=[b_sc2])

    blocks = ([(0, nctx_tile)] if nctx_tile else []) + [(t, min(4, ntile - t)) for t in range(nctx_tile, ntile, 4)]
    for bi, (t0, nt) in enumerate(blocks):
        if bi == 0:
            load_modset(modc if nctx_tile else modl)
        elif bi == 1 and nctx_tile:
            load_modset(modl)
        w = nt * 128
        P.dma("sp", ob[:, :, 0:w], oT.rearrange("(kc p) n -> p kc n", p=128)[:, :, t0 * 128:t0 * 128 + w], writes=[b_ob])
        for tb in range(nt):
            t = t0 + tb
            rows = slice(t * 128, (t + 1) * 128)
            P.dma("sp", xt[:], xin[rows, :], writes=[b_xt])
            for cc in range(4):
                for kc in range(16):
                    P.op("pe", lambda e, cc=cc, kc=kc, tb=tb: e.matmul(pb[cc][:], lhsT=ob[:, kc, tb * 128:(tb + 1) * 128], rhs=wo_bf[:, kc, cc * 512:(cc + 1) * 512],
                                                                      start=(kc == 0), stop=(kc == 15)), reads=[b_ob, b_wo], writes=[b_pb[cc]])
                P.op("dve", lambda e, cc=cc: e.tensor_tensor(out=zt[:, cc * 512:(cc + 1) * 512], in0=pb[cc][:], in1=g1[:, cc * 512:(cc + 1) * 512], op=ALU.mult),
                     reads=[b_pb[cc], b_g1], writes=[b_zt])
            P.op("dve", lambda e: e.scalar_tensor_tensor(out=zt[:], in0=xt[:], scalar=ALPHA, in1=zt[:], op0=ALU.mult, op1=ALU.add),
                 reads=[b_xt, b_zt], writes=[b_zt])
            ln.norm(xm[:], zt[:], b_zt, b_xm)
            P.op("dve", lambda e: e.tensor_tensor(out=xm[:], in0=xm[:], in1=lng_s[:], op=ALU.mult), reads=[b_xm, b_lng], writes=[b_xm])
            P.op("pool", lambda e: e.tensor_tensor(out=xm[:], in0=xm[:], in1=lnb_s[:], op=ALU.add), reads=[b_xm, b_lnb], writes=[b_xm])
            outs.append(P.dma("sp", xmid[rows, :], xm[:], reads=[b_xm]))
            ln.norm(h2[:], xm[:], b_xm, b_h2)
            P.op("dve", lambda e: e.tensor_tensor(out=h2[:], in0=h2[:], in1=sc2[:], op=ALU.mult), reads=[b_h2, b_sc2], writes=[b_h2])
            P.op("pool", lambda e: e.tensor_tensor(out=h2[:], in0=h2[:], in1=sh2[:], op=ALU.add), reads=[b_h2, b_sh2], writes=[b_h2])
            P.op("act", lambda e: e.activation(out=h2b[:], in_=h2[:], func=ACTF.Copy), reads=[b_h2], writes=[b_h2b])
            outs.append(P.dma("sp", h2o[rows, :], h2b[:], reads=[b_h2b]))
            for qi in range(4):
                pq, bpq = pb[4 + qi % 3], b_pb[4 + qi % 3]
                for k in range(4):
                    kc = qi * 4 + k
                    P.op("pe", lambda e, pq=pq, k=k, kc=kc: e.transpose(out=pq[:, k * 128:(k + 1) * 128], in_=h2[:, kc * 128:(kc + 1) * 128], identity=ident[:]),
                         reads=[b_h2, b_ident], writes=[bpq])
                P.op("act", lambda e, pq=pq, qi=qi: e.activation(out=h2T[:, qi * 4:(qi + 1) * 4, :], in_=pq[:].rearrange("p (k t) -> p k t", k=4), func=ACTF.Copy),
                     reads=[bpq], writes=[b_h2T])
            for kc in range(16):
                P.op("pe", lambda e, kc=kc: e.matmul(pb[7][:, 0:16], lhsT=h2T[:, kc, :], rhs=wr_s[:, kc, :], start=(kc == 0), stop=(kc == 15)),
                     reads=[b_h2T, b_wr], writes=[b_pb[7]])
            P.op("dve", lambda e: e.tensor_reduce(out=sm[:, 16:17], in_=pb[7][:, 0:16], axis=AX.X, op=ALU.max), reads=[b_pb[7]], writes=[b_sm])
            P.op("dve", lambda e: e.tensor_scalar(out=sm[:, 17:18], in0=sm[:, 16:17], scalar1=-1.0, scalar2=None, op0=ALU.mult), reads=[b_sm], writes=[b_sm])
            P.op("act", lambda e: e.activation(out=sm[:, 0:16], in_=pb[7][:, 0:16], func=ACTF.Exp, bias=sm[:, 17:18], scale=1.0, accum_out=sm[:, 18:19]),
                 reads=[b_pb[7], b_sm], writes=[b_sm])
            P.op("dve", lambda e: e.reciprocal(out=sm[:, 18:19], in_=sm[:, 18:19]), reads=[b_sm], writes=[b_sm])
            P.op("dve", lambda e: e.tensor_scalar(out=sm[:, 32:48], in0=sm[:, 0:16], scalar1=sm[:, 18:19], scalar2=None, op0=ALU.mult), reads=[b_sm], writes=[b_sm])
            outs.append(P.dma("sp", affo[rows, :], sm[:, 32:48], reads=[b_sm]))
    st = P.emit(final_waits=outs)
    print("LC stats", st, flush=True)
    return nc, P


NE = 16
FF = 1024


def ld_consts():
    p = np.arange(128)
    gmat = (p[:, None] // 8 == p[None, :] // 8).astype(np.float32)
    sel8 = np.zeros((128, 16), np.float32)
    sel8[np.arange(16) * 8, np.arange(16)] = 1.0
    tri = (p[:, None] < p[None, :]).astype(np.float32)
    iota = np.broadcast_to(np.arange(128, dtype=np.float32)[None, :], (128, 128)).copy()
    return {"gmat": gmat, "sel8": sel8, "tri": tri, "ones": np.ones((128, 128), np.float32), "iota3": iota,
            "ident_bf": np.eye(128, dtype=np.float32).astype(BF)}


def build_ld(groups, nseg_lat, kcap_lat, has_ctx, upto=9):
    nc = new_nc()
    ntile = sum(len(g[0]) for g in groups)
    NT = ntile * 128
    NLAT_ALL = nseg_lat * 8
    affT_lat = din(nc, "affT_lat", [16, NLAT_ALL])
    affT_ctx = din(nc, "affT_ctx", [16, 256]) if has_ctx else None
    aff_own = din(nc, "aff_own", [NT, 16])
    h2 = din(nc, "h2", [NT, D], BF16)
    xmid = din(nc, "xmid", [NT, D])
    wg = din(nc, "wg", [NE, D, FF])
    wu = din(nc, "wu", [NE, D, FF])
    wd = din(nc, "wd", [NE, FF, D])
    modl = din(nc, "modl", [1, 12288])
    modc = din(nc, "modc", [1, 12288])
    lng = din(nc, "lng", [1, D])
    lnb = din(nc, "lnb", [1, D])
    cd = {k: din(nc, k, list(v.shape), BF16 if v.dtype == BF else F32) for k, v in ld_consts().items()}
    xout = dout(nc, "xout", [NT, D])
    NG = len(groups)
    soff = []
    o = 0
    for g in groups:
        soff.append(o)
        o += g[1]
    NSLOT = o
    Yd = dscr(nc, "Yd", [NE, NSLOT, D], BF16)
    P = Prog(nc)
    b_Y = [[P.buf() for _ in range(NG)] for _ in range(NE)]

    def cload(name, shape, dt):
        t = P.sb(name + "_s", shape, dt)
        b = P.buf(name)
        P.dma("sp", t[:], cd[name], writes=[b])
        return t, b

    gmat, b_gmat = cload("gmat", [128, 128], F32)
    sel8, b_sel8 = cload("sel8", [128, 16], F32)
    tri, b_tri = cload("tri", [128, 128], F32)
    ones, b_ones = cload("ones", [128, 128], F32)
    ident, b_ident = cload("ident_bf", [128, 128], BF16)
    iota3, b_iota = cload("iota3", [128, 128], F32)
    pb = [P.ps(f"pb{i}", [128, 512], F32) for i in range(8)]
    b_pb = P.bufs(8, "pb", excl=True)
    A = Arena(P, "arena", 44 * 1024)

    thr = P.sb("thr", [128, 2, 16], F32)
    b_thr = P.buf()
    sets = [(affT_lat, nseg_lat, float(kcap_lat), 0)]
    if has_ctx:
        sets.append((affT_ctx, 32, 32.0, 1))
    for (src, nseg, kcap, which) in sets:
        A.reset()
        at = A.alloc([128, nseg], F32)
        junk = A.alloc([128, nseg], F32)
        bs = A.alloc([128, 8], F32)
        b_at, b_bs = P.buf(), P.buf()
        P.dma("sp", at[:], src.rearrange("e (s n) -> (e s) n", s=8), writes=[b_at])
        P.op("dve", lambda e, bs=bs: e.memset(bs[:], 0.0), writes=[b_bs])
        for it in range(32):
            dk = 2.0 ** -(it + 1)
            P.op("dve", lambda e, bs=bs, dk=dk: e.tensor_scalar(out=bs[:, 1:2], in0=bs[:, 0:1], scalar1=dk, scalar2=None, op0=ALU.add), reads=[b_bs], writes=[b_bs])
            P.op("dve", lambda e, bs=bs, at=at, junk=junk: e.tensor_scalar(out=junk[:], in0=at[:], scalar1=bs[:, 1:2], scalar2=None, op0=ALU.is_ge, op1=ALU.add,
                                                                         accum_out=bs[:, 2:3]), reads=[b_bs, b_at], writes=[b_bs])
            P.op("pe", lambda e, bs=bs: e.matmul(pb[0][:, 0:1], lhsT=gmat[:], rhs=bs[:, 2:3], start=True, stop=True), reads=[b_gmat, b_bs], writes=[b_pb[0]])
            P.op("dve", lambda e, bs=bs, dk=dk, kcap=kcap: e.tensor_scalar(out=bs[:, 3:4], in0=pb[0][:, 0:1], scalar1=kcap, scalar2=dk, op0=ALU.is_ge, op1=ALU.mult),
                 reads=[b_pb[0]], writes=[b_bs])
            P.op("dve", lambda e, bs=bs: e.tensor_tensor(out=bs[:, 0:1], in0=bs[:, 0:1], in1=bs[:, 3:4], op=ALU.add), reads=[b_bs], writes=[b_bs])
        tsel = A.alloc([128, 16], F32)
        P.op("dve", lambda e, bs=bs, tsel=tsel: e.tensor_scalar(out=tsel[:], in0=sel8[:], scalar1=bs[:, 0:1], scalar2=None, op0=ALU.mult), reads=[b_bs, b_sel8], writes=[b_at])
        P.op("pe", lambda e, tsel=tsel: e.matmul(pb[1][:, 0:16], lhsT=ones[:], rhs=tsel[:], start=True, stop=True), reads=[b_ones, b_at], writes=[b_pb[1]])
        P.op("dve", lambda e, which=which: e.tensor_copy(out=thr[:, which, :], in_=pb[1][:, 0:16]), reads=[b_pb[1]], writes=[b_thr])
    P.barrier()
    A.reset()

    aff = P.sb("aff_s", [128, ntile, 16], F32)
    b_aff = P.buf()
    P.dma("sp", aff[:], aff_own.rearrange("(t p) e -> p t e", p=128), writes=[b_aff])
    mask = P.sb("mask_s", [128, ntile, 16], F32)
    posm = P.sb("posm_s", [128, ntile, 16], F32)
    affhl = P.sb("affhl", [128, ntile, 16, 2], BF16)
    b_mask, b_posm, b_affhl = P.buf(), P.buf(), P.buf()
    tmp16 = P.sb("tmp16", [128, ntile, 16], F32)
    b_t16 = P.buf()
    tile_group = {}
    ti = 0
    for gi, (tiles, ns) in enumerate(groups):
        for t in tiles:
            tile_group[t] = gi
    is_ctx_group = [has_ctx and gi == 0 for gi in range(NG)]
    for gi, (tiles, ns) in enumerate(groups):
        which = 1 if is_ctx_group[gi] else 0
        for k, t in enumerate(tiles):
            P.op("dve", lambda e, t=t, which=which: e.tensor_tensor(out=mask[:, t, :], in0=aff[:, t, :], in1=thr[:, which, :], op=ALU.is_ge),
                 reads=[b_aff, b_thr], writes=[b_mask])
            P.op("pe", lambda e, t=t, k=k: e.matmul(pb[2][:, 0:16], lhsT=tri[:], rhs=mask[:, t, :], start=True, stop=(k == 0)),
                 reads=[b_tri, b_mask], writes=[b_pb[2]])
            for k2 in range(k):
                P.op("pe", lambda e, t2=tiles[k2], k2=k2, k=k: e.matmul(pb[2][:, 0:16], lhsT=ones[:], rhs=mask[:, t2, :], start=False, stop=(k2 == k - 1)),
                     reads=[b_ones, b_mask], writes=[b_pb[2]])
            P.op("dve", lambda e, t=t: e.scalar_tensor_tensor(out=posm[:, t, :], in0=pb[2][:, 0:16], scalar=1.0, in1=mask[:, t, :], op0=ALU.add, op1=ALU.mult),
                 reads=[b_pb[2], b_mask], writes=[b_posm])
    P.op("dve", lambda e: e.tensor_scalar(out=posm[:], in0=posm[:], scalar1=-1.0, scalar2=None, op0=ALU.add), reads=[b_posm], writes=[b_posm])
    P.op("dve", lambda e: e.tensor_copy(out=affhl[:, :, :, 0], in_=aff[:]), reads=[b_aff], writes=[b_affhl])
    P.op("dve", lambda e: e.tensor_tensor(out=tmp16[:], in0=aff[:], in1=affhl[:, :, :, 0], op=ALU.subtract), reads=[b_aff, b_affhl], writes=[b_t16])
    P.op("dve", lambda e: e.tensor_copy(out=affhl[:, :, :, 1], in_=tmp16[:]), reads=[b_t16], writes=[b_affhl])

    if upto == 0:
        thr_o = dout(nc, "thr_o", [128, 2, 16])
        posm_o = dout(nc, "posm_o", [128, ntile, 16])
        o1 = P.dma("sp", thr_o, thr[:], reads=[b_thr])
        o2 = P.dma("sp", posm_o, posm[:], reads=[b_posm])
        P.emit(final_waits=[o1, o2])
        return nc, P
    h2s = A.alloc([128, ntile, D], BF16)
    b_h2s = P.buf()
    for t in range(ntile):
        P.dma("sp", h2s[:, t, :], h2[t * 128:(t + 1) * 128, :], writes=[b_h2s])
    sel = [A.alloc([128, ntile, 128], BF16) for _ in range(2)]
    b_sel = P.bufs(2)
    Xg = A.alloc([128, 16, NSLOT], BF16)
    b_Xg = P.buf()
    wsl = A.alloc([128, NG, 2], F32)
    wsum = A.alloc([128, NG], F32)
    b_wsl = P.buf()
    P.op("dve", lambda e: e.memset(wsl[:], 0.0), writes=[b_wsl])
    hidT = A.alloc([128, 8, NSLOT], BF16)
    b_hid = P.buf()
    WST = 2
    wst = [A.alloc([128, 16, 256], F32) for _ in range(WST)]
    b_wst = P.bufs(WST)
    NWB = 3
    wbf = [A.alloc([128, 16, 256], BF16) for _ in range(NWB)]
    b_wbf = P.bufs(NWB)
    sg = [A.alloc([128, 512], F32) for _ in range(2)]
    b_sg = P.bufs(2)
    yst = [A.alloc([128, 512], BF16) for _ in range(2)]
    b_yst = P.bufs(2)
    kw = [0, 0, 0]
    cast_eng = ["dve", "act", "pool"]
    pieces = []
    c = 0
    while c < NSLOT:
        pieces.append((c, min(512, NSLOT - c)))
        c += 512
    for e_ in range(NE):
        si = e_ % 2
        for t in range(ntile):
            P.op("dve", lambda e, si=si, t=t, e_=e_: e.tensor_scalar(out=sel[si][:, t, :], in0=iota3[:], scalar1=posm[:, t, e_:e_ + 1], scalar2=None, op0=ALU.is_equal),
                 reads=[b_posm, b_iota], writes=[b_sel[si]])
        for fc in range(16):
            for gi, (tiles, ns) in enumerate(groups):
                pg, bpg = pb[(fc * NG + gi) % 2], b_pb[(fc * NG + gi) % 2]
                for k, t in enumerate(tiles):
                    P.op("pe", lambda e, pg=pg, gi=gi, t=t, k=k, ns=ns, si=si, fc=fc, nt_=len(tiles): e.matmul(
                        pg[:, 0:ns], lhsT=h2s[:, t, fc * 128:(fc + 1) * 128], rhs=sel[si][:, t, 0:ns],
                        start=(k == 0), stop=(k == nt_ - 1)), reads=[b_h2s, b_sel[si]], writes=[bpg])
                P.op("act" if gi % 2 else "dve",
                     (lambda e, pg=pg, gi=gi, ns=ns, fc=fc: e.activation(out=Xg[:, fc, soff[gi]:soff[gi] + ns], in_=pg[:, 0:ns], func=ACTF.Copy)) if gi % 2 else
                     (lambda e, pg=pg, gi=gi, ns=ns, fc=fc: e.tensor_copy(out=Xg[:, fc, soff[gi]:soff[gi] + ns], in_=pg[:, 0:ns])),
                     reads=[bpg], writes=[b_Xg])
        for gi, (tiles, ns) in enumerate(groups):
            for k, t in enumerate(tiles):
                P.op("pe", lambda e, gi=gi, t=t, k=k, ns=ns, si=si, e_=e_, nt_=len(tiles): e.matmul(pb[6][0:ns, 0:2], lhsT=sel[si][:, t, 0:ns], rhs=affhl[:, t, e_, :],
                                                                                              start=(k == 0), stop=(k == nt_ - 1)),
                     reads=[b_sel[si], b_affhl], writes=[b_pb[6]])
            P.op("dve", lambda e, gi=gi, ns=ns: e.tensor_copy(out=wsl[0:ns, gi, :], in_=pb[6][0:ns, 0:2]), reads=[b_pb[6]], writes=[b_wsl])
        P.op("dve", lambda e: e.tensor_tensor(out=wsum[:], in0=wsl[:, :, 0], in1=wsl[:, :, 1], op=ALU.add), reads=[b_wsl], writes=[b_wsl])
        for fp in range(4):
            wts = []
            for (wsrc) in (wg, wu):
                i = kw[0] % WST
                kw[0] += 1
                j = kw[1] % NWB
                kw[1] += 1
                P.dma("sp", wst[i][:], wsrc[e_].rearrange("(kc p) f -> p kc f", p=128)[:, :, fp * 256:(fp + 1) * 256], writes=[b_wst[i]])
                ce = cast_eng[kw[2] % 3]
                kw[2] += 1
                if ce == "act":
                    P.op("act", lambda e, i=i, j=j: e.activation(out=wbf[j][:], in_=wst[i][:], func=ACTF.Copy), reads=[b_wst[i]], writes=[b_wbf[j]])
                else:
                    P.op(ce, lambda e, i=i, j=j: e.tensor_copy(out=wbf[j][:], in_=wst[i][:]), reads=[b_wst[i]], writes=[b_wbf[j]])
                wts.append(j)
            for fl in range(2):
                fc = fp * 2 + fl
                for (c0, cw) in pieces:
                    pgt, bpgt = pb[2 + fl], b_pb[2 + fl]
                    pup, bpup = pb[4 + fl], b_pb[4 + fl]
                    for (pp, bpp, j) in ((pgt, bpgt, wts[0]), (pup, bpup, wts[1])):
                        for kc in range(16):
                            P.op("pe", lambda e, pp=pp, j=j, kc=kc, fl=fl, c0=c0, cw=cw: e.matmul(pp[:, 0:cw], lhsT=wbf[j][:, kc, fl * 128:(fl + 1) * 128], rhs=Xg[:, kc, c0:c0 + cw],
                                                                                              start=(kc == 0), stop=(kc == 15)), reads=[b_wbf[j], b_Xg], writes=[bpp])
                    s_i = fl
                    P.op("act", lambda e, pgt=pgt, s_i=s_i, cw=cw: e.activation(out=sg[s_i][:, 0:cw], in_=pgt[:, 0:cw], func=ACTF.Silu), reads=[bpgt], writes=[b_sg[s_i]])
                    P.op("dve", lambda e, pup=pup, s_i=s_i, fc=fc, c0=c0, cw=cw: e.tensor_tensor(out=hidT[:, fc, c0:c0 + cw], in0=pup[:, 0:cw], in1=sg[s_i][:, 0:cw], op=ALU.mult),
                         reads=[bpup, b_sg[s_i]], writes=[b_hid])
        for cc in range(4):
            i = kw[0] % WST
            kw[0] += 1
            j = kw[1] % NWB
            kw[1] += 1
            wdv = wst[i][:].rearrange("p a b -> p (a b)")[:, 0:4096].rearrange("p (a b) -> p a b", a=8)
            wdb = wbf[j][:].rearrange("p a b -> p (a b)")[:, 0:4096].rearrange("p (a b) -> p a b", a=8)
            P.dma("sp", wdv, wd[e_].rearrange("(fc p) n -> p fc n", p=128)[:, :, cc * 512:(cc + 1) * 512], writes=[b_wst[i]])
            ce = cast_eng[kw[2] % 3]
            kw[2] += 1
            if ce == "act":
                P.op("act", lambda e, wdv=wdv, wdb=wdb: e.activation(out=wdb, in_=wdv, func=ACTF.Copy), reads=[b_wst[i]], writes=[b_wbf[j]])
            else:
                P.op(ce, lambda e, wdv=wdv, wdb=wdb: e.tensor_copy(out=wdb, in_=wdv), reads=[b_wst[i]], writes=[b_wbf[j]])
            for gi, (tiles, ns) in enumerate(groups):
                py, bpy = pb[6 + gi % 2], b_pb[6 + gi % 2]
                for fc in range(8):
                    P.op("pe", lambda e, py=py, gi=gi, ns=ns, fc=fc, wdb=wdb: e.matmul(py[0:ns, :], lhsT=hidT[:, fc, soff[gi]:soff[gi] + ns], rhs=wdb[:, fc, :],
                                                                                      start=(fc == 0), stop=(fc == 7)), reads=[b_hid, b_wbf[j]], writes=[bpy])
                yi = gi % 2
                P.op("act", lambda e, py=py, yi=yi, gi=gi, ns=ns: e.activation(out=yst[yi][0:ns, :], in_=py[0:ns, :], func=ACTF.Identity, bias=0.0, scale=wsum[0:ns, gi:gi + 1]),
                     reads=[bpy, b_wsl], writes=[b_yst[yi]])
                P.dma("sp", Yd[e_, soff[gi]:soff[gi] + ns, cc * 512:(cc + 1) * 512], yst[yi][0:ns, :], reads=[b_yst[yi]], writes=[b_Y[e_][gi]])

    P.barrier()
    A.reset()
    g2 = A.alloc([128, D], F32)
    b_g2 = P.buf()
    lng_s = A.alloc([128, D], F32)
    lnb_s = A.alloc([128, D], F32)
    b_ln = P.buf()
    P.dma("sp", lng_s[:], lng.partition_broadcast(128), writes=[b_ln])
    P.dma("sp", lnb_s[:], lnb.partition_broadcast(128), writes=[b_ln])
    Yq = A.alloc([128, NE, D], BF16)
    b_Yq = P.buf()
    sel3 = A.alloc([128, 16, 128], BF16)
    b_sel3 = P.buf()
    selT = A.alloc([128, 16, 128], BF16)
    b_selT = P.buf()
    xt = A.alloc([128, D], F32)
    zt = A.alloc([128, D], F32)
    xo = A.alloc([128, D], F32)
    b_xt, b_zt, b_xo = P.buf(), P.buf(), P.buf()
    ln = LN(P, "ln")
    pbf = [pb[4][:].bitcast(BF16), pb[5][:].bitcast(BF16)]
    outs = []
    for gi, (tiles, ns) in enumerate(groups):
        m = modc if is_ctx_group[gi] else modl
        if gi == 0 or (gi == 1 and is_ctx_group[0]):
            P.dma("sp", g2[:], m[0:1, 5 * D:6 * D].partition_broadcast(128), writes=[b_g2])
        P.dma("sp", Yq[0:ns], Yd[:, soff[gi]:soff[gi] + ns, :].rearrange("e s n -> s e n"), reads=[b_Y[e_][gi] for e_ in range(NE)], writes=[b_Yq])
        for t in tiles:
            rows = slice(t * 128, (t + 1) * 128)
            P.dma("sp", xt[:], xmid[rows, :], writes=[b_xt])
            posm_b = posm[:, t, :].unsqueeze(2).to_broadcast([128, 16, 128])
            P.op("dve", lambda e, posm_b=posm_b: e.tensor_tensor(out=sel3[:], in0=iota3[:].unsqueeze(1).to_broadcast([128, 16, 128]), in1=posm_b, op=ALU.is_equal), reads=[b_posm, b_iota], writes=[b_sel3])
            for half in range(2):
                for k in range(8):
                    e_ = half * 8 + k
                    P.op("pe", lambda e, half=half, k=k, e_=e_: e.transpose(out=pbf[half][:, k * 128:(k + 1) * 128], in_=sel3[:, e_, :], identity=ident[:]),
                         reads=[b_sel3, b_ident], writes=[b_pb[4 + half]])
                P.op("act" if half else "dve",
                     (lambda e, half=half: e.activation(out=selT[:, half * 8:(half + 1) * 8, :], in_=pbf[half][:].rearrange("p (k n) -> p k n", k=8), func=ACTF.Copy)) if half else
                     (lambda e, half=half: e.tensor_copy(out=selT[:, half * 8:(half + 1) * 8, :], in_=pbf[half][:].rearrange("p (k n) -> p k n", k=8))),
                     reads=[b_pb[4 + half]], writes=[b_selT])
            for cc in range(4):
                for e_ in range(NE):
                    P.op("pe", lambda e, cc=cc, e_=e_, ns=ns: e.matmul(pb[cc][:], lhsT=selT[0:ns, e_, :], rhs=Yq[0:ns, e_, cc * 512:(cc + 1) * 512], start=(e_ == 0), stop=(e_ == NE - 1)),
                         reads=[b_selT, b_Yq], writes=[b_pb[cc]])
                P.op("dve", lambda e, cc=cc: e.tensor_tensor(out=zt[:, cc * 512:(cc + 1) * 512], in0=pb[cc][:], in1=g2[:, cc * 512:(cc + 1) * 512], op=ALU.mult),
                     reads=[b_pb[cc], b_g2], writes=[b_zt])
            P.op("dve", lambda e: e.scalar_tensor_tensor(out=zt[:], in0=xt[:], scalar=ALPHA, in1=zt[:], op0=ALU.mult, op1=ALU.add), reads=[b_xt, b_zt], writes=[b_zt])
            ln.norm(xo[:], zt[:], b_zt, b_xo)
            P.op("dve", lambda e: e.tensor_tensor(out=xo[:], in0=xo[:], in1=lng_s[:], op=ALU.mult), reads=[b_xo, b_ln], writes=[b_xo])
            P.op("pool", lambda e: e.tensor_tensor(out=xo[:], in0=xo[:], in1=lnb_s[:], op=ALU.add), reads=[b_xo, b_ln], writes=[b_xo])
            outs.append(P.dma("sp", xout[rows, :], xo[:], reads=[b_xo]))
    st = P.emit(final_waits=outs)
    print("LD stats", st, flush=True)
    return nc, P


NCTX = 256


def le_consts():
    rot = np.zeros((128, 128), np.float32)
    for m in range(32):
        rot[m + 32, m] = -1.0
    for m in range(32, 64):
        rot[m - 32, m] = 1.0
    return {"rot64": rot, "ones": np.ones((128, 128), np.float32), "ones_bf": np.ones((128, 128), np.float32).astype(BF)}


def build_le(nblk=32, debug=False):
    nc = new_nc()
    NLAT = nblk * 512
    NTOK = NCTX + NLAT
    NT128 = NTOK // 128
    hT = din(nc, "hT", [D, NTOK], BF16)
    wdn = din(nc, "wdn", [D, 1088])
    gqk = din(nc, "gqk", [128, 8])
    wuq = din(nc, "wuq", [2, 512, 192])
    wukv = din(nc, "wukv", [2, 512, 256])
    cosT = din(nc, "cos64", [64, NLAT])
    sinT = din(nc, "sin64", [64, NLAT])
    c_rot = din(nc, "rot64", [128, 128])
    c_ones = din(nc, "ones", [128, 128])
    c_ones_bf = din(nc, "ones_bf", [128, 128], BF16)
    oT = dout(nc, "oT", [256, NLAT], BF16)
    dscr_ = dout if debug else dscr
    s_cq = dscr_(nc, "s_cq", [4, 128, NTOK], BF16)
    s_ckv = dscr_(nc, "s_ckv", [4, 128, NTOK], BF16)
    s_qn = dscr_(nc, "s_qn", [2, 128, NTOK], BF16)
    s_qr = dscr_(nc, "s_qr", [2, 64, NTOK], BF16)
    P = Prog(nc)
    blocks = [(0, NCTX)] + [(NCTX + 512 * i, 512) for i in range(nblk)]
    NBLK = len(blocks)
    db = {n: P.bufs(NBLK, n) for n in ("cq", "ckv", "qn0", "qn1", "qr0", "qr1")}

    def cload(name, src, shape, dt):
        t = P.sb(name, shape, dt)
        b = P.buf(name)
        P.dma("sp", t[:], src, writes=[b])
        return t, b

    rot, b_rot = cload("rot_s", c_rot, [128, 128], F32)
    ones, b_ones = cload("ones_s", c_ones, [128, 128], F32)
    ones_bf, b_onesbf = cload("onesbf_s", c_ones_bf, [128, 128], BF16)
    gq, b_gq = cload("gq_s", gqk, [128, 8], F32)
    KR = P.sb("KR", [64, NTOK], BF16)
    b_KR = P.bufs(NBLK, "KR")
    b_KN = P.bufs(NBLK, "KN")
    b_V = P.bufs(NBLK, "V")
    pb = [P.ps(f"pb{i}", [128, 512], F32) for i in range(8)]
    b_pb = P.bufs(8, "pb", excl=True)
    A = Arena(P, "arena", 31 * 1024)

    wdn_bf = A.alloc([128, 16, 1088], BF16)
    b_w = P.buf("w")
    wst = [A.alloc([128, 16, 136], F32) for _ in range(2)]
    b_wst = P.bufs(2)
    for c in range(8):
        i = c % 2
        P.dma("sp", wst[i][:], wdn.rearrange("(kc p) n -> p kc n", p=128)[:, :, c * 136:(c + 1) * 136], writes=[b_wst[i]])
        if c % 2:
            P.op("act", lambda e, i=i, c=c: e.activation(out=wdn_bf[:, :, c * 136:(c + 1) * 136], in_=wst[i][:], func=ACTF.Copy), reads=[b_wst[i]], writes=[b_w])
        else:
            P.op("dve", lambda e, i=i, c=c: e.tensor_copy(out=wdn_bf[:, :, c * 136:(c + 1) * 136], in_=wst[i][:]), reads=[b_wst[i]], writes=[b_w])
    hb = [A.alloc([128, 16, 512], BF16) for _ in range(2)]
    b_hb = P.bufs(2, "hb")
    cf = A.alloc([128, 4, 512], F32)
    b_cf = P.buf()
    sq = [A.alloc([128, 512], F32) for _ in range(2)]
    b_sq = P.bufs(2)
    rs = A.alloc([128, 512], F32)
    b_rs = P.buf()
    cn = [A.alloc([128, 4, 512], BF16) for _ in range(2)]
    b_cn = P.bufs(2)
    cs = [A.alloc([64, 2, 512], F32) for _ in range(2)]
    b_cs = P.bufs(2)
    t64 = [A.alloc([128, 512], F32) for _ in range(3)]
    b_t64 = P.bufs(3)
    P.op("dve", lambda e, t=t64[0]: e.memset(t[:], 0.0), writes=[b_t64[0]])
    kk = [0, 0]

    def rope64(src_ps, bsrc, dest, bdest, w, cs_t, bcs, tt, btt, lat):
        if not lat:
            P.op("act", lambda e: e.activation(out=dest, in_=src_ps, func=ACTF.Copy), reads=[bsrc], writes=[bdest])
            return
        a, ba = tt[0], btt[0]
        b_, bb = tt[1], btt[1]
        c_, bc = tt[2], btt[2]
        P.op("act", lambda e: e.activation(out=a[0:64, 0:w], in_=src_ps, func=ACTF.Copy), reads=[bsrc], writes=[ba])
        P.op("pe", lambda e: e.matmul(pb[7][:, 0:w], lhsT=rot[:], rhs=a[:, 0:w], start=True, stop=True), reads=[b_rot, ba], writes=[b_pb[7]])
        P.op("dve", lambda e: e.tensor_tensor(out=b_[0:64, 0:w], in0=pb[7][0:64, 0:w], in1=cs_t[:, 1, 0:w], op=ALU.mult), reads=[b_pb[7], bcs], writes=[bb])
        P.op("pool", lambda e: e.tensor_tensor(out=c_[0:64, 0:w], in0=a[0:64, 0:w], in1=cs_t[:, 0, 0:w], op=ALU.mult), reads=[ba, bcs], writes=[bc])
        P.op("dve", lambda e: e.tensor_tensor(out=dest, in0=b_[0:64, 0:w], in1=c_[0:64, 0:w], op=ALU.add), reads=[bb, bc], writes=[bdest])

    for bi, (s0, w) in enumerate(blocks):
        lat = bi > 0
        i = bi % 2
        P.dma("sp", hb[i][:, :, 0:w], hT.rearrange("(kc p) n -> p kc n", p=128)[:, :, s0:s0 + w], writes=[b_hb[i]])
        if lat:
            l0 = s0 - NCTX
            P.dma("sp", cs[i][:, 0, 0:w], cosT[:, l0:l0 + w], writes=[b_cs[i]])
            P.dma("sp", cs[i][:, 1, 0:w], sinT[:, l0:l0 + w], writes=[b_cs[i]])
        for which in ((0, 1) if lat else (1,)):
            for c in range(4):
                col = which * 512 + c * 128
                pf, bpf = pb[c % 3], b_pb[c % 3]
                for kc in range(16):
                    P.op("pe", lambda e, pf=pf, kc=kc, col=col, i=i, w=w: e.matmul(pf[:, 0:w], lhsT=wdn_bf[:, kc, col:col + 128], rhs=hb[i][:, kc, 0:w],
                                                                                start=(kc == 0), stop=(kc == 15)), reads=[b_w, b_hb[i]], writes=[bpf])
                P.op("act", lambda e, pf=pf, c=c, w=w: e.activation(out=cf[:, c, 0:w], in_=pf[:, 0:w], func=ACTF.Copy), reads=[bpf], writes=[b_cf])
                si = kk[0] % 2
                kk[0] += 1
                P.op("act", lambda e, pf=pf, si=si, w=w: e.activation(out=sq[si][:, 0:w], in_=pf[:, 0:w], func=ACTF.Square), reads=[bpf], writes=[b_sq[si]])
                P.op("pe", lambda e, si=si, c=c, w=w: e.matmul(pb[3][:, 0:w], lhsT=ones[:], rhs=sq[si][:, 0:w], start=(c == 0), stop=(c == 3)),
                     reads=[b_ones, b_sq[si]], writes=[b_pb[3]])
            P.op("act", lambda e, w=w: e.activation(out=rs[:, 0:w], in_=pb[3][:, 0:w], func=ACTF.Sqrt, bias=EPS, scale=1.0 / 512), reads=[b_pb[3]], writes=[b_rs])
            P.op("dve", lambda e, w=w: e.reciprocal(out=rs[:, 0:w], in_=rs[:, 0:w]), reads=[b_rs], writes=[b_rs])
            ci = kk[1] % 2
            kk[1] += 1
            for c in range(4):
                P.op("dve", lambda e, c=c, w=w, ci=ci, which=which: e.scalar_tensor_tensor(out=cn[ci][:, c, 0:w], in0=cf[:, c, 0:w], scalar=gq[:, which * 4 + c:which * 4 + c + 1],
                                                                                          in1=rs[:, 0:w], op0=ALU.mult, op1=ALU.mult), reads=[b_cf, b_rs, b_gq], writes=[b_cn[ci]])
            dst, dbuf = (s_cq, db["cq"]) if which == 0 else (s_ckv, db["ckv"])
            P.dma("sp", dst[:, :, s0:s0 + w].rearrange("c p n -> p c n"), cn[ci][:, :, 0:w], reads=[b_cn[ci]], writes=[dbuf[bi]])
        for kc in range(16):
            P.op("pe", lambda e, kc=kc, i=i, w=w: e.matmul(pb[4][0:64, 0:w], lhsT=wdn_bf[:, kc, 1024:1088], rhs=hb[i][:, kc, 0:w], start=(kc == 0), stop=(kc == 15)),
                 reads=[b_w, b_hb[i]], writes=[b_pb[4]])
        rope64(pb[4][0:64, 0:w], b_pb[4], KR[:, s0:s0 + w], b_KR[bi], w, cs[i], b_cs[i], t64, b_t64, lat)

    SCALE = 192 ** -0.5
    out_dmas = []
    P.barrier()
    A.reset()
    KN = A.alloc([128, NTOK], BF16)
    Vres = A.alloc([128, NT128, 128], BF16)
    A.mark()
    for hh in range(2):
        P.barrier()
        A.reset()
        wuq_bf = A.alloc([128, 4, 192], BF16)
        wukv_bf = A.alloc([128, 4, 256], BF16)
        wst2 = A.alloc([128, 4, 256], F32)
        b_w2, b_wst2 = P.buf(), P.buf()
        P.dma("sp", wst2[:, :, 0:192], wuq[hh].rearrange("(c p) n -> p c n", p=128), writes=[b_wst2])
        P.op("dve", lambda e: e.tensor_copy(out=wuq_bf[:], in_=wst2[:, :, 0:192]), reads=[b_wst2], writes=[b_w2])
        P.dma("sp", wst2[:], wukv[hh].rearrange("(c p) n -> p c n", p=128), writes=[b_wst2])
        P.op("dve", lambda e: e.tensor_copy(out=wukv_bf[:], in_=wst2[:]), reads=[b_wst2], writes=[b_w2])
        cqb = [A.alloc([128, 4, 512], BF16) for _ in range(2)]
        ckb = [A.alloc([128, 4, 512], BF16) for _ in range(2)]
        b_cqb, b_ckb = P.bufs(2), P.bufs(2)
        cs2 = [A.alloc([64, 2, 512], F32) for _ in range(2)]
        b_cs2 = P.bufs(2)
        qno = [A.alloc([128, 512], BF16) for _ in range(2)]
        qro = [A.alloc([64, 512], BF16) for _ in range(2)]
        b_qno, b_qro = P.bufs(2), P.bufs(2)
        t64b = [A.alloc([128, 512], F32) for _ in range(3)]
        b_t64b = P.bufs(3)
        P.op("dve", lambda e, t=t64b[0]: e.memset(t[:], 0.0), writes=[b_t64b[0]])
        for bi, (s0, w) in enumerate(blocks):
            lat = bi > 0
            i = bi % 2
            P.dma("sp", ckb[i][:, :, 0:w], s_ckv[:, :, s0:s0 + w].rearrange("c p n -> p c n"), reads=[db["ckv"][bi]], writes=[b_ckb[i]])
            if lat:
                l0 = s0 - NCTX
                P.dma("sp", cqb[i][:, :, 0:w], s_cq[:, :, s0:s0 + w].rearrange("c p n -> p c n"), reads=[db["cq"][bi]], writes=[b_cqb[i]])
                P.dma("sp", cs2[i][:, 0, 0:w], cosT[:, l0:l0 + w], writes=[b_cs2[i]])
                P.dma("sp", cs2[i][:, 1, 0:w], sinT[:, l0:l0 + w], writes=[b_cs2[i]])
                for c in range(4):
                    P.op("pe", lambda e, c=c, i=i, w=w, wuq_bf=wuq_bf, cqb=cqb: e.matmul(pb[0][:, 0:w], lhsT=wuq_bf[:, c, 0:128], rhs=cqb[i][:, c, 0:w], start=(c == 0), stop=(c == 3)),
                         reads=[b_w2, b_cqb[i]], writes=[b_pb[0]])
                P.op("act", lambda e, i=i, w=w: e.activation(out=qno[i][:, 0:w], in_=pb[0][:, 0:w], func=ACTF.Copy), reads=[b_pb[0]], writes=[b_qno[i]])
                P.dma("sp", s_qn[hh, :, s0:s0 + w], qno[i][:, 0:w], reads=[b_qno[i]], writes=[db[f"qn{hh}"][bi]])
                for c in range(4):
                    P.op("pe", lambda e, c=c, i=i, w=w: e.matmul(pb[1][0:64, 0:w], lhsT=wuq_bf[:, c, 128:192], rhs=cqb[i][:, c, 0:w], start=(c == 0), stop=(c == 3)),
                         reads=[b_w2, b_cqb[i]], writes=[b_pb[1]])
                rope64(pb[1][0:64, 0:w], b_pb[1], qro[i][:, 0:w], b_qro[i], w, cs2[i], b_cs2[i], t64b, b_t64b, True)
                P.dma("sp", s_qr[hh, :, s0:s0 + w], qro[i][:, 0:w], reads=[b_qro[i]], writes=[db[f"qr{hh}"][bi]])
            for c in range(4):
                P.op("pe", lambda e, c=c, i=i, w=w: e.matmul(pb[2][:, 0:w], lhsT=wukv_bf[:, c, 0:128], rhs=ckb[i][:, c, 0:w], start=(c == 0), stop=(c == 3)),
                     reads=[b_w2, b_ckb[i]], writes=[b_pb[2]])
            P.op("act", lambda e, s0=s0, w=w: e.activation(out=KN[:, s0:s0 + w], in_=pb[2][:, 0:w], func=ACTF.Copy), reads=[b_pb[2]], writes=[b_KN[bi]])
            for tt in range(w // 128):
                pt, bpt = pb[3 + tt % 2], b_pb[3 + tt % 2]
                for c in range(4):
                    P.op("pe", lambda e, pt=pt, c=c, i=i, tt=tt: e.matmul(pt[:, 0:128], lhsT=ckb[i][:, c, tt * 128:(tt + 1) * 128], rhs=wukv_bf[:, c, 128:256], start=(c == 0), stop=(c == 3)),
                         reads=[b_w2, b_ckb[i]], writes=[bpt])
                tok = s0 + tt * 128
                P.op("dve", lambda e, pt=pt, tok=tok: e.tensor_copy(out=Vres[:, tok // 128, :], in_=pt[:, 0:128]), reads=[bpt], writes=[b_V[bi]])
        P.barrier()
        A.reset()
        qn_s = [A.alloc([128, 512], BF16) for _ in range(2)]
        qr_s = [A.alloc([64, 512], BF16) for _ in range(2)]
        b_qns, b_qrs = P.bufs(2), P.bufs(2)
        NPT = 4
        pT_sb = [A.alloc([128, 512], BF16) for _ in range(NPT)]
        b_pTs = P.bufs(NPT)
        rz = [A.alloc([128, 512], F32) for _ in range(2)]
        b_rz = P.bufs(2)
        ob = [A.alloc([128, 512], BF16) for _ in range(2)]
        b_ob = P.bufs(2)
        kcount = 0
        for bi, (s0, w) in enumerate(blocks):
            if bi == 0:
                continue
            i = bi % 2
            P.dma("sp", qn_s[i][:, 0:w], s_qn[hh, :, s0:s0 + w], reads=[db[f"qn{hh}"][bi]], writes=[b_qns[i]])
            P.dma("sp", qr_s[i][:, 0:w], s_qr[hh, :, s0:s0 + w], reads=[db[f"qr{hh}"][bi]], writes=[b_qrs[i]])
            pO, bpO = pb[3 + i], b_pb[3 + i]
            pZ, bpZ = pb[5 + i], b_pb[5 + i]
            nkc = NT128
            for kc in range(nkc):
                kblk = 0 if kc < 2 else 1 + (kc * 128 - NCTX) // 512
                si = kcount % 3
                ti = kcount % NPT
                kcount += 1
                pS, bpS = pb[si], b_pb[si]
                P.op("pe", lambda e, pS=pS, kc=kc, i=i, w=w: e.matmul(pS[:, 0:w], lhsT=KN[:, kc * 128:(kc + 1) * 128], rhs=qn_s[i][:, 0:w], start=True, stop=False),
                     reads=[b_KN[kblk], b_qns[i]], writes=[bpS])
                P.op("pe", lambda e, pS=pS, kc=kc, i=i, w=w: e.matmul(pS[:, 0:w], lhsT=KR[:, kc * 128:(kc + 1) * 128], rhs=qr_s[i][:, 0:w], start=False, stop=True),
                     reads=[b_KR[kblk], b_qrs[i]], writes=[bpS])
                P.op("act", lambda e, pS=pS, ti=ti, w=w: e.activation(out=pT_sb[ti][:, 0:w], in_=pS[:, 0:w], func=ACTF.Exp, scale=SCALE), reads=[bpS], writes=[b_pTs[ti]])
                P.op("pe", lambda e, pO=pO, kc=kc, ti=ti, w=w, nkc=nkc: e.matmul(pO[:, 0:w], lhsT=Vres[:, kc, :], rhs=pT_sb[ti][:, 0:w], start=(kc == 0), stop=(kc == nkc - 1)),
                     reads=[b_V[kblk], b_pTs[ti]], writes=[bpO])
                P.op("pe", lambda e, pZ=pZ, kc=kc, ti=ti, w=w, nkc=nkc: e.matmul(pZ[:, 0:w], lhsT=ones_bf[:], rhs=pT_sb[ti][:, 0:w], start=(kc == 0), stop=(kc == nkc - 1)),
                     reads=[b_onesbf, b_pTs[ti]], writes=[bpZ])
            P.op("dve", lambda e, i=i, pZ=pZ, w=w: e.reciprocal(out=rz[i][:, 0:w], in_=pZ[:, 0:w]), reads=[bpZ], writes=[b_rz[i]])
            P.op("dve", lambda e, i=i, pO=pO, w=w: e.tensor_tensor(out=ob[i][:, 0:w], in0=pO[:, 0:w], in1=rz[i][:, 0:w], op=ALU.mult), reads=[bpO, b_rz[i]], writes=[b_ob[i]])
            l0 = s0 - NCTX
            out_dmas.append(P.dma("sp", oT[hh * 128:(hh + 1) * 128, l0:l0 + w], ob[i][:, 0:w], reads=[b_ob[i]]))
    st = P.emit(final_waits=out_dmas)
    print("LE stats", st, flush=True)
    return nc, P


def le_inputs(inp, hT_all, nblk=32):
    NLAT = nblk * 512
    consts = le_consts()
    n_tok = 16384
    rows = n_tok // 64
    row = np.repeat(np.arange(rows, dtype=np.float32), 64)
    col = np.tile(np.arange(64, dtype=np.float32), rows)
    n_freq = 16
    inv = (10000.0 ** (-np.arange(n_freq, dtype=np.float32) / n_freq)).astype(np.float32)
    ang = np.concatenate([row[:, None] * inv, col[:, None] * inv], -1)
    cos = np.cos(ang).astype(np.float32)
    sin = np.sin(ang).astype(np.float32)
    cos64 = np.ascontiguousarray(np.concatenate([cos, cos], -1).T[:, :NLAT])
    sin64 = np.ascontiguousarray(np.concatenate([sin, sin], -1).T[:, :NLAT])
    hTs = np.ascontiguousarray(hT_all[:, :NCTX + NLAT])
    wdn = np.asarray(inp["mla_w_down"][0])
    gqk = np.ascontiguousarray(np.concatenate([np.asarray(inp["mla_q_norm_g"][0]).reshape(4, 128).T, np.asarray(inp["mla_kv_norm_g"][0]).reshape(4, 128).T], 1))
    wuq_all = np.asarray(inp["mla_w_uq"][0]).reshape(512, 16, 192)
    wukv_all = np.asarray(inp["mla_w_ukv"][0]).reshape(512, 16, 256)
    maps = []
    for j in range(NCORE):
        m = {"hT": hTs, "wdn": wdn, "gqk": gqk,
             "wuq": np.ascontiguousarray(wuq_all[:, 2 * j:2 * j + 2].transpose(1, 0, 2)),
             "wukv": np.ascontiguousarray(wukv_all[:, 2 * j:2 * j + 2].transpose(1, 0, 2)),
             "cos64": cos64, "sin64": sin64}
        m.update(consts)
        maps.append(m)
    return maps

import time

NLAT_CORE = 2048
NCTX = 256
VERBOSE = True


def _run(nc, P, maps, tag):
    t0 = time.time()
    res = run_bass_kernel_spmd(nc, maps, core_ids=list(range(NCORE)))
    if VERBOSE:
        print(f"[kernel] {tag}: {time.time() - t0:.1f}s", flush=True)
    return res.results


def _f32(a):
    return np.ascontiguousarray(np.asarray(a, dtype=np.float32))


def kernel(x, c, ctx, c_ctx, ada_w, ada_b, ln_g, ln_b, ev_w_in, ev_w_out, hgrn_lb, hgrn_norm_g,
           gqa_q_norm_g, gqa_k_norm_g, mla_w_down, mla_q_norm_g, mla_kv_norm_g, mla_w_uq, mla_w_ukv,
           mla_w_o, moe_router, moe_w_gate, moe_w_up, moe_w_down):
    inp = dict(x=x, c=c, ctx=ctx, c_ctx=c_ctx, ada_w=ada_w, ada_b=ada_b, ln_g=ln_g, ln_b=ln_b, ev_w_in=ev_w_in,
               ev_w_out=ev_w_out, hgrn_lb=hgrn_lb, hgrn_norm_g=hgrn_norm_g, gqa_q_norm_g=gqa_q_norm_g,
               gqa_k_norm_g=gqa_k_norm_g, mla_w_down=mla_w_down, mla_q_norm_g=mla_q_norm_g, mla_kv_norm_g=mla_kv_norm_g,
               mla_w_uq=mla_w_uq, mla_w_ukv=mla_w_ukv, mla_w_o=mla_w_o, moe_router=moe_router, moe_w_gate=moe_w_gate,
               moe_w_up=moe_w_up, moe_w_down=moe_w_down)
    inp = {k: _f32(v) for k, v in inp.items()}
    xs = inp["x"][0]
    ctx0 = inp["ctx"][0]
    ident = np.eye(128, dtype=np.float32)
    cD = ld_consts()

    nc, P = build_l0()
    modv = l0_gather(_run(nc, P, l0_inputs(inp), "L0"))

    def modmaps(l):
        return {"modl": np.ascontiguousarray(modv[l, 0][None]), "modc": np.ascontiguousarray(modv[l, 1][None])}

    def run_la(l, x_lat, x_ctx):
        nc_la, P_la = build_la()
        maps = []
        for j in range(NCORE):
            m = {"xin": np.concatenate([x_ctx, x_lat[j * NLAT_CORE:(j + 1) * NLAT_CORE]], 0), "ident": ident}
            m.update(modmaps(l))
            maps.append(m)
        res = _run(nc_la, P_la, maps, f"LA{l}")
        return np.concatenate([np.asarray(res[0]["hT"])[:, :NCTX]] + [np.asarray(r["hT"])[:, NCTX:] for r in res], axis=1)

    def run_ld(l, res_c, groups, has_ctx):
        nc, P = build_ld(groups, 2048, 2048, has_ctx)
        o = NCTX if has_ctx else 0
        affT_lat = np.ascontiguousarray(np.concatenate([np.asarray(r["aff"])[o:] for r in res_c], 0).T)
        maps = []
        for j in range(NCORE):
            r = res_c[j]
            m = {"affT_lat": affT_lat, "aff_own": np.asarray(r["aff"]), "h2": np.asarray(r["h2"]), "xmid": np.asarray(r["xmid"]),
                 "wg": inp["moe_w_gate"][l], "wu": inp["moe_w_up"][l], "wd": inp["moe_w_down"][l],
                 "lng": inp["ln_g"][l, 1][None], "lnb": inp["ln_b"][l, 1][None]}
            if has_ctx:
                m["affT_ctx"] = np.ascontiguousarray(np.asarray(r["aff"])[:NCTX].T)
            m.update(modmaps(l))
            m.update(cD)
            maps.append(m)
        return _run(nc, P, maps, f"LD{l}")

    hT_all = run_la(0, xs, ctx0)
    nc, P = build_lb()
    res_b = _run(nc, P, lb_inputs(inp, hT_all), "LB")
    oT_full = np.concatenate([np.asarray(r["aT"]) for r in res_b] + [np.asarray(r["bT"]) for r in res_b], axis=0)
    nc, P = build_lc(18, 2)
    maps = []
    for j in range(NCORE):
        lo = NCTX + j * NLAT_CORE
        m = {"oT": np.ascontiguousarray(np.concatenate([oT_full[:, :NCTX], oT_full[:, lo:lo + NLAT_CORE]], 1)), "wo": inp["ev_w_out"][0],
             "xin": np.concatenate([ctx0, xs[j * NLAT_CORE:(j + 1) * NLAT_CORE]], 0),
             "lng": inp["ln_g"][0, 0][None], "lnb": inp["ln_b"][0, 0][None], "wr": inp["moe_router"][0], "ident": ident}
        m.update(modmaps(0))
        maps.append(m)
    res_c = _run(nc, P, maps, "LC0")
    groups0 = [([0, 1], 32)] + [([2 + 4 * q + k for k in range(4)], 128) for q in range(4)]
    res_d = run_ld(0, res_c, groups0, True)
    x1 = np.concatenate([np.asarray(r["xout"])[NCTX:] for r in res_d], 0)
    ctx1 = np.asarray(res_d[0]["xout"])[:NCTX]

    hT_all1 = run_la(1, x1, ctx1)
    nc, P = build_le()
    res_e = _run(nc, P, le_inputs(inp, hT_all1), "LE")
    oT1 = np.concatenate([np.asarray(r["oT"]) for r in res_e], axis=0)
    nc, P = build_lc(16, 0)
    maps = []
    for j in range(NCORE):
        m = {"oT": np.ascontiguousarray(oT1[:, j * NLAT_CORE:(j + 1) * NLAT_CORE]), "wo": inp["mla_w_o"][0],
             "xin": np.ascontiguousarray(x1[j * NLAT_CORE:(j + 1) * NLAT_CORE]),
             "lng": inp["ln_g"][1, 0][None], "lnb": inp["ln_b"][1, 0][None], "wr": inp["moe_router"][1], "ident": ident}
        m.update(modmaps(1))
        maps.append(m)
    res_c1 = _run(nc, P, maps, "LC1")
    groups1 = [([4 * q + k for k in range(4)], 128) for q in range(4)]
    res_d1 = run_ld(1, res_c1, groups1, False)
    out = np.concatenate([np.asarray(r["xout"]) for r in res_d1], 0)
    return out[None].astype(np.float32)
```

```python
import contextlib
import numpy as np
import concourse.bass as bass
import concourse.mybir as mybir

F32 = mybir.dt.float32
BF16 = mybir.dt.bfloat16
I32 = mybir.dt.int32
ALU = mybir.AluOpType
ACTF = mybir.ActivationFunctionType
AX = mybir.AxisListType

COMPUTE = ("pe", "act", "dve", "pool")
EPOCH = 4000
NSEM_ENG = 14
NSEM_DMA = 20


class Buf:
    __slots__ = ("name", "w", "rs", "excl")

    def __init__(self, name="", excl=False):
        self.name = name
        self.w = None
        self.rs = []
        self.excl = excl


class Op:
    __slots__ = ("eng", "idx", "fn", "waits", "is_dma", "dma_id", "sig", "clock", "q")

    def __init__(self, eng, idx, fn, is_dma=False):
        self.eng = eng
        self.idx = idx
        self.fn = fn
        self.waits = []
        self.is_dma = is_dma
        self.dma_id = None
        self.sig = None
        self.clock = None
        self.q = None


class Prog:
    def __init__(self, nc, same_engine_sync=True):
        self.nc = nc
        self.es = contextlib.ExitStack()
        self.streams = {e: [] for e in ("pe", "act", "dve", "pool", "sp")}
        self.known = {e: {c: -1 for c in COMPUTE} for e in self.streams}
        self.known_dma = {e: set() for e in self.streams}
        self.dma_count = {"sp": 0, "pool": 0, "act": 0}
        self.dma_ops = {"sp": [], "pool": [], "act": []}
        self.same_engine_sync = same_engine_sync
        self.nbuf = 0
        self.pending = {}

    def barrier(self):
        deps = []
        for f in COMPUTE:
            for o in reversed(self.streams[f]):
                if not o.is_dma:
                    deps.append(o)
                    break
        for q in self.dma_ops:
            deps.extend(self.dma_ops[q][-NSEM_DMA:])
        for e in self.streams:
            self.pending[e] = list(deps)

    def _pend(self, o):
        for d in self.pending.pop(o.eng, []):
            self._need(o, d)

    def sb(self, name, shape, dtype):
        t = self.es.enter_context(self.nc.sbuf_tensor(name, list(shape), dtype))
        return t

    def ps(self, name, shape, dtype=F32):
        t = self.es.enter_context(self.nc.psum_tensor(name, list(shape), dtype))
        return t

    def buf(self, name="", excl=False):
        self.nbuf += 1
        return Buf(name or f"b{self.nbuf}", excl)

    def bufs(self, n, name="", excl=False):
        return [self.buf(f"{name}{i}", excl) for i in range(n)]

    def _need(self, op, dep):
        if dep is None or dep is op:
            return
        e = op.eng
        if dep.is_dma:
            if dep.dma_id in self.known_dma[e]:
                return
            self.known_dma[e].add(dep.dma_id)
            op.waits.append(dep)
            for c, v in dep.clock.items():
                if v > self.known[e][c]:
                    self.known[e][c] = v
            return
        f = dep.eng
        if f == e and (f == "pe" or not self.same_engine_sync):
            return
        if self.known[e][f] >= dep.idx:
            return
        op.waits.append(dep)
        for c, v in dep.clock.items():
            if v > self.known[e][c]:
                self.known[e][c] = v
        if dep.idx > self.known[e][f]:
            self.known[e][f] = dep.idx

    def _track(self, op, reads, writes):
        ex = [r for r in reads if r.excl]
        if ex:
            reads = [r for r in reads if not r.excl]
            writes = list(writes) + [r for r in ex if r not in writes]
        for r in reads:
            self._need(op, r.w)
        for w in writes:
            self._need(op, w.w)
            for rd in w.rs:
                self._need(op, rd)
        for w in writes:
            w.w = op
            w.rs = []
        for r in reads:
            if r.w is not op:
                r.rs.append(op)

    def op(self, eng, fn, reads=(), writes=()):
        st = self.streams[eng]
        o = Op(eng, len(st), fn)
        self._pend(o)
        self._track(o, reads, writes)
        o.clock = dict(self.known[eng])
        if eng in COMPUTE:
            o.clock[eng] = o.idx
        st.append(o)
        return o

    def dma(self, q, out, in_, reads=(), writes=(), **kw):
        st = self.streams[q]
        o = Op(q, len(st), lambda e: e.dma_start(out=out, in_=in_, **kw), is_dma=True)
        o.q = q
        n = self.dma_count[q]
        o.dma_id = (q, n)
        self.dma_count[q] = n + 1
        if n >= NSEM_DMA:
            self._need(o, self.dma_ops[q][n - NSEM_DMA])
        self.dma_ops[q].append(o)
        self._pend(o)
        self._track(o, reads, writes)
        o.clock = dict(self.known[q])
        st.append(o)
        return o

    def emit(self, final_waits=()):
        nc = self.nc
        es = self.es
        needed = set()
        for e, st in self.streams.items():
            for o in st:
                for d in o.waits:
                    if not d.is_dma:
                        needed.add((d.eng, d.idx))
        nsig = {}
        for e in COMPUTE:
            k = 0
            for o in self.streams[e]:
                if (e, o.idx) in needed:
                    o.sig = k
                    k += 1
            nsig[e] = k
        sems = {}
        for e in COMPUTE:
            ne = max(1, (nsig[e] + EPOCH - 1) // EPOCH)
            assert ne <= NSEM_ENG, (e, nsig[e])
            sems[e] = [es.enter_context(nc.semaphore(f"s_{e}{i}")) for i in range(ne)]
        dsems = {}
        for q in ("sp", "pool", "act"):
            if self.dma_count[q]:
                dsems[q] = [es.enter_context(nc.semaphore(f"d_{q}{i}")) for i in range(NSEM_DMA)]
        engobj = {"pe": "tensor", "act": "scalar", "dve": "vector", "pool": "gpsimd", "sp": "sync"}

        def wait_for(eobj, d):
            if d.is_dma:
                q, n = d.dma_id
                eobj.wait_ge(dsems[q][n % NSEM_DMA], 16 * (n // NSEM_DMA + 1))
            else:
                eobj.wait_ge(sems[d.eng][d.sig // EPOCH], d.sig % EPOCH + 1)

        stats = {}
        with nc.Block() as block:
            for e in ("sp", "act", "pe", "dve", "pool"):
                st = self.streams[e]
                if not st and e != "sp":
                    continue

                def body(eobj, st=st, e=e):
                    nw = 0
                    for o in st:
                        for d in o.waits:
                            wait_for(eobj, d)
                            nw += 1
                        ins = o.fn(eobj)
                        if o.is_dma:
                            q, n = o.dma_id
                            ins.then_inc(dsems[q][n % NSEM_DMA], 16)
                        elif o.sig is not None:
                            ins.then_inc(sems[e][o.sig // EPOCH], 1)
                    if e == "sp":
                        for d in final_waits:
                            wait_for(eobj, d)
                    stats[e] = (len(st), nw)

                getattr(block, engobj[e])(body)
        self.stats = stats
        self.nsig = nsig
        print("signals", nsig, flush=True)
        return stats


class Arena:
    def __init__(self, P, name, nwords):
        self.t = P.sb(name, [128, nwords], F32)
        self.n = nwords
        self.off = 0
        self.floor = 0

    def mark(self):
        self.floor = self.off

    def reset(self):
        self.off = self.floor

    def alloc(self, shape, dtype, parts=128):
        per = 1
        for s_ in shape[1:]:
            per *= s_
        words = per if dtype in (F32, I32) else (per + 1) // 2
        words = (words + 7) // 8 * 8
        assert self.off + words <= self.n, ("arena overflow", self.off, words, self.n)
        v = self.t[0:shape[0], self.off:self.off + words]
        self.off += words
        if dtype not in (F32,):
            v = v.bitcast(dtype)
        v = v[:, 0:per]
        if len(shape) == 3:
            v = v.rearrange("p (a b) -> p a b", a=shape[1])
        elif len(shape) == 4:
            v = v.rearrange("p (a b c) -> p a b c", a=shape[1], b=shape[2])
        return v

import numpy as np
import ml_dtypes
from concourse.bass_utils import run_bass_kernel_spmd

D = 2048
EPS = 1e-6
NCORE = 8
ALPHA = (2.0 * 2) ** 0.25
BF = ml_dtypes.bfloat16


def new_nc():
    return bass.Bass("TRN2", target_bir_lowering=False)


def din(nc, name, shape, dt=F32):
    return nc.dram_tensor(name, list(shape), dt, kind="ExternalInput").ap()


def dout(nc, name, shape, dt=F32):
    return nc.dram_tensor(name, list(shape), dt, kind="ExternalOutput").ap()


def dscr(nc, name, shape, dt=F32):
    return nc.dram_tensor(name, list(shape), dt, kind="Internal").ap()


def run(nc, P, in_maps):
    with P.es:
        res = run_bass_kernel_spmd(nc, in_maps, core_ids=list(range(NCORE)))
    return res.results


class LN:
    def __init__(self, P, name, nb=2):
        self.P = P
        self.nb = nb
        self.st = [P.sb(f"{name}_st{i}", [128, 4, 6], F32) for i in range(nb)]
        self.mv = [P.sb(f"{name}_mv{i}", [128, 4], F32) for i in range(nb)]
        self.b = P.bufs(nb, f"{name}_st")
        self.k = 0

    def norm(self, out_ap, in_ap, b_in, b_out, rows=128):
        P = self.P
        i = self.k % self.nb
        self.k += 1
        st, mv, b = self.st[i], self.mv[i], self.b[i]
        for c in range(4):
            P.op("dve", lambda e, c=c: e.bn_stats(out=st[:rows, c, :], in_=in_ap[:, c * 512:(c + 1) * 512]),
                 reads=[b_in], writes=[b])
        P.op("dve", lambda e: e.bn_aggr(out=mv[:rows, 0:2], in_=st[:rows].rearrange("p a b -> p (a b)")), reads=[b], writes=[b])
        P.op("act", lambda e: e.activation(out=mv[:rows, 2:3], in_=mv[:rows, 1:2], func=ACTF.Sqrt, bias=EPS, scale=1.0),
             reads=[b], writes=[b])
        P.op("dve", lambda e: e.reciprocal(out=mv[:rows, 2:3], in_=mv[:rows, 2:3]), reads=[b], writes=[b])
        P.op("dve", lambda e: e.tensor_scalar(out=mv[:rows, 3:4], in0=mv[:rows, 0:1], scalar1=mv[:rows, 2:3], scalar2=-1.0,
                                              op0=ALU.mult, op1=ALU.mult), reads=[b], writes=[b])
        P.op("act", lambda e: e.activation(out=out_ap, in_=in_ap, func=ACTF.Identity, bias=mv[:rows, 3:4], scale=mv[:rows, 2:3]),
             reads=[b_in, b], writes=[b_out])


def load_bcast(P, name, src_row_ap, width, q="sp"):
    t = P.sb(name, [128, width], F32)
    b = P.buf(name)
    P.dma(q, t[:], src_row_ap.partition_broadcast(128), writes=[b])
    return t, b


def build_l0():
    nc = new_nc()
    CW = 12288 // NCORE
    cv = din(nc, "cv", [128, 16, 2])
    adaw = din(nc, "adaw", [2, D, CW])
    adab = din(nc, "adab", [2, CW])
    modv = dout(nc, "modv", [2, 2, CW])
    P = Prog(nc)
    cvs = P.sb("cvs", [128, 16, 2], F32)
    b_cv = P.buf()
    P.dma("sp", cvs[:], cv, writes=[b_cv])
    scv = P.sb("scv", [128, 16, 2], F32)
    P.op("act", lambda e: e.activation(out=scv[:], in_=cvs[:], func=ACTF.Silu), reads=[b_cv], writes=[b_cv])
    bias = P.sb("bias", [2, 2, CW], F32)
    b_bias = P.buf()
    for l in range(2):
        P.dma("sp", bias[:, l, :], adab[l:l + 1, :].partition_broadcast(2), writes=[b_bias])
    wt = [P.sb(f"wt{i}", [128, 16, 512], F32) for i in range(2)]
    b_wt = P.bufs(2)
    pm = [P.ps(f"pm{i}", [2, 512]) for i in range(2)]
    b_pm = P.bufs(2)
    res = P.sb("res", [2, 2, CW], F32)
    b_res = P.buf()
    k = 0
    for l in range(2):
        for cc in range(CW // 512):
            i = k % 2
            k += 1
            P.dma("sp", wt[i][:], adaw[l].rearrange("(kc p) n -> p kc n", p=128)[:, :, cc * 512:(cc + 1) * 512], writes=[b_wt[i]])
            for kc in range(16):
                P.op("pe", lambda e, i=i, kc=kc: e.matmul(pm[i][:], lhsT=scv[:, kc, :], rhs=wt[i][:, kc, :], start=(kc == 0), stop=(kc == 15)),
                     reads=[b_cv, b_wt[i]], writes=[b_pm[i]])
            P.op("dve", lambda e, i=i, l=l, cc=cc: e.tensor_tensor(out=res[:, l, cc * 512:(cc + 1) * 512], in0=pm[i][:],
                                                                  in1=bias[:, l, cc * 512:(cc + 1) * 512], op=ALU.add),
                 reads=[b_pm[i], b_bias], writes=[b_res])
    o = P.dma("sp", modv.rearrange("l r n -> r l n"), res[:], reads=[b_res])
    P.emit(final_waits=[o])
    return nc, P


def l0_inputs(inp):
    c = np.asarray(inp["c"], np.float32).reshape(D)
    cc = np.asarray(inp["c_ctx"], np.float32).reshape(D)
    cv = np.stack([c, cc], -1).reshape(16, 128, 2).transpose(1, 0, 2).copy()
    CW = 12288 // NCORE
    maps = []
    for j in range(NCORE):
        maps.append({"cv": cv, "adaw": np.ascontiguousarray(inp["ada_w"][:, :, j * CW:(j + 1) * CW]),
                     "adab": np.ascontiguousarray(inp["ada_b"][:, j * CW:(j + 1) * CW])})
    return maps


def l0_gather(results):
    return np.concatenate([np.asarray(r["modv"]) for r in results], axis=-1)


def load_mods(P, modl, modc, slots):
    out = {}
    for which, m in (("l", modl), ("c", modc)):
        for s in slots:
            t, b = load_bcast(P, f"mod_{which}{s}", m[0:1, s * D:(s + 1) * D], D)
            if s in (1, 4):
                P.op("pool", lambda e, t=t: e.tensor_scalar(out=t[:], in0=t[:], scalar1=1.0, scalar2=None, op0=ALU.add),
                     reads=[b], writes=[b])
            out[(s, which)] = (t, b)
    return out


def build_la(ntile=18, nctx_tile=2):
    nc = new_nc()
    NT = ntile * 128
    xin = din(nc, "xin", [NT, D])
    modl = din(nc, "modl", [1, 12288])
    modc = din(nc, "modc", [1, 12288])
    ident_d = din(nc, "ident", [128, 128])
    hT = dout(nc, "hT", [D, NT], BF16)
    P = Prog(nc)
    ident = P.sb("ident_s", [128, 128], F32)
    b_ident = P.buf("ident")
    P.dma("sp", ident[:], ident_d, writes=[b_ident])
    mods = load_mods(P, modl, modc, (0, 1))
    ln = LN(P, "ln")
    NB = 2
    xt = [P.sb(f"xt{i}", [128, D], F32) for i in range(NB)]
    b_xt = P.bufs(NB, "xt")
    ht = [P.sb(f"ht{i}", [128, D], F32) for i in range(NB)]
    b_ht = P.bufs(NB, "ht")
    pT = [P.ps(f"pT{i}", [128, 1024], F32) for i in range(2)]
    b_pT = P.bufs(2, "pT")
    hTs = [P.sb(f"hTs{i}", [128, 16, 512], BF16) for i in range(2)]
    b_hTs = P.bufs(2, "hTs")
    outs = []
    blocks = [(0, nctx_tile)] + [(t, min(4, ntile - t)) for t in range(nctx_tile, ntile, 4)]
    for bi, (t0, nt) in enumerate(blocks):
        hb = bi % 2
        for tb in range(nt):
            t = t0 + tb
            i = t % NB
            which = "c" if t < nctx_tile else "l"
            P.dma("sp", xt[i][:], xin[t * 128:(t + 1) * 128, :], writes=[b_xt[i]])
            ln.norm(ht[i][:], xt[i][:], b_xt[i], b_ht[i])
            sc, bsc = mods[(1, which)]
            sh, bsh = mods[(0, which)]
            P.op("dve", lambda e, i=i, sc=sc: e.tensor_tensor(out=ht[i][:], in0=ht[i][:], in1=sc[:], op=ALU.mult),
                 reads=[b_ht[i], bsc], writes=[b_ht[i]])
            P.op("pool", lambda e, i=i, sh=sh: e.tensor_tensor(out=ht[i][:], in0=ht[i][:], in1=sh[:], op=ALU.add),
                 reads=[b_ht[i], bsh], writes=[b_ht[i]])
            for half in range(2):
                for k in range(8):
                    kc = half * 8 + k
                    P.op("pe", lambda e, i=i, kc=kc, k=k, half=half: e.transpose(out=pT[half][:, k * 128:(k + 1) * 128],
                                                                                in_=ht[i][:, kc * 128:(kc + 1) * 128], identity=ident[:]),
                         reads=[b_ht[i], b_ident], writes=[b_pT[half]])
                P.op("act", lambda e, half=half, hb=hb, tb=tb: e.activation(
                    out=hTs[hb][:, half * 8:(half + 1) * 8, tb * 128:(tb + 1) * 128],
                    in_=pT[half][:].rearrange("p (k t) -> p k t", k=8), func=ACTF.Copy),
                    reads=[b_pT[half]], writes=[b_hTs[hb]])
        w = nt * 128
        o = P.dma("sp", hT.rearrange("(k p) n -> p k n", p=128)[:, :, t0 * 128:t0 * 128 + w], hTs[hb][:, :, 0:w],
                  reads=[b_hTs[hb]], writes=[])
        outs.append(o)
    P.emit(final_waits=outs)
    return nc, P


NCTX = 256


def lb_consts():
    ident_bf = np.eye(128, dtype=np.float32).astype(BF)
    rotT = np.zeros((128, 128), np.float32)
    for m in range(64):
        rotT[m + 64, m] = -1.0
    for m in range(64, 128):
        rotT[m - 64, m] = 1.0
    ones = np.ones((128, 128), np.float32)
    s = np.arange(64)[:, None]
    t = np.arange(64)[None, :]
    masks = np.stack([(s <= t), (s >= t)]).astype(np.float32)
    reset01 = np.ones((128, 512), np.float32)
    reset01[:, ::64] = 0.0
    return {"ident_bf": ident_bf, "rotT": rotT, "ones": ones, "ones_bf": ones.astype(BF), "masks": masks, "reset01": reset01}


def build_lb(nblk=32, upto=3, debug=False):
    nc = new_nc()
    NLAT = nblk * 512
    NTOK = NCTX + NLAT
    NT128 = NTOK // 128
    hT = din(nc, "hT", [D, NTOK], BF16)
    wfm = din(nc, "wfm", [6, D, 128])
    wtm = din(nc, "wtm", [D, 256])
    lbv = din(nc, "lbv", [128, 2, 3])
    gvec_d = din(nc, "gvec", [128, 3])
    cosT = din(nc, "cosT", [128, NLAT])
    sinT = din(nc, "sinT", [128, NLAT])
    c_ident = din(nc, "ident_bf", [128, 128], BF16)
    c_rotT = din(nc, "rotT", [128, 128])
    c_ones = din(nc, "ones", [128, 128])
    c_ones_bf = din(nc, "ones_bf", [128, 128], BF16)
    c_masks = din(nc, "masks", [2, 64, 64])
    c_reset = din(nc, "reset01", [128, 512])
    aT = dout(nc, "aT", [128, NTOK], BF16)
    bT = dout(nc, "bT", [128, NTOK], BF16)
    dscr_ = dout if debug else dscr
    s_q = dscr_(nc, "s_q", [128, NTOK], BF16)
    s_g = dscr_(nc, "s_g", [128, NTOK], BF16)
    s_lf = [dscr_(nc, f"s_lf{d}", [128, NTOK], F32) for d in range(2)]
    s_v = dscr_(nc, "s_v", [NTOK, 128], BF16)
    s_qb = dscr_(nc, "s_qb", [128, NTOK], BF16)
    s_of = dscr_(nc, "s_of", [128, NTOK], F32)
    P = Prog(nc)
    blocks = [(0, NCTX)] + [(NCTX + 512 * i, 512) for i in range(nblk)]
    NBLK = len(blocks)
    db = {n: P.bufs(NBLK, n) for n in ("q", "g", "lf0", "lf1", "v", "qb", "of")}

    def cload(name, src, shape, dt):
        t = P.sb(name, shape, dt)
        b = P.buf(name)
        P.dma("sp", t[:], src, writes=[b])
        return t, b

    ident, b_ident = cload("ident", c_ident, [128, 128], BF16)
    rotT, b_rot = cload("rotT_s", c_rotT, [128, 128], F32)
    ones, b_ones = cload("ones_s", c_ones, [128, 128], F32)
    ones_bf, b_onesbf = cload("onesbf_s", c_ones_bf, [128, 128], BF16)
    masks = P.sb("masks_s", [64, 2, 64], F32)
    b_masks = P.buf()
    P.dma("sp", masks[:], c_masks.rearrange("d s t -> s d t"), writes=[b_masks])
    reset01, b_reset = cload("reset_s", c_reset, [128, 512], F32)
    gvec, b_gvec = cload("gvec_s", gvec_d, [128, 3], F32)
    lbr = P.sb("lbr", [128, 2, 3], F32)
    b_lb = P.buf()
    P.dma("sp", lbr[:], lbv, writes=[b_lb])
    lbt = P.sb("lbt", [128, 8], F32)
    P.op("act", lambda e: e.activation(out=lbr[:], in_=lbr[:], func=ACTF.Exp), reads=[b_lb], writes=[b_lb])
    P.op("dve", lambda e: e.tensor_reduce(out=lbt[:, 0:2], in_=lbr[:], axis=AX.X, op=ALU.add), reads=[b_lb], writes=[b_lb])
    P.op("dve", lambda e: e.reciprocal(out=lbt[:, 0:2], in_=lbt[:, 0:2]), reads=[b_lb], writes=[b_lb])
    P.op("dve", lambda e: e.tensor_tensor(out=lbt[:, 2:4], in0=lbr[:, :, 0], in1=lbt[:, 0:2], op=ALU.mult), reads=[b_lb], writes=[b_lb])
    P.op("dve", lambda e: e.tensor_scalar(out=lbt[:, 4:6], in0=lbt[:, 2:4], scalar1=-1.0, scalar2=1.0, op0=ALU.mult, op1=ALU.add),
         reads=[b_lb], writes=[b_lb])

    KT = P.sb("KT", [128, NTOK], BF16)
    Vres = P.sb("Vres", [128, NT128, 128], BF16)
    b_KT = P.bufs(NBLK, "KT")
    b_V = P.bufs(NBLK, "V")
    pb = [P.ps(f"pb{i}", [128, 512], F32) for i in range(8)]
    b_pb = P.bufs(8, "pb", excl=True)
    pbf = [pb[6][:].bitcast(BF16), pb[7][:].bitcast(BF16)]
    b_pbf = [b_pb[6], b_pb[7]]
    A = Arena(P, "arena", 27 * 1024)

    wfm_bf = A.alloc([128, 6, 16, 128], BF16)
    wtm_bf = A.alloc([128, 16, 256], BF16)
    b_w = P.buf("w")
    wst = [A.alloc([128, 16, 128], F32) for _ in range(2)]
    b_wst = P.bufs(2)
    for idx in range(8):
        i = idx % 2
        if idx < 6:
            src = wfm[idx].rearrange("(kc p) n -> p kc n", p=128)
            dst = wfm_bf[:, idx]
        else:
            h = idx - 6
            src = wtm.rearrange("(kc p) n -> p kc n", p=128)[:, :, h * 128:(h + 1) * 128]
            dst = wtm_bf[:, :, h * 128:(h + 1) * 128]
        P.dma("sp", wst[i][:], src, writes=[b_wst[i]])
        if idx % 2:
            P.op("act", lambda e, i=i, dst=dst: e.activation(out=dst, in_=wst[i][:], func=ACTF.Copy), reads=[b_wst[i]], writes=[b_w])
        else:
            P.op("dve", lambda e, i=i, dst=dst: e.tensor_copy(out=dst, in_=wst[i][:]), reads=[b_wst[i]], writes=[b_w])
    hb = [A.alloc([128, 16, 512], BF16) for _ in range(2)]
    b_hb = P.bufs(2, "hb")
    NTMP = 2
    tmpA = [A.alloc([128, 512], F32) for _ in range(NTMP)]
    tmpB = [A.alloc([128, 512], F32) for _ in range(NTMP)]
    tmpC = [A.alloc([128, 512], F32) for _ in range(NTMP)]
    b_tA, b_tB, b_tC = P.bufs(NTMP), P.bufs(NTMP), P.bufs(NTMP)
    obf = [A.alloc([128, 512], BF16) for _ in range(4)]
    b_obf = P.bufs(4)
    cs = [A.alloc([128, 2, 512], F32) for _ in range(2)]
    b_cs = P.bufs(2)
    vo = [A.alloc([128, 128], BF16) for _ in range(2)]
    b_vo = P.bufs(2)
    kk = [0, 0, 0, 0]

    for bi, (s0, w) in enumerate(blocks):
        lat = bi > 0
        i = bi % 2
        P.dma("sp", hb[i][:, :, 0:w], hT.rearrange("(kc p) n -> p kc n", p=128)[:, :, s0:s0 + w], writes=[b_hb[i]])
        if lat:
            l0 = s0 - NCTX
            P.dma("sp", cs[i][:, 0, 0:w], cosT[:, l0:l0 + w], writes=[b_cs[i]])
            P.dma("sp", cs[i][:, 1, 0:w], sinT[:, l0:l0 + w], writes=[b_cs[i]])
        for idx in range(6):
            pf_i = kk[2] % 3
            kk[2] += 1
            pf, bpf = pb[pf_i], b_pb[pf_i]
            for kc in range(16):
                P.op("pe", lambda e, pf=pf, idx=idx, kc=kc, i=i, w=w: e.matmul(pf[:, 0:w], lhsT=wfm_bf[:, idx, kc, :], rhs=hb[i][:, kc, 0:w],
                                                                             start=(kc == 0), stop=(kc == 15)),
                     reads=[b_w, b_hb[i]], writes=[bpf])
            if idx == 0 or idx == 3:
                oi = kk[1] % 4
                kk[1] += 1
                fn = ACTF.Copy if idx == 0 else ACTF.Silu
                P.op("act", lambda e, oi=oi, pf=pf, w=w, fn=fn: e.activation(out=obf[oi][:, 0:w], in_=pf[:, 0:w], func=fn),
                     reads=[bpf], writes=[b_obf[oi]])
                dst, dbuf = (s_q, db["q"]) if idx == 0 else (s_g, db["g"])
                P.dma("sp", dst[:, s0:s0 + w], obf[oi][:, 0:w], reads=[b_obf[oi]], writes=[dbuf[bi]])
            elif idx in (1, 2):
                d = idx - 1
                ti = kk[0] % NTMP
                kk[0] += 1
                tA, bA = tmpA[ti], b_tA[ti]
                P.op("act", lambda e, tA=tA, pf=pf, w=w: e.activation(out=tA[:, 0:w], in_=pf[:, 0:w], func=ACTF.Sigmoid), reads=[bpf], writes=[bA])
                P.op("dve", lambda e, tA=tA, w=w, d=d: e.tensor_scalar(out=tA[:, 0:w], in0=tA[:, 0:w], scalar1=lbt[:, 4 + d:5 + d], scalar2=lbt[:, 2 + d:3 + d],
                                                                      op0=ALU.mult, op1=ALU.add), reads=[bA, b_lb], writes=[bA])
                P.op("act", lambda e, tA=tA, w=w: e.activation(out=tA[:, 0:w], in_=tA[:, 0:w], func=ACTF.Ln), reads=[bA], writes=[bA])
                P.dma("sp", s_lf[d][:, s0:s0 + w], tA[:, 0:w], reads=[bA], writes=[db[f"lf{d}"][bi]])
            else:
                ti = kk[0] % NTMP
                kk[0] += 1
                tA, bA, tB, bB, tC, bC = tmpA[ti], b_tA[ti], tmpB[ti], b_tB[ti], tmpC[ti], b_tC[ti]
                pn_i = 3 + kk[3] % 2
                kk[3] += 1
                pn, bpn = pb[pn_i], b_pb[pn_i]
                gcol = 1 if idx == 4 else 2
                P.op("act", lambda e, tA=tA, pf=pf, w=w: e.activation(out=tA[:, 0:w], in_=pf[:, 0:w], func=ACTF.Square), reads=[bpf], writes=[bA])
                P.op("pe", lambda e, pn=pn, tA=tA, w=w: e.matmul(pn[:, 0:w], lhsT=ones[:], rhs=tA[:, 0:w], start=True, stop=True),
                     reads=[b_ones, bA], writes=[bpn])
                P.op("act", lambda e, tB=tB, pn=pn, w=w: e.activation(out=tB[:, 0:w], in_=pn[:, 0:w], func=ACTF.Sqrt, bias=EPS, scale=1.0 / 128),
                     reads=[bpn], writes=[bB])
                P.op("dve", lambda e, tB=tB, w=w: e.reciprocal(out=tB[:, 0:w], in_=tB[:, 0:w]), reads=[bB], writes=[bB])
                P.op("dve", lambda e, tA=tA, tB=tB, pf=pf, w=w, gcol=gcol: e.scalar_tensor_tensor(out=tA[:, 0:w], in0=pf[:, 0:w], scalar=gvec[:, gcol:gcol + 1],
                                                                                                 in1=tB[:, 0:w], op0=ALU.mult, op1=ALU.mult),
                     reads=[bpf, bB, b_gvec], writes=[bA])
                if idx == 4:
                    oi = kk[1] % 4
                    kk[1] += 1
                    dest, bdest = obf[oi][:, 0:w], b_obf[oi]
                else:
                    dest, bdest = KT[:, s0:s0 + w], b_KT[bi]
                if lat:
                    pn2_i = 3 + kk[3] % 2
                    kk[3] += 1
                    pr, bpr = pb[pn2_i], b_pb[pn2_i]
                    P.op("pe", lambda e, pr=pr, tA=tA, w=w: e.matmul(pr[:, 0:w], lhsT=rotT[:], rhs=tA[:, 0:w], start=True, stop=True),
                         reads=[b_rot, bA], writes=[bpr])
                    P.op("dve", lambda e, tB=tB, pr=pr, w=w, i=i: e.tensor_tensor(out=tB[:, 0:w], in0=pr[:, 0:w], in1=cs[i][:, 1, 0:w], op=ALU.mult),
                         reads=[bpr, b_cs[i]], writes=[bB])
                    P.op("pool", lambda e, tC=tC, tA=tA, w=w, i=i: e.tensor_tensor(out=tC[:, 0:w], in0=tA[:, 0:w], in1=cs[i][:, 0, 0:w], op=ALU.mult),
                         reads=[bA, b_cs[i]], writes=[bC])
                    P.op("dve", lambda e, dest=dest, tB=tB, tC=tC, w=w: e.tensor_tensor(out=dest, in0=tB[:, 0:w], in1=tC[:, 0:w], op=ALU.add),
                         reads=[bB, bC], writes=[bdest])
                else:
                    P.op("act", lambda e, dest=dest, tA=tA, w=w: e.activation(out=dest, in_=tA[:, 0:w], func=ACTF.Copy), reads=[bA], writes=[bdest])
                if idx == 4:
                    P.dma("sp", s_qb[:, s0:s0 + w], dest, reads=[bdest], writes=[db["qb"][bi]])
        for tt in range(w // 128):
            pt_i = 5 + tt % 2
            pt, bpt = pb[pt_i], b_pb[pt_i]
            for kc in range(16):
                P.op("pe", lambda e, pt=pt, kc=kc, i=i, tt=tt: e.matmul(pt[:, 0:256], lhsT=hb[i][:, kc, tt * 128:(tt + 1) * 128], rhs=wtm_bf[:, kc, :],
                                                                       start=(kc == 0), stop=(kc == 15)),
                     reads=[b_w, b_hb[i]], writes=[bpt])
            vi = tt % 2
            P.op("act", lambda e, pt=pt, vi=vi: e.activation(out=vo[vi][:], in_=pt[:, 0:128], func=ACTF.Copy), reads=[bpt], writes=[b_vo[vi]])
            tok = s0 + tt * 128
            P.dma("sp", s_v[tok:tok + 128, :], vo[vi][:], reads=[b_vo[vi]], writes=[db["v"][bi]])
            P.op("dve", lambda e, pt=pt, tok=tok: e.tensor_copy(out=Vres[:, tok // 128, :], in_=pt[:, 128:256]), reads=[bpt], writes=[b_V[bi]])

    if upto == 1:
        kd_ = dout(nc, "KTo", [128, NTOK], BF16)
        vd_ = dout(nc, "Vo", [128, NT128, 128], BF16)
        o1 = P.dma("sp", kd_, KT[:], reads=b_KT)
        o2 = P.dma("sp", vd_, Vres[:], reads=b_V)
        P.barrier()
        o3 = P.dma("sp", kd_[:, 0:8], KT[:, 0:8])
        P.emit(final_waits=[o1, o2, o3])
        return nc, P
    P.barrier()
    A.reset()
    S_f = A.alloc([128, 128], F32)
    S_bf = [A.alloc([128, 128], BF16) for _ in range(2)]
    b_S = P.buf("S")
    b_Sbf = P.bufs(2, "Sbf")
    NS = 2
    qt = [A.alloc([128, 512], BF16) for _ in range(NS)]
    lf = [A.alloc([128, 512], F32) for _ in range(NS)]
    vch = [A.alloc([64, 8, 128], BF16) for _ in range(NS)]
    b_qt, b_lf, b_vch = P.bufs(NS), P.bufs(NS), P.bufs(NS)
    cum = [A.alloc([128, 512], F32) for _ in range(NS)]
    G = [A.alloc([128, 512], F32) for _ in range(NS)]
    Gr = [A.alloc([128, 512], F32) for _ in range(NS)]
    EE = [A.alloc([128, 512], F32) for _ in range(NS)]
    kkt = [A.alloc([128, 512], F32) for _ in range(NS)]
    tot = [A.alloc([128, 8], F32) for _ in range(NS)]
    etot = [A.alloc([128, 8], F32) for _ in range(NS)]
    qd = [A.alloc([128, 512], BF16) for _ in range(NS)]
    kd = [A.alloc([128, 512], BF16) for _ in range(NS)]
    qe = [A.alloc([128, 512], BF16) for _ in range(NS)]
    klT = [A.alloc([128, 512], BF16) for _ in range(NS)]
    b_el = P.bufs(NS, "el")
    b_qd, b_kd, b_qe, b_klT = P.bufs(NS), P.bufs(NS), P.bufs(NS), P.bufs(NS)
    kl_sb = [A.alloc([64, 128], BF16) for _ in range(2)]
    b_kl = P.bufs(2)
    sT_sb = [A.alloc([64, 64], BF16) for _ in range(2)]
    b_sT = P.bufs(2)
    ofl = [A.alloc([128, 512], F32) for _ in range(2)]
    b_ofl = P.bufs(2)
    gsl = [A.alloc([128, 512], BF16) for _ in range(2)]
    b_gsl = P.bufs(2)
    osb = [A.alloc([128, 512], F32) for _ in range(2)]
    b_osb = P.bufs(2)
    tsq = [A.alloc([128, 512], F32) for _ in range(2)]
    b_tsq = P.bufs(2)
    abf = [A.alloc([128, 512], BF16) for _ in range(2)]
    b_abf = P.bufs(2)
    out_dmas = []
    cnt = 0
    for d in range(2):
        P.op("dve", lambda e: e.memset(S_f[:], 0.0), writes=[b_S])
        P.op("dve", lambda e: e.memset(S_bf[0][:], 0.0), writes=[b_Sbf[0]])
        sbi = 0
        order = list(range(NBLK)) if d == 0 else [0] + list(range(NBLK - 1, 0, -1))
        for bi in order:
            s0, w = blocks[bi]
            nch = w // 64
            i = cnt % NS
            cnt += 1
            P.dma("sp", qt[i][:, 0:w], s_q[:, s0:s0 + w], reads=[db["q"][bi]], writes=[b_qt[i]])
            P.dma("sp", lf[i][:, 0:w], s_lf[d][:, s0:s0 + w], reads=[db[f"lf{d}"][bi]], writes=[b_lf[i]])
            P.dma("sp", vch[i][:, 0:nch, :], s_v[s0:s0 + w, :].rearrange("(n p) v -> p n v", p=64), reads=[db["v"][bi]], writes=[b_vch[i]])
            be = b_el[i]
            c3 = cum[i][:, 0:w].rearrange("p (n t) -> p n t", t=64)
            G3 = G[i][:, 0:w].rearrange("p (n t) -> p n t", t=64)
            P.op("dve", lambda e, i=i, w=w: e.tensor_tensor_scan(out=cum[i][:, 0:w], data0=reset01[:, 0:w], data1=lf[i][:, 0:w], initial=0.0,
                                                                op0=ALU.mult, op1=ALU.add), reads=[b_reset, b_lf[i]], writes=[be])
            P.op("dve", lambda e, i=i, c3=c3, nch=nch: e.tensor_copy(out=tot[i][:, 0:nch], in_=c3[:, :, 63]), reads=[be], writes=[be])
            tot_b = tot[i][:, 0:nch].unsqueeze(2).to_broadcast([128, nch, 64])
            if d == 0:
                P.op("pool", lambda e, i=i, w=w: e.tensor_copy(out=G[i][:, 0:w], in_=cum[i][:, 0:w]), reads=[be], writes=[be])
                ref = G3[:, :, 31:32]
            else:
                P.op("dve", lambda e, i=i, w=w: e.tensor_tensor(out=G[i][:, 0:w], in0=lf[i][:, 0:w], in1=cum[i][:, 0:w], op=ALU.subtract),
                     reads=[be, b_lf[i]], writes=[be])
                P.op("dve", lambda e, G3=G3, tot_b=tot_b: e.tensor_tensor(out=G3, in0=G3, in1=tot_b, op=ALU.add), reads=[be], writes=[be])
                ref = G3[:, :, 32:33]
            ref_b = ref.to_broadcast([128, nch, 64])
            Gr3 = Gr[i][:, 0:w].rearrange("p (n t) -> p n t", t=64)
            P.op("dve", lambda e, Gr3=Gr3, G3=G3, ref_b=ref_b: e.tensor_tensor(out=Gr3, in0=G3, in1=ref_b, op=ALU.subtract), reads=[be], writes=[be])
            P.op("act", lambda e, i=i, w=w: e.activation(out=kkt[i][:, 0:w], in_=lf[i][:, 0:w], func=ACTF.Exp), reads=[b_lf[i]], writes=[be])
            P.op("pool", lambda e, i=i, w=w: e.tensor_scalar(out=kkt[i][:, 0:w], in0=kkt[i][:, 0:w], scalar1=-1.0, scalar2=1.0, op0=ALU.mult, op1=ALU.add),
                 reads=[be], writes=[be])
            P.op("act", lambda e, i=i, w=w: e.activation(out=EE[i][:, 0:w], in_=Gr[i][:, 0:w], func=ACTF.Exp), reads=[be], writes=[be])
            P.op("dve", lambda e, i=i, w=w: e.tensor_tensor(out=qd[i][:, 0:w], in0=qt[i][:, 0:w], in1=EE[i][:, 0:w], op=ALU.mult),
                 reads=[be, b_qt[i]], writes=[b_qd[i]])
            P.op("act", lambda e, i=i, w=w: e.activation(out=EE[i][:, 0:w], in_=Gr[i][:, 0:w], func=ACTF.Exp, scale=-1.0), reads=[be], writes=[be])
            P.op("dve", lambda e, i=i, w=w: e.tensor_tensor(out=kd[i][:, 0:w], in0=kkt[i][:, 0:w], in1=EE[i][:, 0:w], op=ALU.mult),
                 reads=[be], writes=[b_kd[i]])
            P.op("act", lambda e, i=i, w=w: e.activation(out=EE[i][:, 0:w], in_=G[i][:, 0:w], func=ACTF.Exp), reads=[be], writes=[be])
            P.op("dve", lambda e, i=i, w=w: e.tensor_tensor(out=qe[i][:, 0:w], in0=qt[i][:, 0:w], in1=EE[i][:, 0:w], op=ALU.mult),
                 reads=[be, b_qt[i]], writes=[b_qe[i]])
            P.op("dve", lambda e, Gr3=Gr3, G3=G3, tot_b=tot_b: e.tensor_tensor(out=Gr3, in0=tot_b, in1=G3, op=ALU.subtract), reads=[be], writes=[be])
            P.op("act", lambda e, i=i, w=w: e.activation(out=EE[i][:, 0:w], in_=Gr[i][:, 0:w], func=ACTF.Exp), reads=[be], writes=[be])
            P.op("dve", lambda e, i=i, w=w: e.tensor_tensor(out=klT[i][:, 0:w], in0=kkt[i][:, 0:w], in1=EE[i][:, 0:w], op=ALU.mult),
                 reads=[be], writes=[b_klT[i]])
            P.op("act", lambda e, i=i, nch=nch: e.activation(out=etot[i][:, 0:nch], in_=tot[i][:, 0:nch], func=ACTF.Exp), reads=[be], writes=[be])
            po_i = cnt % 2
            po, bpo = pb[po_i], b_pb[po_i]
            chunks = list(range(nch)) if d == 0 else list(range(nch - 1, -1, -1))
            for ci, n in enumerate(chunks):
                c0, c1 = n * 64, (n + 1) * 64
                j = ci % 2
                P.op("pe", lambda e, j=j, i=i, c0=c0, c1=c1: e.transpose(out=pbf[j][0:64, 0:128], in_=klT[i][:, c0:c1], identity=ident[:]),
                     reads=[b_klT[i], b_ident], writes=[b_pbf[j]])
                P.op("act", lambda e, j=j: e.activation(out=kl_sb[j][:], in_=pbf[j][0:64, 0:128], func=ACTF.Copy), reads=[b_pbf[j]], writes=[b_kl[j]])
                ps_s, bps = pb[2 + j], b_pb[2 + j]
                P.op("pe", lambda e, ps_s=ps_s, i=i, c0=c0, c1=c1: e.matmul(ps_s[0:64, 0:64], lhsT=kd[i][:, c0:c1], rhs=qd[i][:, c0:c1], start=True, stop=True),
                     reads=[b_kd[i], b_qd[i]], writes=[bps])
                P.op("dve", lambda e, ps_s=ps_s, j=j, d=d: e.tensor_tensor(out=sT_sb[j][:], in0=ps_s[0:64, 0:64], in1=masks[:, d, :], op=ALU.mult),
                     reads=[bps, b_masks], writes=[b_sT[j]])
                P.op("pe", lambda e, po=po, i=i, n=n, j=j, c0=c0, c1=c1: e.matmul(po[:, c0:c1], lhsT=vch[i][:, n, :], rhs=sT_sb[j][:], start=True, stop=False),
                     reads=[b_vch[i], b_sT[j]], writes=[bpo])
                P.op("pe", lambda e, po=po, i=i, sbi=sbi, c0=c0, c1=c1: e.matmul(po[:, c0:c1], lhsT=S_bf[sbi][:], rhs=qe[i][:, c0:c1], start=False, stop=True),
                     reads=[b_Sbf[sbi], b_qe[i]], writes=[bpo])
                pst, bpst = pb[4], b_pb[4]
                P.op("pe", lambda e, pst=pst, j=j, i=i, n=n: e.matmul(pst[:, 0:128], lhsT=kl_sb[j][:], rhs=vch[i][:, n, :], start=True, stop=True),
                     reads=[b_kl[j], b_vch[i]], writes=[bpst])
                P.op("dve", lambda e, pst=pst, i=i, n=n: e.scalar_tensor_tensor(out=S_f[:], in0=S_f[:], scalar=etot[i][:, n:n + 1], in1=pst[:, 0:128],
                                                                               op0=ALU.mult, op1=ALU.add), reads=[b_S, be, bpst], writes=[b_S])
                sbi = 1 - sbi
                P.op("act", lambda e, sbi=sbi: e.activation(out=S_bf[sbi][:], in_=S_f[:], func=ACTF.Copy), reads=[b_S], writes=[b_Sbf[sbi]])
            if d == 0:
                oi = cnt % 2
                P.op("act", lambda e, oi=oi, po=po, w=w: e.activation(out=osb[oi][:, 0:w], in_=po[:, 0:w], func=ACTF.Copy), reads=[bpo], writes=[b_osb[oi]])
                P.dma("sp", s_of[:, s0:s0 + w], osb[oi][:, 0:w], reads=[b_osb[oi]], writes=[db["of"][bi]])
            else:
                oi = cnt % 2
                P.dma("sp", ofl[oi][:, 0:w], s_of[:, s0:s0 + w], reads=[db["of"][bi]], writes=[b_ofl[oi]])
                P.dma("sp", gsl[oi][:, 0:w], s_g[:, s0:s0 + w], reads=[db["g"][bi]], writes=[b_gsl[oi]])
                P.op("dve", lambda e, oi=oi, po=po, w=w: e.tensor_tensor(out=osb[oi][:, 0:w], in0=po[:, 0:w], in1=ofl[oi][:, 0:w], op=ALU.add),
                     reads=[bpo, b_ofl[oi]], writes=[b_osb[oi]])
                P.op("act", lambda e, oi=oi, w=w: e.activation(out=tsq[oi][:, 0:w], in_=osb[oi][:, 0:w], func=ACTF.Square), reads=[b_osb[oi]], writes=[b_tsq[oi]])
                pn, bpn = pb[5], b_pb[5]
                P.op("pe", lambda e, pn=pn, oi=oi, w=w: e.matmul(pn[:, 0:w], lhsT=ones[:], rhs=tsq[oi][:, 0:w], start=True, stop=True),
                     reads=[b_ones, b_tsq[oi]], writes=[bpn])
                P.op("act", lambda e, pn=pn, oi=oi, w=w: e.activation(out=tsq[oi][:, 0:w], in_=pn[:, 0:w], func=ACTF.Sqrt, bias=EPS, scale=1.0 / 128),
                     reads=[bpn], writes=[b_tsq[oi]])
                P.op("dve", lambda e, oi=oi, w=w: e.reciprocal(out=tsq[oi][:, 0:w], in_=tsq[oi][:, 0:w]), reads=[b_tsq[oi]], writes=[b_tsq[oi]])
                P.op("dve", lambda e, oi=oi, w=w: e.tensor_tensor(out=osb[oi][:, 0:w], in0=osb[oi][:, 0:w], in1=tsq[oi][:, 0:w], op=ALU.mult),
                     reads=[b_osb[oi], b_tsq[oi]], writes=[b_osb[oi]])
                P.op("dve", lambda e, oi=oi, w=w: e.scalar_tensor_tensor(out=abf[oi][:, 0:w], in0=osb[oi][:, 0:w], scalar=gvec[:, 0:1], in1=gsl[oi][:, 0:w],
                                                                        op0=ALU.mult, op1=ALU.mult), reads=[b_osb[oi], b_gsl[oi], b_gvec], writes=[b_abf[oi]])
                out_dmas.append(P.dma("sp", aT[:, s0:s0 + w], abf[oi][:, 0:w], reads=[b_abf[oi]]))

    if upto == 2:
        P.barrier()
        o3 = P.dma("sp", bT[:, 0:8], KT[:, 0:8])
        P.emit(final_waits=out_dmas + [o3])
        return nc, P
    P.barrier()
    A.reset()
    SCALE = 128 ** -0.5
    qb_s = [A.alloc([128, 512], BF16) for _ in range(2)]
    b_qbs = P.bufs(2)
    NPT = 4
    pT_sb = [A.alloc([128, 512], BF16) for _ in range(NPT)]
    b_pTs = P.bufs(NPT)
    rz = [A.alloc([128, 512], F32) for _ in range(2)]
    b_rz = P.bufs(2)
    ob = [A.alloc([128, 512], BF16) for _ in range(2)]
    b_ob = P.bufs(2)
    accD = [A.alloc([128, 512], F32) for _ in range(2)]
    accP = [A.alloc([128, 512], F32) for _ in range(2)]
    b_accD, b_accP = P.bufs(2), P.bufs(2)
    allKT = b_KT
    allV = b_V
    kcount = 0
    for bi, (s0, w) in enumerate(blocks):
        i = bi % 2
        P.dma("sp", qb_s[i][:, 0:w], s_qb[:, s0:s0 + w], reads=[db["qb"][bi]], writes=[b_qbs[i]])
        nkc = (NCTX // 128) if bi == 0 else NT128
        pO, bpO = pb[3 + i], b_pb[3 + i]
        pZ, bpZ = pb[5 + i], b_pb[5 + i]
        for kc in range(nkc):
            kblk = 0 if kc < 2 else 1 + (kc * 128 - NCTX) // 512
            si = kcount % 3
            ti = kcount % NPT
            kcount += 1
            pS, bpS = pb[si], b_pb[si]
            P.op("pe", lambda e, pS=pS, kc=kc, i=i, w=w: e.matmul(pS[:, 0:w], lhsT=KT[:, kc * 128:(kc + 1) * 128], rhs=qb_s[i][:, 0:w], start=True, stop=True),
                 reads=[allKT[kblk], b_qbs[i]], writes=[bpS])
            P.op("act", lambda e, pS=pS, ti=ti, w=w: e.activation(out=pT_sb[ti][:, 0:w], in_=pS[:, 0:w], func=ACTF.Exp, scale=SCALE),
                 reads=[bpS], writes=[b_pTs[ti]])
            P.op("pe", lambda e, pO=pO, kc=kc, ti=ti, w=w, nkc=nkc: e.matmul(pO[:, 0:w], lhsT=Vres[:, kc, :], rhs=pT_sb[ti][:, 0:w], start=(kc == 0), stop=(kc == nkc - 1)),
                 reads=[allV[kblk], b_pTs[ti]], writes=[bpO])
            eng = "dve" if kc % 2 == 0 else "pool"
            acc, bacc = (accD[i], b_accD[i]) if kc % 2 == 0 else (accP[i], b_accP[i])
            if kc < 2:
                P.op(eng, lambda e, acc=acc, ti=ti, w=w: e.tensor_copy(out=acc[:, 0:w], in_=pT_sb[ti][:, 0:w]), reads=[b_pTs[ti]], writes=[bacc])
            else:
                P.op(eng, lambda e, acc=acc, ti=ti, w=w: e.tensor_tensor(out=acc[:, 0:w], in0=acc[:, 0:w], in1=pT_sb[ti][:, 0:w], op=ALU.add), reads=[b_pTs[ti], bacc], writes=[bacc])
        P.op("pe", lambda e, pZ=pZ, i=i, w=w: e.matmul(pZ[:, 0:w], lhsT=ones[:], rhs=accD[i][:, 0:w], start=True, stop=False), reads=[b_ones, b_accD[i]], writes=[bpZ])
        P.op("pe", lambda e, pZ=pZ, i=i, w=w: e.matmul(pZ[:, 0:w], lhsT=ones[:], rhs=accP[i][:, 0:w], start=False, stop=True), reads=[b_ones, b_accP[i]], writes=[bpZ])
        P.op("dve", lambda e, i=i, pZ=pZ, w=w: e.reciprocal(out=rz[i][:, 0:w], in_=pZ[:, 0:w]), reads=[bpZ], writes=[b_rz[i]])
        P.op("dve", lambda e, i=i, pO=pO, w=w: e.tensor_tensor(out=ob[i][:, 0:w], in0=pO[:, 0:w], in1=rz[i][:, 0:w], op=ALU.mult),
             reads=[bpO, b_rz[i]], writes=[b_ob[i]])
        out_dmas.append(P.dma("sp", bT[:, s0:s0 + w], ob[i][:, 0:w], reads=[b_ob[i]]))
    st = P.emit(final_waits=out_dmas)
    print("LB stats", st, flush=True)
    return nc, P


def lb_inputs(inp, hT_all, nblk=32):
    NLAT = nblk * 512
    w_in = np.asarray(inp["ev_w_in"][0])
    consts = lb_consts()
    n_tok = 16384
    rows = n_tok // 64
    row = np.repeat(np.arange(rows, dtype=np.float32), 64)
    col = np.tile(np.arange(64, dtype=np.float32), rows)
    n_freq = 32
    inv = (10000.0 ** (-np.arange(n_freq, dtype=np.float32) / n_freq)).astype(np.float32)
    ang = np.concatenate([row[:, None] * inv, col[:, None] * inv], -1)
    cos = np.cos(ang).astype(np.float32)
    sin = np.sin(ang).astype(np.float32)
    cosT = np.ascontiguousarray(np.concatenate([cos, cos], -1).T[:, :NLAT])
    sinT = np.ascontiguousarray(np.concatenate([sin, sin], -1).T[:, :NLAT])
    hTs = np.ascontiguousarray(hT_all[:, :NCTX + NLAT])
    maps = []
    for j in range(NCORE):
        c = lambda base, wd=128: w_in[:, base + j * wd: base + (j + 1) * wd]
        kvh = j // 4
        wq, wff, wfb, wi, wg = c(0), c(1024), c(2048), c(3072), c(4096)
        wqb = c(5120)
        wkb = w_in[:, 6144 + kvh * 128: 6144 + (kvh + 1) * 128]
        wvb = w_in[:, 6400 + kvh * 128: 6400 + (kvh + 1) * 128]
        wfm = np.ascontiguousarray(np.stack([wq, wff, wfb, wg, wqb, wkb]))
        wtm = np.ascontiguousarray(np.concatenate([wi, wvb], 1))
        lbv = np.ascontiguousarray(np.asarray(inp["hgrn_lb"])[:, :, j * 128:(j + 1) * 128].transpose(2, 0, 1))
        gvec = np.ascontiguousarray(np.stack([inp["hgrn_norm_g"][0], inp["gqa_q_norm_g"][0], inp["gqa_k_norm_g"][0]], -1))
        m = {"hT": hTs, "wfm": wfm, "wtm": wtm, "lbv": lbv, "gvec": gvec, "cosT": cosT, "sinT": sinT}
        m.update(consts)
        maps.append(m)
    return maps


def build_lc(ntile=18, nctx_tile=2):
    nc = new_nc()
    NT = ntile * 128
    oT = din(nc, "oT", [D, NT], BF16)
    wo = din(nc, "wo", [D, D])
    xin = din(nc, "xin", [NT, D])
    modl = din(nc, "modl", [1, 12288])
    modc = din(nc, "modc", [1, 12288])
    lng = din(nc, "lng", [1, D])
    lnb = din(nc, "lnb", [1, D])
    wr = din(nc, "wr", [D, 16])
    ident_d = din(nc, "ident", [128, 128])
    xmid = dout(nc, "xmid", [NT, D])
    h2o = dout(nc, "h2", [NT, D], BF16)
    affo = dout(nc, "aff", [NT, 16])
    P = Prog(nc)
    ident = P.sb("ident_s", [128, 128], F32)
    b_ident = P.buf()
    P.dma("sp", ident[:], ident_d, writes=[b_ident])
    wr_s = P.sb("wr_s", [128, 16, 16], F32)
    b_wr = P.buf()
    P.dma("sp", wr_s[:], wr.rearrange("(kc p) n -> p kc n", p=128), writes=[b_wr])
    lng_s, b_lng = load_bcast(P, "lng_s", lng, D)
    lnb_s, b_lnb = load_bcast(P, "lnb_s", lnb, D)
    wo_bf = P.sb("wo_bf", [128, 16, D], BF16)
    b_wo = P.buf()
    pb = [P.ps(f"pb{i}", [128, 512], F32) for i in range(8)]
    b_pb = P.bufs(8, "pb", excl=True)
    A = Arena(P, "arena", 22 * 1024)
    wst = [A.alloc([128, 16, 256], F32) for _ in range(2)]
    b_wst = P.bufs(2)
    for c in range(8):
        i = c % 2
        P.dma("sp", wst[i][:], wo.rearrange("(kc p) n -> p kc n", p=128)[:, :, c * 256:(c + 1) * 256], writes=[b_wst[i]])
        if c % 2:
            P.op("act", lambda e, i=i, c=c: e.activation(out=wo_bf[:, :, c * 256:(c + 1) * 256], in_=wst[i][:], func=ACTF.Copy), reads=[b_wst[i]], writes=[b_wo])
        else:
            P.op("dve", lambda e, i=i, c=c: e.tensor_copy(out=wo_bf[:, :, c * 256:(c + 1) * 256], in_=wst[i][:]), reads=[b_wst[i]], writes=[b_wo])
    P.barrier()
    A.reset()
    g1 = A.alloc([128, D], F32)
    sh2 = A.alloc([128, D], F32)
    sc2 = A.alloc([128, D], F32)
    b_g1, b_sh2, b_sc2 = P.buf(), P.buf(), P.buf()
    ob = A.alloc([128, 16, 512], BF16)
    b_ob = P.buf()
    xt = A.alloc([128, D], F32)
    zt = A.alloc([128, D], F32)
    xm = A.alloc([128, D], F32)
    h2 = A.alloc([128, D], F32)
    h2T = A.alloc([128, 16, 128], F32)
    h2b = A.alloc([128, D], BF16)
    b_xt, b_zt, b_xm, b_h2, b_h2T, b_h2b = (P.buf() for _ in range(6))
    sm = A.alloc([128, 64], F32)
    b_sm = P.buf()
    ln = LN(P, "ln")
    outs = []

    def load_modset(m):
        P.dma("sp", g1[:], m[0:1, 2 * D:3 * D].partition_broadcast(128), writes=[b_g1])
        P.dma("sp", sh2[:], m[0:1, 3 * D:4 * D].partition_broadcast(128), writes=[b_sh2])
        P.dma("sp", sc2[:], m[0:1, 4 * D:5 * D].partition_broadcast(128), writes=[b_sc2])
        P.op("pool", lambda e: e.tensor_scalar(out=sc2[:], in0=sc2[:], scalar1=1.0, scalar2=None, op0=ALU.add), reads=[b_sc2], writes=[b_sc2])

    blocks = ([(0, nctx_tile)] if nctx_tile else []) + [(t, min(4, ntile - t)) for t in range(nctx_tile, ntile, 4)]
    for bi, (t0, nt) in enumerate(blocks):
        if bi == 0:
            load_modset(modc if nctx_tile else modl)
        elif bi == 1 and nctx_tile:
            load_modset(modl)
        w = nt * 128
        P.dma("sp", ob[:, :, 0:w], oT.rearrange("(kc p) n -> p kc n", p=128)[:, :, t0 * 128:t0 * 128 + w], writes=[b_ob])
        for tb in range(nt):
            t = t0 + tb
            rows = slice(t * 128, (t + 1) * 128)
            P.dma("sp", xt[:], xin[rows, :], writes=[b_xt])
            for cc in range(4):
                for kc in range(16):
                    P.op("pe", lambda e, cc=cc, kc=kc, tb=tb: e.matmul(pb[cc][:], lhsT=ob[:, kc, tb * 128:(tb + 1) * 128], rhs=wo_bf[:, kc, cc * 512:(cc + 1) * 512],
                                                                      start=(kc == 0), stop=(kc == 15)), reads=[b_ob, b_wo], writes=[b_pb[cc]])
                P.op("dve", lambda e, cc=cc: e.tensor_tensor(out=zt[:, cc * 512:(cc + 1) * 512], in0=pb[cc][:], in1=g1[:, cc * 512:(cc + 1) * 512], op=ALU.mult),
                     reads=[b_pb[cc], b_g1], writes=[b_zt])
            P.op("dve", lambda e: e.scalar_tensor_tensor(out=zt[:], in0=xt[:], scalar=ALPHA, in1=zt[:], op0=ALU.mult, op1=ALU.add),
                 reads=[b_xt, b_zt], writes=[b_zt])
            ln.norm(xm[:], zt[:], b_zt, b_xm)
            P.op("dve", lambda e: e.tensor_tensor(out=xm[:], in0=xm[:], in1=lng_s[:], op=ALU.mult), reads=[b_xm, b_lng], writes=[b_xm])
            P.op("pool", lambda e: e.tensor_tensor(out=xm[:], in0=xm[:], in1=lnb_s[:], op=ALU.add), reads=[b_xm, b_lnb], writes=[b_xm])
            outs.append(P.dma("sp", xmid[rows, :], xm[:], reads=[b_xm]))
            ln.norm(h2[:], xm[:], b_xm, b_h2)
            P.op("dve", lambda e: e.tensor_tensor(out=h2[:], in0=h2[:], in1=sc2[:], op=ALU.mult), reads=[b_h2, b_sc2], writes=[b_h2])
            P.op("pool", lambda e: e.tensor_tensor(out=h2[:], in0=h2[:], in1=sh2[:], op=ALU.add), reads=[b_h2, b_sh2], writes=[b_h2])
            P.op("act", lambda e: e.activation(out=h2b[:], in_=h2[:], func=ACTF.Copy), reads=[b_h2], writes=[b_h2b])
            outs.append(P.dma("sp", h2o[rows, :], h2b[:], reads=[b_h2b]))
            for qi in range(4):
                pq, bpq = pb[4 + qi % 3], b_pb[4 + qi % 3]
                for k in range(4):
                    kc = qi * 4 + k
                    P.op("pe", lambda e, pq=pq, k=k, kc=kc: e.transpose(out=pq[:, k * 128:(k + 1) * 128], in_=h2[:, kc * 128:(kc + 1) * 128], identity=ident[:]),
                         reads=[b_h2, b_ident], writes=[bpq])
                P.op("act", lambda e, pq=pq, qi=qi: e.activation(out=h2T[:, qi * 4:(qi + 1) * 4, :], in_=pq[:].rearrange("p (k t) -> p k t", k=4), func=ACTF.Copy),
                     reads=[bpq], writes=[b_h2T])
            for kc in range(16):
                P.op("pe", lambda e, kc=kc: e.matmul(pb[7][:, 0:16], lhsT=h2T[:, kc, :], rhs=wr_s[:, kc, :], start=(kc == 0), stop=(kc == 15)),
                     reads=[b_h2T, b_wr], writes=[b_pb[7]])
            P.op("dve", lambda e: e.tensor_reduce(out=sm[:, 16:17], in_=pb[7][:, 0:16], axis=AX.X, op=ALU.max), reads=[b_pb[7]], writes=[b_sm])
            P.op("dve", lambda e: e.tensor_scalar(out=sm[:, 17:18], in0=sm[:, 16:17], scalar1=-1.0, scalar2=None, op0=ALU.mult), reads=[b_sm], writes=[b_sm])
            P.op("act", lambda e: e.activation(out=sm[:, 0:16], in_=pb[7][:, 0:16], func=ACTF.Exp, bias=sm[:, 17:18], scale=1.0, accum_out=sm[:, 18:19]),
                 reads=[b_pb[7], b_sm], writes=[b_sm])
            P.op("dve", lambda e: e.reciprocal(out=sm[:, 18:19], in_=sm[:, 18:19]), reads=[b_sm], writes=[b_sm])
            P.op("dve", lambda e: e.tensor_scalar(out=sm[:, 32:48], in0=sm[:, 0:16], scalar1=sm[:, 18:19], scalar2=None, op0=ALU.mult), reads=[b_sm], writes=[b_sm])
            outs.append(P.dma("sp", affo[rows, :], sm[:, 32:48], reads=[b_sm]))
    st = P.emit(final_waits=outs)
    print("LC stats", st, flush=True)
    return nc, P


NE = 16
FF = 1024


def ld_consts():
    p = np.arange(128)
    gmat = (p[:, None] // 8 == p[None, :] // 8).astype(np.float32)
    sel8 = np.zeros((128, 16), np.float32)
    sel8[np.arange(16) * 8, np.arange(16)] = 1.0
    tri = (p[:, None] < p[None, :]).astype(np.float32)
    iota = np.broadcast_to(np.arange(128, dtype=np.float32)[None, :], (128, 128)).copy()
    return {"gmat": gmat, "sel8": sel8, "tri": tri, "ones": np.ones((128, 128), np.float32), "iota3": iota,
            "ident_bf": np.eye(128, dtype=np.float32).astype(BF)}


class MoeCommon:
    def __init__(self, nc, P, nseg_lat, kcap_lat, has_ctx, arena_words):
        self.nc, self.P = nc, P
        self.has_ctx = has_ctx
        self.affT_lat = din(nc, "affT_lat", [16, nseg_lat * 8])
        self.affT_ctx = din(nc, "affT_ctx", [16, 256]) if has_ctx else None
        cd = {k: din(nc, k, list(v.shape), BF16 if v.dtype == BF else F32) for k, v in ld_consts().items()}

        def cload(name, shape, dt):
            t = P.sb(name + "_s", shape, dt)
            b = P.buf(name)
            P.dma("sp", t[:], cd[name], writes=[b])
            return t, b

        self.gmat, self.b_gmat = cload("gmat", [128, 128], F32)
        self.sel8, self.b_sel8 = cload("sel8", [128, 16], F32)
        self.tri, self.b_tri = cload("tri", [128, 128], F32)
        self.ones, self.b_ones = cload("ones", [128, 128], F32)
        self.ident, self.b_ident = cload("ident_bf", [128, 128], BF16)
        self.iota, self.b_iota = cload("iota3", [128, 128], F32)
        self.pb = [P.ps(f"pb{i}", [128, 512], F32) for i in range(8)]
        self.b_pb = P.bufs(8, "pb", excl=True)
        self.A = Arena(P, "arena", arena_words)
        self.nseg_lat, self.kcap_lat = nseg_lat, kcap_lat

    def thresholds(self):
        P, A, pb, b_pb = self.P, self.A, self.pb, self.b_pb
        thr = P.sb("thr", [128, 2, 16], F32)
        b_thr = P.buf()
        sets = [(self.affT_lat, self.nseg_lat, float(self.kcap_lat), 0)]
        if self.has_ctx:
            sets.append((self.affT_ctx, 32, 32.0, 1))
        for si_, (src, nseg, kcap, which) in enumerate(sets):
            if si_:
                P.barrier()
            A.reset()
            at = A.alloc([128, nseg], F32)
            junk = A.alloc([128, nseg], F32)
            bs = A.alloc([128, 8], F32)
            b_at, b_bs = P.buf(), P.buf()
            P.dma("sp", at[:], src.rearrange("e (s n) -> (e s) n", s=8), writes=[b_at])
            P.op("dve", lambda e, bs=bs: e.memset(bs[:], 0.0), writes=[b_bs])
            for it in range(32):
                dk = 2.0 ** -(it + 1)
                P.op("dve", lambda e, bs=bs, dk=dk: e.tensor_scalar(out=bs[:, 1:2], in0=bs[:, 0:1], scalar1=dk, scalar2=None, op0=ALU.add), reads=[b_bs], writes=[b_bs])
                P.op("dve", lambda e, bs=bs, at=at, junk=junk: e.tensor_scalar(out=junk[:], in0=at[:], scalar1=bs[:, 1:2], scalar2=None, op0=ALU.is_ge, op1=ALU.add,
                                                                             accum_out=bs[:, 2:3]), reads=[b_bs, b_at], writes=[b_bs])
                P.op("dve", lambda e, bs=bs: e.tensor_copy(out=bs[:, 4:5], in_=bs[:, 2:3]), reads=[b_bs], writes=[b_bs])
                P.op("pe", lambda e, bs=bs: e.matmul(pb[0][:, 0:1], lhsT=self.gmat[:], rhs=bs[:, 4:5], start=True, stop=True), reads=[self.b_gmat, b_bs], writes=[b_pb[0]])
                P.op("dve", lambda e, bs=bs, dk=dk, kcap=kcap: e.tensor_scalar(out=bs[:, 3:4], in0=pb[0][:, 0:1], scalar1=kcap - 0.5, scalar2=dk, op0=ALU.is_ge, op1=ALU.mult),
                     reads=[b_pb[0]], writes=[b_bs])
                P.op("dve", lambda e, bs=bs: e.tensor_tensor(out=bs[:, 0:1], in0=bs[:, 0:1], in1=bs[:, 3:4], op=ALU.add), reads=[b_bs], writes=[b_bs])
            tsel = A.alloc([128, 16], F32)
            P.op("dve", lambda e, bs=bs, tsel=tsel: e.tensor_scalar(out=tsel[:], in0=self.sel8[:], scalar1=bs[:, 0:1], scalar2=None, op0=ALU.mult), reads=[b_bs, self.b_sel8], writes=[b_at])
            P.op("pe", lambda e, tsel=tsel: e.matmul(pb[1][:, 0:16], lhsT=self.ones[:], rhs=tsel[:], start=True, stop=True), reads=[self.b_ones, b_at], writes=[b_pb[1]])
            P.op("dve", lambda e, which=which: e.tensor_copy(out=thr[:, which, :], in_=pb[1][:, 0:16]), reads=[b_pb[1]], writes=[b_thr])
        P.barrier()
        A.reset()
        self.thr, self.b_thr = thr, b_thr

    def select(self, aff_src, ntile, groups, want_hl):
        P, pb, b_pb = self.P, self.pb, self.b_pb
        aff = self.A.alloc([128, ntile, 16], F32)
        b_aff = P.buf()
        P.dma("sp", aff[:], aff_src.rearrange("(t p) e -> p t e", p=128), writes=[b_aff])
        mask = self.A.alloc([128, ntile, 16], F32)
        posm = P.sb("posm_s", [128, ntile, 16], F32)
        b_mask, b_posm = P.buf(), P.buf()
        is_ctx_group = [self.has_ctx and gi == 0 for gi in range(len(groups))]
        for gi, (tiles, ns) in enumerate(groups):
            which = 1 if is_ctx_group[gi] else 0
            for k, t in enumerate(tiles):
                P.op("dve", lambda e, t=t, which=which: e.tensor_tensor(out=mask[:, t, :], in0=aff[:, t, :], in1=self.thr[:, which, :], op=ALU.is_ge),
                     reads=[b_aff, self.b_thr], writes=[b_mask])
                P.op("pe", lambda e, t=t, k=k: e.matmul(pb[2][:, 0:16], lhsT=self.tri[:], rhs=mask[:, t, :], start=True, stop=(k == 0)),
                     reads=[self.b_tri, b_mask], writes=[b_pb[2]])
                for k2 in range(k):
                    P.op("pe", lambda e, t2=tiles[k2], k2=k2, k=k: e.matmul(pb[2][:, 0:16], lhsT=self.ones[:], rhs=mask[:, t2, :], start=False, stop=(k2 == k - 1)),
                         reads=[self.b_ones, b_mask], writes=[b_pb[2]])
                P.op("dve", lambda e, t=t: e.scalar_tensor_tensor(out=posm[:, t, :], in0=pb[2][:, 0:16], scalar=1.0, in1=mask[:, t, :], op0=ALU.add, op1=ALU.mult),
                     reads=[b_pb[2], b_mask], writes=[b_posm])
        P.op("dve", lambda e: e.tensor_scalar(out=posm[:], in0=posm[:], scalar1=-1.0, scalar2=None, op0=ALU.add), reads=[b_posm], writes=[b_posm])
        self.posm, self.b_posm, self.is_ctx_group = posm, b_posm, is_ctx_group
        if want_hl:
            affhl = P.sb("affhl", [128, ntile, 16, 2], BF16)
            b_affhl = P.buf()
            P.op("dve", lambda e: e.tensor_copy(out=affhl[:, :, :, 0], in_=aff[:]), reads=[b_aff], writes=[b_affhl])
            P.op("dve", lambda e: e.tensor_tensor(out=mask[:], in0=aff[:], in1=affhl[:, :, :, 0], op=ALU.subtract), reads=[b_aff, b_affhl, b_posm], writes=[b_mask])
            P.op("dve", lambda e: e.tensor_copy(out=affhl[:, :, :, 1], in_=mask[:]), reads=[b_mask], writes=[b_affhl])
            self.affhl, self.b_affhl = affhl, b_affhl


def build_ld1(groups, chunks, nseg_lat, kcap_lat, has_ctx):
    nc = new_nc()
    ntile = sum(len(g[0]) for g in groups)
    NT = ntile * 128
    P = Prog(nc)
    M = MoeCommon(nc, P, nseg_lat, kcap_lat, has_ctx, 39 * 1024)
    aff_all = din(nc, "aff_all", [NT, 16])
    h2 = din(nc, "h2", [NT, D], BF16)
    wg = din(nc, "wg", [2, D, FF])
    wu = din(nc, "wu", [2, D, FF])
    wd = din(nc, "wd", [2, FF, D])
    soff = []
    o = 0
    for g in groups:
        soff.append(o)
        o += g[1]
    NSLOT = o
    Yo = dout(nc, "Yo", [2, NSLOT, D], BF16)
    M.thresholds()
    M.select(aff_all, ntile, groups, True)
    A, pb, b_pb = M.A, M.pb, M.b_pb
    posm, b_posm, affhl, b_affhl, iota, b_iota = M.posm, M.b_posm, M.affhl, M.b_affhl, M.iota, M.b_iota
    outs = []
    P.barrier()
    A.reset()
    Wg_bf = A.alloc([128, 16, FF], BF16)
    Wu_bf = A.alloc([128, 16, FF], BF16)
    Wd_bf = A.alloc([128, 8, D], BF16)
    b_W = P.buf("W")
    wst = [A.alloc([128, 16, 128], F32) for _ in range(1)]
    b_wst = P.bufs(1)
    h2g = [A.alloc([128, 4, D], BF16) for _ in range(1)]
    b_h2g = P.bufs(1)
    sel = [A.alloc([128, 4, 128], BF16) for _ in range(2)]
    b_sel = P.bufs(2)
    Xg = A.alloc([128, 16, 512], BF16)
    b_Xg = P.buf()
    hidT = A.alloc([128, 8, 512], BF16)
    b_hid = P.buf()
    wsl = A.alloc([128, 4, 2], F32)
    wsum = A.alloc([128, 4], F32)
    b_wsl = P.buf()
    sg = [A.alloc([128, 512], F32) for _ in range(2)]
    b_sg = P.bufs(2)
    yst = [A.alloc([128, 512], BF16) for _ in range(2)]
    b_yst = P.bufs(2)
    P.op("dve", lambda e: e.memset(wsl[:], 0.0), writes=[b_wsl])
    cast_eng = ["dve", "act", "pool"]
    kw = [0, 0, 0]

    def cast(dst, src_t, bsrc):
        ce = cast_eng[kw[0] % 3]
        kw[0] += 1
        if ce == "act":
            P.op("act", lambda e: e.activation(out=dst, in_=src_t, func=ACTF.Copy), reads=[bsrc], writes=[b_W])
        else:
            P.op(ce, lambda e: e.tensor_copy(out=dst, in_=src_t), reads=[bsrc], writes=[b_W])

    for e_ in range(2):
        for (wsrc, wdst) in ((wg, Wg_bf), (wu, Wu_bf)):
            for c in range(8):
                i = 0
                P.dma("sp", wst[i][:], wsrc[e_].rearrange("(kc p) f -> p kc f", p=128)[:, :, c * 128:(c + 1) * 128], writes=[b_wst[i]])
                cast(wdst[:, :, c * 128:(c + 1) * 128], wst[i][:], b_wst[i])
        for c in range(8):
            i = 0
            wv = wst[i][:].rearrange("p a b -> p (a b)").rearrange("p (a b) -> p a b", a=8)
            P.dma("sp", wv, wd[e_].rearrange("(fc p) n -> p fc n", p=128)[:, :, c * 256:(c + 1) * 256], writes=[b_wst[i]])
            cast(Wd_bf[:, :, c * 256:(c + 1) * 256], wv, b_wst[i])
        for ch in chunks:
            c0 = soff[ch[0]]
            cw = sum(groups[g][1] for g in ch)
            for li, gi in enumerate(ch):
                tiles, ns = groups[gi]
                hi = kw[2] % 2
                kw[2] += 1
                lo = soff[gi] - c0
                for k, t in enumerate(tiles):
                    P.dma("sp", h2g[0][:, k, :], h2[t * 128:(t + 1) * 128, :], writes=[b_h2g[0]])
                    P.op("dve", lambda e, hi=hi, k=k, t=t, e_=e_: e.tensor_scalar(out=sel[hi][:, k, :], in0=iota[:], scalar1=posm[:, t, e_:e_ + 1], scalar2=None, op0=ALU.is_equal),
                         reads=[b_posm, b_iota], writes=[b_sel[hi]])
                for fc in range(16):
                    pg, bpg = pb[fc % 2], b_pb[fc % 2]
                    for k, t in enumerate(tiles):
                        P.op("pe", lambda e, pg=pg, hi=hi, k=k, ns=ns, fc=fc, nt_=len(tiles): e.matmul(pg[:, 0:ns], lhsT=h2g[0][:, k, fc * 128:(fc + 1) * 128], rhs=sel[hi][:, k, 0:ns],
                                                                                                  start=(k == 0), stop=(k == nt_ - 1)), reads=[b_h2g[0], b_sel[hi]], writes=[bpg])
                    if fc % 2:
                        P.op("act", lambda e, pg=pg, ns=ns, fc=fc, lo=lo: e.activation(out=Xg[:, fc, lo:lo + ns], in_=pg[:, 0:ns], func=ACTF.Copy), reads=[bpg], writes=[b_Xg])
                    else:
                        P.op("dve", lambda e, pg=pg, ns=ns, fc=fc, lo=lo: e.tensor_copy(out=Xg[:, fc, lo:lo + ns], in_=pg[:, 0:ns]), reads=[bpg], writes=[b_Xg])
                for k, t in enumerate(tiles):
                    P.op("pe", lambda e, hi=hi, k=k, t=t, ns=ns, e_=e_, nt_=len(tiles): e.matmul(pb[6][0:ns, 0:2], lhsT=sel[hi][:, k, 0:ns], rhs=affhl[:, t, e_, :],
                                                                                            start=(k == 0), stop=(k == nt_ - 1)), reads=[b_sel[hi], b_affhl], writes=[b_pb[6]])
                P.op("dve", lambda e, li=li, ns=ns: e.tensor_copy(out=wsl[0:ns, li, :], in_=pb[6][0:ns, 0:2]), reads=[b_pb[6]], writes=[b_wsl])
            P.op("dve", lambda e: e.tensor_tensor(out=wsum[:], in0=wsl[:, :, 0], in1=wsl[:, :, 1], op=ALU.add), reads=[b_wsl], writes=[b_wsl])
            for fc in range(8):
                fl = fc % 2
                pgt, bpgt = pb[2 + fl], b_pb[2 + fl]
                pup, bpup = pb[4 + fl], b_pb[4 + fl]
                for (pp, bpp, W_) in ((pgt, bpgt, Wg_bf), (pup, bpup, Wu_bf)):
                    for kc in range(16):
                        P.op("pe", lambda e, pp=pp, W_=W_, kc=kc, fc=fc, cw=cw: e.matmul(pp[:, 0:cw], lhsT=W_[:, kc, fc * 128:(fc + 1) * 128], rhs=Xg[:, kc, 0:cw],
                                                                                     start=(kc == 0), stop=(kc == 15)), reads=[b_W, b_Xg], writes=[bpp])
                P.op("act", lambda e, pgt=pgt, fl=fl, cw=cw: e.activation(out=sg[fl][:, 0:cw], in_=pgt[:, 0:cw], func=ACTF.Silu), reads=[bpgt], writes=[b_sg[fl]])
                P.op("dve", lambda e, pup=pup, fl=fl, fc=fc, cw=cw: e.tensor_tensor(out=hidT[:, fc, 0:cw], in0=pup[:, 0:cw], in1=sg[fl][:, 0:cw], op=ALU.mult),
                     reads=[bpup, b_sg[fl]], writes=[b_hid])
            cnt = 0
            for cc in range(4):
                for li, gi in enumerate(ch):
                    tiles, ns = groups[gi]
                    lo = soff[gi] - c0
                    yi = cnt % 2
                    cnt += 1
                    py, bpy = pb[6 + yi], b_pb[6 + yi]
                    for fc in range(8):
                        P.op("pe", lambda e, py=py, ns=ns, fc=fc, lo=lo, cc=cc: e.matmul(py[0:ns, :], lhsT=hidT[:, fc, lo:lo + ns], rhs=Wd_bf[:, fc, cc * 512:(cc + 1) * 512],
                                                                                     start=(fc == 0), stop=(fc == 7)), reads=[b_hid, b_W], writes=[bpy])
                    P.op("act", lambda e, py=py, yi=yi, li=li, ns=ns: e.activation(out=yst[yi][0:ns, :], in_=py[0:ns, :], func=ACTF.Identity, bias=0.0, scale=wsum[0:ns, li:li + 1]),
                         reads=[bpy, b_wsl], writes=[b_yst[yi]])
                    outs.append(P.dma("sp", Yo[e_, soff[gi]:soff[gi] + ns, cc * 512:(cc + 1) * 512], yst[yi][0:ns, :], reads=[b_yst[yi]]))
    st = P.emit(final_waits=outs)
    print("LD1 stats", st, flush=True)
    return nc, P


def build_ld2(groups, nseg_lat, kcap_lat, has_ctx):
    nc = new_nc()
    ntile = sum(len(g[0]) for g in groups)
    NT = ntile * 128
    P = Prog(nc)
    M = MoeCommon(nc, P, nseg_lat, kcap_lat, has_ctx, 32 * 1024)
    aff_own = din(nc, "aff_own", [NT, 16])
    xmid = din(nc, "xmid", [NT, D])
    soff = []
    o = 0
    for g in groups:
        soff.append(o)
        o += g[1]
    NSLOT = o
    Yin = din(nc, "Yin", [NE, NSLOT, D], BF16)
    modl = din(nc, "modl", [1, 12288])
    modc = din(nc, "modc", [1, 12288])
    lng = din(nc, "lng", [1, D])
    lnb = din(nc, "lnb", [1, D])
    xout = dout(nc, "xout", [NT, D])
    M.thresholds()
    M.select(aff_own, ntile, groups, False)
    A, pb, b_pb = M.A, M.pb, M.b_pb
    posm, b_posm, iota, b_iota, ident, b_ident = M.posm, M.b_posm, M.iota, M.b_iota, M.ident, M.b_ident
    g2 = A.alloc([128, D], F32)
    b_g2 = P.buf()
    lng_s = A.alloc([128, D], F32)
    lnb_s = A.alloc([128, D], F32)
    b_ln = P.buf()
    P.dma("sp", lng_s[:], lng.partition_broadcast(128), writes=[b_ln])
    P.dma("sp", lnb_s[:], lnb.partition_broadcast(128), writes=[b_ln])
    Yq = A.alloc([128, NE, D], BF16)
    b_Yq = P.buf()
    sel3 = A.alloc([128, 16, 128], BF16)
    b_sel3 = P.buf()
    selT = A.alloc([128, 16, 128], BF16)
    b_selT = P.buf()
    xt = A.alloc([128, D], F32)
    zt = A.alloc([128, D], F32)
    xo = A.alloc([128, D], F32)
    b_xt, b_zt, b_xo = P.buf(), P.buf(), P.buf()
    ln = LN(P, "ln")
    pbf = [pb[4][:].bitcast(BF16), pb[5][:].bitcast(BF16)]
    outs = []
    for gi, (tiles, ns) in enumerate(groups):
        m = modc if M.is_ctx_group[gi] else modl
        if gi == 0 or (gi == 1 and M.is_ctx_group[0]):
            P.dma("sp", g2[:], m[0:1, 5 * D:6 * D].partition_broadcast(128), writes=[b_g2])
        P.dma("sp", Yq[0:ns], Yin[:, soff[gi]:soff[gi] + ns, :].rearrange("e s n -> s e n"), writes=[b_Yq])
        for t in tiles:
            rows = slice(t * 128, (t + 1) * 128)
            P.dma("sp", xt[:], xmid[rows, :], writes=[b_xt])
            posm_b = posm[:, t, :].unsqueeze(2).to_broadcast([128, 16, 128])
            P.op("dve", lambda e, posm_b=posm_b: e.tensor_tensor(out=sel3[:], in0=iota[:].unsqueeze(1).to_broadcast([128, 16, 128]), in1=posm_b, op=ALU.is_equal),
                 reads=[b_posm, b_iota], writes=[b_sel3])
            for half in range(2):
                for k in range(8):
                    e_ = half * 8 + k
                    P.op("pe", lambda e, half=half, k=k, e_=e_: e.transpose(out=pbf[half][:, k * 128:(k + 1) * 128], in_=sel3[:, e_, :], identity=ident[:]),
                         reads=[b_sel3, b_ident], writes=[b_pb[4 + half]])
                if half:
                    P.op("act", lambda e, half=half: e.activation(out=selT[:, half * 8:(half + 1) * 8, :], in_=pbf[half][:].rearrange("p (k n) -> p k n", k=8), func=ACTF.Copy),
                         reads=[b_pb[4 + half]], writes=[b_selT])
                else:
                    P.op("dve", lambda e, half=half: e.tensor_copy(out=selT[:, half * 8:(half + 1) * 8, :], in_=pbf[half][:].rearrange("p (k n) -> p k n", k=8)),
                         reads=[b_pb[4 + half]], writes=[b_selT])
            for cc in range(4):
                for e_ in range(NE):
                    P.op("pe", lambda e, cc=cc, e_=e_, ns=ns: e.matmul(pb[cc][:], lhsT=selT[0:ns, e_, :], rhs=Yq[0:ns, e_, cc * 512:(cc + 1) * 512], start=(e_ == 0), stop=(e_ == NE - 1)),
                         reads=[b_selT, b_Yq], writes=[b_pb[cc]])
                P.op("dve", lambda e, cc=cc: e.tensor_tensor(out=zt[:, cc * 512:(cc + 1) * 512], in0=pb[cc][:], in1=g2[:, cc * 512:(cc + 1) * 512], op=ALU.mult),
                     reads=[b_pb[cc], b_g2], writes=[b_zt])
            P.op("dve", lambda e: e.scalar_tensor_tensor(out=zt[:], in0=xt[:], scalar=ALPHA, in1=zt[:], op0=ALU.mult, op1=ALU.add), reads=[b_xt, b_zt], writes=[b_zt])
            ln.norm(xo[:], zt[:], b_zt, b_xo)
            P.op("dve", lambda e: e.tensor_tensor(out=xo[:], in0=xo[:], in1=lng_s[:], op=ALU.mult), reads=[b_xo, b_ln], writes=[b_xo])
            P.op("pool", lambda e: e.tensor_tensor(out=xo[:], in0=xo[:], in1=lnb_s[:], op=ALU.add), reads=[b_xo, b_ln], writes=[b_xo])
            outs.append(P.dma("sp", xout[rows, :], xo[:], reads=[b_xo]))
    st = P.emit(final_waits=outs)
    print("LD2 stats", st, flush=True)
    return nc, P


NCTX = 256


def le_consts():
    rot = np.zeros((128, 128), np.float32)
    for m in range(32):
        rot[m + 32, m] = -1.0
    for m in range(32, 64):
        rot[m - 32, m] = 1.0
    return {"rot64": rot, "ones": np.ones((128, 128), np.float32), "ones_bf": np.ones((128, 128), np.float32).astype(BF)}


def build_le(nblk=32, debug=False):
    nc = new_nc()
    NLAT = nblk * 512
    NTOK = NCTX + NLAT
    NT128 = NTOK // 128
    hT = din(nc, "hT", [D, NTOK], BF16)
    wdn = din(nc, "wdn", [D, 1088])
    gqk = din(nc, "gqk", [128, 8])
    wuq = din(nc, "wuq", [2, 512, 192])
    wukv = din(nc, "wukv", [2, 512, 256])
    cosT = din(nc, "cos64", [64, NLAT])
    sinT = din(nc, "sin64", [64, NLAT])
    c_rot = din(nc, "rot64", [128, 128])
    c_ones = din(nc, "ones", [128, 128])
    c_ones_bf = din(nc, "ones_bf", [128, 128], BF16)
    oT = dout(nc, "oT", [256, NLAT], BF16)
    dscr_ = dout if debug else dscr
    s_cq = dscr_(nc, "s_cq", [4, 128, NTOK], BF16)
    s_ckv = dscr_(nc, "s_ckv", [4, 128, NTOK], BF16)
    s_qn = dscr_(nc, "s_qn", [2, 128, NTOK], BF16)
    s_qr = dscr_(nc, "s_qr", [2, 64, NTOK], BF16)
    P = Prog(nc)
    blocks = [(0, NCTX)] + [(NCTX + 512 * i, 512) for i in range(nblk)]
    NBLK = len(blocks)
    db = {n: P.bufs(NBLK, n) for n in ("cq", "ckv", "qn0", "qn1", "qr0", "qr1")}

    def cload(name, src, shape, dt):
        t = P.sb(name, shape, dt)
        b = P.buf(name)
        P.dma("sp", t[:], src, writes=[b])
        return t, b

    rot, b_rot = cload("rot_s", c_rot, [128, 128], F32)
    ones, b_ones = cload("ones_s", c_ones, [128, 128], F32)
    ones_bf, b_onesbf = cload("onesbf_s", c_ones_bf, [128, 128], BF16)
    gq, b_gq = cload("gq_s", gqk, [128, 8], F32)
    KR = P.sb("KR", [64, NTOK], BF16)
    b_KR = P.bufs(NBLK, "KR")
    b_KN = P.bufs(NBLK, "KN")
    b_V = P.bufs(NBLK, "V")
    pb = [P.ps(f"pb{i}", [128, 512], F32) for i in range(8)]
    b_pb = P.bufs(8, "pb", excl=True)
    A = Arena(P, "arena", 31 * 1024)

    wdn_bf = A.alloc([128, 16, 1088], BF16)
    b_w = P.buf("w")
    wst = [A.alloc([128, 16, 136], F32) for _ in range(2)]
    b_wst = P.bufs(2)
    for c in range(8):
        i = c % 2
        P.dma("sp", wst[i][:], wdn.rearrange("(kc p) n -> p kc n", p=128)[:, :, c * 136:(c + 1) * 136], writes=[b_wst[i]])
        if c % 2:
            P.op("act", lambda e, i=i, c=c: e.activation(out=wdn_bf[:, :, c * 136:(c + 1) * 136], in_=wst[i][:], func=ACTF.Copy), reads=[b_wst[i]], writes=[b_w])
        else:
            P.op("dve", lambda e, i=i, c=c: e.tensor_copy(out=wdn_bf[:, :, c * 136:(c + 1) * 136], in_=wst[i][:]), reads=[b_wst[i]], writes=[b_w])
    hb = [A.alloc([128, 16, 512], BF16) for _ in range(2)]
    b_hb = P.bufs(2, "hb")
    cf = A.alloc([128, 4, 512], F32)
    b_cf = P.buf()
    sq = [A.alloc([128, 512], F32) for _ in range(2)]
    b_sq = P.bufs(2)
    rs = A.alloc([128, 512], F32)
    b_rs = P.buf()
    cn = [A.alloc([128, 4, 512], BF16) for _ in range(2)]
    b_cn = P.bufs(2)
    cs = [A.alloc([64, 2, 512], F32) for _ in range(2)]
    b_cs = P.bufs(2)
    t64 = [A.alloc([128, 512], F32) for _ in range(3)]
    b_t64 = P.bufs(3)
    P.op("dve", lambda e, t=t64[0]: e.memset(t[:], 0.0), writes=[b_t64[0]])
    kk = [0, 0]

    def rope64(src_ps, bsrc, dest, bdest, w, cs_t, bcs, tt, btt, lat):
        if not lat:
            P.op("act", lambda e: e.activation(out=dest, in_=src_ps, func=ACTF.Copy), reads=[bsrc], writes=[bdest])
            return
        a, ba = tt[0], btt[0]
        b_, bb = tt[1], btt[1]
        c_, bc = tt[2], btt[2]
        P.op("act", lambda e: e.activation(out=a[0:64, 0:w], in_=src_ps, func=ACTF.Copy), reads=[bsrc], writes=[ba])
        P.op("pe", lambda e: e.matmul(pb[7][:, 0:w], lhsT=rot[:], rhs=a[:, 0:w], start=True, stop=True), reads=[b_rot, ba], writes=[b_pb[7]])
        P.op("dve", lambda e: e.tensor_tensor(out=b_[0:64, 0:w], in0=pb[7][0:64, 0:w], in1=cs_t[:, 1, 0:w], op=ALU.mult), reads=[b_pb[7], bcs], writes=[bb])
        P.op("pool", lambda e: e.tensor_tensor(out=c_[0:64, 0:w], in0=a[0:64, 0:w], in1=cs_t[:, 0, 0:w], op=ALU.mult), reads=[ba, bcs], writes=[bc])
        P.op("dve", lambda e: e.tensor_tensor(out=dest, in0=b_[0:64, 0:w], in1=c_[0:64, 0:w], op=ALU.add), reads=[bb, bc], writes=[bdest])

    for bi, (s0, w) in enumerate(blocks):
        lat = bi > 0
        i = bi % 2
        P.dma("sp", hb[i][:, :, 0:w], hT.rearrange("(kc p) n -> p kc n", p=128)[:, :, s0:s0 + w], writes=[b_hb[i]])
        if lat:
            l0 = s0 - NCTX
            P.dma("sp", cs[i][:, 0, 0:w], cosT[:, l0:l0 + w], writes=[b_cs[i]])
            P.dma("sp", cs[i][:, 1, 0:w], sinT[:, l0:l0 + w], writes=[b_cs[i]])
        for which in ((0, 1) if lat else (1,)):
            for c in range(4):
                col = which * 512 + c * 128
                pf, bpf = pb[c % 3], b_pb[c % 3]
                for kc in range(16):
                    P.op("pe", lambda e, pf=pf, kc=kc, col=col, i=i, w=w: e.matmul(pf[:, 0:w], lhsT=wdn_bf[:, kc, col:col + 128], rhs=hb[i][:, kc, 0:w],
                                                                                start=(kc == 0), stop=(kc == 15)), reads=[b_w, b_hb[i]], writes=[bpf])
                P.op("act", lambda e, pf=pf, c=c, w=w: e.activation(out=cf[:, c, 0:w], in_=pf[:, 0:w], func=ACTF.Copy), reads=[bpf], writes=[b_cf])
                si = kk[0] % 2
                kk[0] += 1
                P.op("act", lambda e, pf=pf, si=si, w=w: e.activation(out=sq[si][:, 0:w], in_=pf[:, 0:w], func=ACTF.Square), reads=[bpf], writes=[b_sq[si]])
                P.op("pe", lambda e, si=si, c=c, w=w: e.matmul(pb[3][:, 0:w], lhsT=ones[:], rhs=sq[si][:, 0:w], start=(c == 0), stop=(c == 3)),
                     reads=[b_ones, b_sq[si]], writes=[b_pb[3]])
            P.op("act", lambda e, w=w: e.activation(out=rs[:, 0:w], in_=pb[3][:, 0:w], func=ACTF.Sqrt, bias=EPS, scale=1.0 / 512), reads=[b_pb[3]], writes=[b_rs])
            P.op("dve", lambda e, w=w: e.reciprocal(out=rs[:, 0:w], in_=rs[:, 0:w]), reads=[b_rs], writes=[b_rs])
            ci = kk[1] % 2
            kk[1] += 1
            for c in range(4):
                P.op("dve", lambda e, c=c, w=w, ci=ci, which=which: e.scalar_tensor_tensor(out=cn[ci][:, c, 0:w], in0=cf[:, c, 0:w], scalar=gq[:, which * 4 + c:which * 4 + c + 1],
                                                                                          in1=rs[:, 0:w], op0=ALU.mult, op1=ALU.mult), reads=[b_cf, b_rs, b_gq], writes=[b_cn[ci]])
            dst, dbuf = (s_cq, db["cq"]) if which == 0 else (s_ckv, db["ckv"])
            P.dma("sp", dst[:, :, s0:s0 + w].rearrange("c p n -> p c n"), cn[ci][:, :, 0:w], reads=[b_cn[ci]], writes=[dbuf[bi]])
        for kc in range(16):
            P.op("pe", lambda e, kc=kc, i=i, w=w: e.matmul(pb[4][0:64, 0:w], lhsT=wdn_bf[:, kc, 1024:1088], rhs=hb[i][:, kc, 0:w], start=(kc == 0), stop=(kc == 15)),
                 reads=[b_w, b_hb[i]], writes=[b_pb[4]])
        rope64(pb[4][0:64, 0:w], b_pb[4], KR[:, s0:s0 + w], b_KR[bi], w, cs[i], b_cs[i], t64, b_t64, lat)

    SCALE = 192 ** -0.5
    out_dmas = []
    P.barrier()
    A.reset()
    KN = A.alloc([128, NTOK], BF16)
    Vres = A.alloc([128, NT128, 128], BF16)
    A.mark()
    for hh in range(2):
        P.barrier()
        A.reset()
        wuq_bf = A.alloc([128, 4, 192], BF16)
        wukv_bf = A.alloc([128, 4, 256], BF16)
        wst2 = A.alloc([128, 4, 256], F32)
        b_w2, b_wst2 = P.buf(), P.buf()
        P.dma("sp", wst2[:, :, 0:192], wuq[hh].rearrange("(c p) n -> p c n", p=128), writes=[b_wst2])
        P.op("dve", lambda e: e.tensor_copy(out=wuq_bf[:], in_=wst2[:, :, 0:192]), reads=[b_wst2], writes=[b_w2])
        P.dma("sp", wst2[:], wukv[hh].rearrange("(c p) n -> p c n", p=128), writes=[b_wst2])
        P.op("dve", lambda e: e.tensor_copy(out=wukv_bf[:], in_=wst2[:]), reads=[b_wst2], writes=[b_w2])
        cqb = [A.alloc([128, 4, 512], BF16) for _ in range(2)]
        ckb = [A.alloc([128, 4, 512], BF16) for _ in range(2)]
        b_cqb, b_ckb = P.bufs(2), P.bufs(2)
        cs2 = [A.alloc([64, 2, 512], F32) for _ in range(2)]
        b_cs2 = P.bufs(2)
        qno = [A.alloc([128, 512], BF16) for _ in range(2)]
        qro = [A.alloc([64, 512], BF16) for _ in range(2)]
        b_qno, b_qro = P.bufs(2), P.bufs(2)
        t64b = [A.alloc([128, 512], F32) for _ in range(3)]
        b_t64b = P.bufs(3)
        P.op("dve", lambda e, t=t64b[0]: e.memset(t[:], 0.0), writes=[b_t64b[0]])
        for bi, (s0, w) in enumerate(blocks):
            lat = bi > 0
            i = bi % 2
            P.dma("sp", ckb[i][:, :, 0:w], s_ckv[:, :, s0:s0 + w].rearrange("c p n -> p c n"), reads=[db["ckv"][bi]], writes=[b_ckb[i]])
            if lat:
                l0 = s0 - NCTX
                P.dma("sp", cqb[i][:, :, 0:w], s_cq[:, :, s0:s0 + w].rearrange("c p n -> p c n"), reads=[db["cq"][bi]], writes=[b_cqb[i]])
                P.dma("sp", cs2[i][:, 0, 0:w], cosT[:, l0:l0 + w], writes=[b_cs2[i]])
                P.dma("sp", cs2[i][:, 1, 0:w], sinT[:, l0:l0 + w], writes=[b_cs2[i]])
                for c in range(4):
                    P.op("pe", lambda e, c=c, i=i, w=w, wuq_bf=wuq_bf, cqb=cqb: e.matmul(pb[0][:, 0:w], lhsT=wuq_bf[:, c, 0:128], rhs=cqb[i][:, c, 0:w], start=(c == 0), stop=(c == 3)),
                         reads=[b_w2, b_cqb[i]], writes=[b_pb[0]])
                P.op("act", lambda e, i=i, w=w: e.activation(out=qno[i][:, 0:w], in_=pb[0][:, 0:w], func=ACTF.Copy), reads=[b_pb[0]], writes=[b_qno[i]])
                P.dma("sp", s_qn[hh, :, s0:s0 + w], qno[i][:, 0:w], reads=[b_qno[i]], writes=[db[f"qn{hh}"][bi]])
                for c in range(4):
                    P.op("pe", lambda e, c=c, i=i, w=w: e.matmul(pb[1][0:64, 0:w], lhsT=wuq_bf[:, c, 128:192], rhs=cqb[i][:, c, 0:w], start=(c == 0), stop=(c == 3)),
                         reads=[b_w2, b_cqb[i]], writes=[b_pb[1]])
                rope64(pb[1][0:64, 0:w], b_pb[1], qro[i][:, 0:w], b_qro[i], w, cs2[i], b_cs2[i], t64b, b_t64b, True)
                P.dma("sp", s_qr[hh, :, s0:s0 + w], qro[i][:, 0:w], reads=[b_qro[i]], writes=[db[f"qr{hh}"][bi]])
            for c in range(4):
                P.op("pe", lambda e, c=c, i=i, w=w: e.matmul(pb[2][:, 0:w], lhsT=wukv_bf[:, c, 0:128], rhs=ckb[i][:, c, 0:w], start=(c == 0), stop=(c == 3)),
                     reads=[b_w2, b_ckb[i]], writes=[b_pb[2]])
            P.op("act", lambda e, s0=s0, w=w: e.activation(out=KN[:, s0:s0 + w], in_=pb[2][:, 0:w], func=ACTF.Copy), reads=[b_pb[2]], writes=[b_KN[bi]])
            for tt in range(w // 128):
                pt, bpt = pb[3 + tt % 2], b_pb[3 + tt % 2]
                for c in range(4):
                    P.op("pe", lambda e, pt=pt, c=c, i=i, tt=tt: e.matmul(pt[:, 0:128], lhsT=ckb[i][:, c, tt * 128:(tt + 1) * 128], rhs=wukv_bf[:, c, 128:256], start=(c == 0), stop=(c == 3)),
                         reads=[b_w2, b_ckb[i]], writes=[bpt])
                tok = s0 + tt * 128
                P.op("dve", lambda e, pt=pt, tok=tok: e.tensor_copy(out=Vres[:, tok // 128, :], in_=pt[:, 0:128]), reads=[bpt], writes=[b_V[bi]])
        P.barrier()
        A.reset()
        qn_s = [A.alloc([128, 512], BF16) for _ in range(2)]
        qr_s = [A.alloc([64, 512], BF16) for _ in range(2)]
        b_qns, b_qrs = P.bufs(2), P.bufs(2)
        NPT = 4
        pT_sb = [A.alloc([128, 512], BF16) for _ in range(NPT)]
        b_pTs = P.bufs(NPT)
        rz = [A.alloc([128, 512], F32) for _ in range(2)]
        b_rz = P.bufs(2)
        ob = [A.alloc([128, 512], BF16) for _ in range(2)]
        b_ob = P.bufs(2)
        accD = [A.alloc([128, 512], F32) for _ in range(2)]
        accP = [A.alloc([128, 512], F32) for _ in range(2)]
        b_accD, b_accP = P.bufs(2), P.bufs(2)
        kcount = 0
        for bi, (s0, w) in enumerate(blocks):
            if bi == 0:
                continue
            i = bi % 2
            P.dma("sp", qn_s[i][:, 0:w], s_qn[hh, :, s0:s0 + w], reads=[db[f"qn{hh}"][bi]], writes=[b_qns[i]])
            P.dma("sp", qr_s[i][:, 0:w], s_qr[hh, :, s0:s0 + w], reads=[db[f"qr{hh}"][bi]], writes=[b_qrs[i]])
            pO, bpO = pb[3 + i], b_pb[3 + i]
            pZ, bpZ = pb[5 + i], b_pb[5 + i]
            nkc = NT128
            for kc in range(nkc):
                kblk = 0 if kc < 2 else 1 + (kc * 128 - NCTX) // 512
                si = kcount % 3
                ti = kcount % NPT
                kcount += 1
                pS, bpS = pb[si], b_pb[si]
                P.op("pe", lambda e, pS=pS, kc=kc, i=i, w=w: e.matmul(pS[:, 0:w], lhsT=KN[:, kc * 128:(kc + 1) * 128], rhs=qn_s[i][:, 0:w], start=True, stop=False),
                     reads=[b_KN[kblk], b_qns[i]], writes=[bpS])
                P.op("pe", lambda e, pS=pS, kc=kc, i=i, w=w: e.matmul(pS[:, 0:w], lhsT=KR[:, kc * 128:(kc + 1) * 128], rhs=qr_s[i][:, 0:w], start=False, stop=True),
                     reads=[b_KR[kblk], b_qrs[i]], writes=[bpS])
                P.op("act", lambda e, pS=pS, ti=ti, w=w: e.activation(out=pT_sb[ti][:, 0:w], in_=pS[:, 0:w], func=ACTF.Exp, scale=SCALE), reads=[bpS], writes=[b_pTs[ti]])
                P.op("pe", lambda e, pO=pO, kc=kc, ti=ti, w=w, nkc=nkc: e.matmul(pO[:, 0:w], lhsT=Vres[:, kc, :], rhs=pT_sb[ti][:, 0:w], start=(kc == 0), stop=(kc == nkc - 1)),
                     reads=[b_V[kblk], b_pTs[ti]], writes=[bpO])
                eng = "dve" if kc % 2 == 0 else "pool"
                acc, bacc = (accD[i], b_accD[i]) if kc % 2 == 0 else (accP[i], b_accP[i])
                if kc < 2:
                    P.op(eng, lambda e, acc=acc, ti=ti, w=w: e.tensor_copy(out=acc[:, 0:w], in_=pT_sb[ti][:, 0:w]), reads=[b_pTs[ti]], writes=[bacc])
                else:
                    P.op(eng, lambda e, acc=acc, ti=ti, w=w: e.tensor_tensor(out=acc[:, 0:w], in0=acc[:, 0:w], in1=pT_sb[ti][:, 0:w], op=ALU.add), reads=[b_pTs[ti], bacc], writes=[bacc])
            P.op("pe", lambda e, pZ=pZ, i=i, w=w, accD=accD: e.matmul(pZ[:, 0:w], lhsT=ones[:], rhs=accD[i][:, 0:w], start=True, stop=False), reads=[b_ones, b_accD[i]], writes=[bpZ])
            P.op("pe", lambda e, pZ=pZ, i=i, w=w, accP=accP: e.matmul(pZ[:, 0:w], lhsT=ones[:], rhs=accP[i][:, 0:w], start=False, stop=True), reads=[b_ones, b_accP[i]], writes=[bpZ])
            P.op("dve", lambda e, i=i, pZ=pZ, w=w: e.reciprocal(out=rz[i][:, 0:w], in_=pZ[:, 0:w]), reads=[bpZ], writes=[b_rz[i]])
            P.op("dve", lambda e, i=i, pO=pO, w=w: e.tensor_tensor(out=ob[i][:, 0:w], in0=pO[:, 0:w], in1=rz[i][:, 0:w], op=ALU.mult), reads=[bpO, b_rz[i]], writes=[b_ob[i]])
            l0 = s0 - NCTX
            out_dmas.append(P.dma("sp", oT[hh * 128:(hh + 1) * 128, l0:l0 + w], ob[i][:, 0:w], reads=[b_ob[i]]))
    st = P.emit(final_waits=out_dmas)
    print("LE stats", st, flush=True)
    return nc, P


def le_inputs(inp, hT_all, nblk=32):
    NLAT = nblk * 512
    consts = le_consts()
    n_tok = 16384
    rows = n_tok // 64
    row = np.repeat(np.arange(rows, dtype=np.float32), 64)
    col = np.tile(np.arange(64, dtype=np.float32), rows)
    n_freq = 16
    inv = (10000.0 ** (-np.arange(n_freq, dtype=np.float32) / n_freq)).astype(np.float32)
    ang = np.concatenate([row[:, None] * inv, col[:, None] * inv], -1)
    cos = np.cos(ang).astype(np.float32)
    sin = np.sin(ang).astype(np.float32)
    cos64 = np.ascontiguousarray(np.concatenate([cos, cos], -1).T[:, :NLAT])
    sin64 = np.ascontiguousarray(np.concatenate([sin, sin], -1).T[:, :NLAT])
    hTs = np.ascontiguousarray(hT_all[:, :NCTX + NLAT])
    wdn = np.asarray(inp["mla_w_down"][0])
    gqk = np.ascontiguousarray(np.concatenate([np.asarray(inp["mla_q_norm_g"][0]).reshape(4, 128).T, np.asarray(inp["mla_kv_norm_g"][0]).reshape(4, 128).T], 1))
    wuq_all = np.asarray(inp["mla_w_uq"][0]).reshape(512, 16, 192)
    wukv_all = np.asarray(inp["mla_w_ukv"][0]).reshape(512, 16, 256)
    maps = []
    for j in range(NCORE):
        m = {"hT": hTs, "wdn": wdn, "gqk": gqk,
             "wuq": np.ascontiguousarray(wuq_all[:, 2 * j:2 * j + 2].transpose(1, 0, 2)),
             "wukv": np.ascontiguousarray(wukv_all[:, 2 * j:2 * j + 2].transpose(1, 0, 2)),
             "cos64": cos64, "sin64": sin64}
        m.update(consts)
        maps.append(m)
    return maps

import time

NLAT_CORE = 2048
NCTX = 256
VERBOSE = True


def _run(nc, P, maps, tag):
    t0 = time.time()
    res = run_bass_kernel_spmd(nc, maps, core_ids=list(range(NCORE)))
    if VERBOSE:
        print(f"[kernel] {tag}: {time.time() - t0:.1f}s dev_ns={getattr(res, 'exec_time_ns', None)}", flush=True)
    return res.results


def run_moe(run_fn, l, inp, modv, aff_list, h2_list, xmid_list, has_ctx):
    cD = ld_consts()
    o = NCTX if has_ctx else 0
    aff_lat = np.concatenate([np.asarray(a)[o:] for a in aff_list], 0)
    h2_lat = np.concatenate([np.asarray(h)[o:] for h in h2_list], 0)
    if has_ctx:
        aff_c = np.asarray(aff_list[0])[:NCTX]
        aff_all = np.concatenate([aff_c, aff_lat], 0)
        h2_all = np.concatenate([np.asarray(h2_list[0])[:NCTX], h2_lat], 0)
        groups_all = [([0, 1], 32)] + [([2 + 4 * q + k for k in range(4)], 128) for q in range(32)]
        chunks = [[0]] + [[1 + 4 * c + k for k in range(4)] for c in range(8)]
        groups_own = [([0, 1], 32)] + [([2 + 4 * q + k for k in range(4)], 128) for q in range(4)]
        cs = 32
    else:
        aff_all, h2_all = aff_lat, h2_lat
        groups_all = [([4 * q + k for k in range(4)], 128) for q in range(32)]
        chunks = [[4 * c + k for k in range(4)] for c in range(8)]
        groups_own = [([4 * q + k for k in range(4)], 128) for q in range(4)]
        cs = 0
    h2_all = np.ascontiguousarray(h2_all)
    nc, P = build_ld1(groups_all, chunks, 2048, 2048, has_ctx)
    maps = []
    for j in range(NCORE):
        perm = [2 * j, 2 * j + 1] + [e for e in range(16) if e not in (2 * j, 2 * j + 1)]
        m = {"affT_lat": np.ascontiguousarray(aff_lat[:, perm].T), "aff_all": np.ascontiguousarray(aff_all[:, perm]), "h2": h2_all,
             "wg": np.ascontiguousarray(inp["moe_w_gate"][l][2 * j:2 * j + 2]), "wu": np.ascontiguousarray(inp["moe_w_up"][l][2 * j:2 * j + 2]),
             "wd": np.ascontiguousarray(inp["moe_w_down"][l][2 * j:2 * j + 2])}
        if has_ctx:
            m["affT_ctx"] = np.ascontiguousarray(aff_c[:, perm].T)
        m.update(cD)
        maps.append(m)
    res1 = run_fn(nc, P, maps, f"LD1_{l}")
    Yo = [np.asarray(r["Yo"]) for r in res1]
    nc, P = build_ld2(groups_own, 2048, 2048, has_ctx)
    affT_lat = np.ascontiguousarray(aff_lat.T)
    maps = []
    for j in range(NCORE):
        lo = cs + 512 * j
        parts = []
        for e in range(16):
            y = Yo[e // 2][e % 2]
            parts.append(np.concatenate([y[:cs], y[lo:lo + 512]], 0)[None])
        m = {"affT_lat": affT_lat, "aff_own": np.asarray(aff_list[j]), "xmid": np.asarray(xmid_list[j]), "Yin": np.ascontiguousarray(np.concatenate(parts, 0)),
             "modl": np.ascontiguousarray(modv[l, 0][None]), "modc": np.ascontiguousarray(modv[l, 1][None]),
             "lng": inp["ln_g"][l, 1][None], "lnb": inp["ln_b"][l, 1][None]}
        if has_ctx:
            m["affT_ctx"] = np.ascontiguousarray(aff_c.T)
        m.update(cD)
        maps.append(m)
    res2 = run_fn(nc, P, maps, f"LD2_{l}")
    return [np.asarray(r["xout"]) for r in res2]


def _f32(a):
    return np.ascontiguousarray(np.asarray(a, dtype=np.float32))


def kernel(x, c, ctx, c_ctx, ada_w, ada_b, ln_g, ln_b, ev_w_in, ev_w_out, hgrn_lb, hgrn_norm_g,
           gqa_q_norm_g, gqa_k_norm_g, mla_w_down, mla_q_norm_g, mla_kv_norm_g, mla_w_uq, mla_w_ukv,
           mla_w_o, moe_router, moe_w_gate, moe_w_up, moe_w_down):
    inp = dict(x=x, c=c, ctx=ctx, c_ctx=c_ctx, ada_w=ada_w, ada_b=ada_b, ln_g=ln_g, ln_b=ln_b, ev_w_in=ev_w_in,
               ev_w_out=ev_w_out, hgrn_lb=hgrn_lb, hgrn_norm_g=hgrn_norm_g, gqa_q_norm_g=gqa_q_norm_g,
               gqa_k_norm_g=gqa_k_norm_g, mla_w_down=mla_w_down, mla_q_norm_g=mla_q_norm_g, mla_kv_norm_g=mla_kv_norm_g,
               mla_w_uq=mla_w_uq, mla_w_ukv=mla_w_ukv, mla_w_o=mla_w_o, moe_router=moe_router, moe_w_gate=moe_w_gate,
               moe_w_up=moe_w_up, moe_w_down=moe_w_down)
    inp = {k: _f32(v) for k, v in inp.items()}
    xs = inp["x"][0]
    ctx0 = inp["ctx"][0]
    ident = np.eye(128, dtype=np.float32)

    nc, P = build_l0()
    modv = l0_gather(_run(nc, P, l0_inputs(inp), "L0"))

    def modmaps(l):
        return {"modl": np.ascontiguousarray(modv[l, 0][None]), "modc": np.ascontiguousarray(modv[l, 1][None])}

    def run_la(l, x_lat, x_ctx):
        nc_la, P_la = build_la()
        maps = []
        for j in range(NCORE):
            m = {"xin": np.concatenate([x_ctx, x_lat[j * NLAT_CORE:(j + 1) * NLAT_CORE]], 0), "ident": ident}
            m.update(modmaps(l))
            maps.append(m)
        res = _run(nc_la, P_la, maps, f"LA{l}")
        return np.concatenate([np.asarray(res[0]["hT"])[:, :NCTX]] + [np.asarray(r["hT"])[:, NCTX:] for r in res], axis=1)

    def run_ld(l, res_c, has_ctx):
        return run_moe(_run, l, inp, modv, [r["aff"] for r in res_c], [r["h2"] for r in res_c], [r["xmid"] for r in res_c], has_ctx)

    hT_all = run_la(0, xs, ctx0)
    nc, P = build_lb()
    res_b = _run(nc, P, lb_inputs(inp, hT_all), "LB")
    oT_full = np.concatenate([np.asarray(r["aT"]) for r in res_b] + [np.asarray(r["bT"]) for r in res_b], axis=0)
    nc, P = build_lc(18, 2)
    maps = []
    for j in range(NCORE):
        lo = NCTX + j * NLAT_CORE
        m = {"oT": np.ascontiguousarray(np.concatenate([oT_full[:, :NCTX], oT_full[:, lo:lo + NLAT_CORE]], 1)), "wo": inp["ev_w_out"][0],
             "xin": np.concatenate([ctx0, xs[j * NLAT_CORE:(j + 1) * NLAT_CORE]], 0),
             "lng": inp["ln_g"][0, 0][None], "lnb": inp["ln_b"][0, 0][None], "wr": inp["moe_router"][0], "ident": ident}
        m.update(modmaps(0))
        maps.append(m)
    res_c = _run(nc, P, maps, "LC0")
    res_d = run_ld(0, res_c, True)
    x1 = np.concatenate([r[NCTX:] for r in res_d], 0)
    ctx1 = res_d[0][:NCTX]

    hT_all1 = run_la(1, x1, ctx1)
    nc, P = build_le()
    res_e = _run(nc, P, le_inputs(inp, hT_all1), "LE")
    oT1 = np.concatenate([np.asarray(r["oT"]) for r in res_e], axis=0)
    nc, P = build_lc(16, 0)
    maps = []
    for j in range(NCORE):
        m = {"oT": np.ascontiguousarray(oT1[:, j * NLAT_CORE:(j + 1) * NLAT_CORE]), "wo": inp["mla_w_o"][0],
             "xin": np.ascontiguousarray(x1[j * NLAT_CORE:(j + 1) * NLAT_CORE]),
             "lng": inp["ln_g"][1, 0][None], "lnb": inp["ln_b"][1, 0][None], "wr": inp["moe_router"][1], "ident": ident}
        m.update(modmaps(1))
        maps.append(m)
    res_c1 = _run(nc, P, maps, "LC1")
    res_d1 = run_ld(1, res_c1, False)
    out = np.concatenate(res_d1, 0)
    return out[None].astype(np.float32)
```

```python
import contextlib
import numpy as np
import concourse.bass as bass
import concourse.mybir as mybir

F32 = mybir.dt.float32
BF16 = mybir.dt.bfloat16
I32 = mybir.dt.int32
ALU = mybir.AluOpType
ACTF = mybir.ActivationFunctionType
AX = mybir.AxisListType

COMPUTE = ("pe", "act", "dve", "pool")
EPOCH = 4000
NSEM_ENG = 14
NSEM_DMA = 20


class Buf:
    __slots__ = ("name", "w", "rs", "excl")

    def __init__(self, name="", excl=False):
        self.name = name
        self.w = None
        self.rs = []
        self.excl = excl


class Op:
    __slots__ = ("eng", "idx", "fn", "waits", "is_dma", "dma_id", "sig", "clock", "q")

    def __init__(self, eng, idx, fn, is_dma=False):
        self.eng = eng
        self.idx = idx
        self.fn = fn
        self.waits = []
        self.is_dma = is_dma
        self.dma_id = None
        self.sig = None
        self.clock = None
        self.q = None


class Prog:
    def __init__(self, nc, same_engine_sync=True):
        self.nc = nc
        self.es = contextlib.ExitStack()
        self.streams = {e: [] for e in ("pe", "act", "dve", "pool", "sp")}
        self.known = {e: {c: -1 for c in COMPUTE} for e in self.streams}
        self.known_dma = {e: set() for e in self.streams}
        self.dma_count = {"sp": 0, "pool": 0, "act": 0}
        self.dma_ops = {"sp": [], "pool": [], "act": []}
        self.same_engine_sync = same_engine_sync
        self.nbuf = 0
        self.pending = {}

    def barrier(self):
        deps = []
        for f in COMPUTE:
            for o in reversed(self.streams[f]):
                if not o.is_dma:
                    deps.append(o)
                    break
        for q in self.dma_ops:
            deps.extend(self.dma_ops[q][-NSEM_DMA:])
        for e in self.streams:
            self.pending[e] = list(deps)

    def _pend(self, o):
        for d in self.pending.pop(o.eng, []):
            self._need(o, d)

    def sb(self, name, shape, dtype):
        t = self.es.enter_context(self.nc.sbuf_tensor(name, list(shape), dtype))
        return t

    def ps(self, name, shape, dtype=F32):
        t = self.es.enter_context(self.nc.psum_tensor(name, list(shape), dtype))
        return t

    def buf(self, name="", excl=False):
        self.nbuf += 1
        return Buf(name or f"b{self.nbuf}", excl)

    def bufs(self, n, name="", excl=False):
        return [self.buf(f"{name}{i}", excl) for i in range(n)]

    def _need(self, op, dep):
        if dep is None or dep is op:
            return
        e = op.eng
        if dep.is_dma:
            if dep.dma_id in self.known_dma[e]:
                return
            self.known_dma[e].add(dep.dma_id)
            op.waits.append(dep)
            for c, v in dep.clock.items():
                if v > self.known[e][c]:
                    self.known[e][c] = v
            return
        f = dep.eng
        if f == e and (f == "pe" or not self.same_engine_sync):
            return
        if self.known[e][f] >= dep.idx:
            return
        op.waits.append(dep)
        for c, v in dep.clock.items():
            if v > self.known[e][c]:
                self.known[e][c] = v
        if dep.idx > self.known[e][f]:
            self.known[e][f] = dep.idx

    def _track(self, op, reads, writes):
        ex = [r for r in reads if r.excl]
        if ex:
            reads = [r for r in reads if not r.excl]
            writes = list(writes) + [r for r in ex if r not in writes]
        for r in reads:
            self._need(op, r.w)
        for w in writes:
            self._need(op, w.w)
            for rd in w.rs:
                self._need(op, rd)
        for w in writes:
            w.w = op
            w.rs = []
        for r in reads:
            if r.w is not op:
                r.rs.append(op)

    def op(self, eng, fn, reads=(), writes=()):
        st = self.streams[eng]
        o = Op(eng, len(st), fn)
        self._pend(o)
        self._track(o, reads, writes)
        o.clock = dict(self.known[eng])
        if eng in COMPUTE:
            o.clock[eng] = o.idx
        st.append(o)
        return o

    def dma(self, q, out, in_, reads=(), writes=(), **kw):
        st = self.streams[q]
        o = Op(q, len(st), lambda e: e.dma_start(out=out, in_=in_, **kw), is_dma=True)
        o.q = q
        n = self.dma_count[q]
        o.dma_id = (q, n)
        self.dma_count[q] = n + 1
        if n >= NSEM_DMA:
            self._need(o, self.dma_ops[q][n - NSEM_DMA])
        self.dma_ops[q].append(o)
        self._pend(o)
        self._track(o, reads, writes)
        o.clock = dict(self.known[q])
        st.append(o)
        return o

    def emit(self, final_waits=()):
        nc = self.nc
        es = self.es
        needed = set()
        for e, st in self.streams.items():
            for o in st:
                for d in o.waits:
                    if not d.is_dma:
                        needed.add((d.eng, d.idx))
        nsig = {}
        for e in COMPUTE:
            k = 0
            for o in self.streams[e]:
                if (e, o.idx) in needed:
                    o.sig = k
                    k += 1
            nsig[e] = k
        sems = {}
        for e in COMPUTE:
            ne = max(1, (nsig[e] + EPOCH - 1) // EPOCH)
            assert ne <= NSEM_ENG, (e, nsig[e])
            sems[e] = [es.enter_context(nc.semaphore(f"s_{e}{i}")) for i in range(ne)]
        dsems = {}
        for q in ("sp", "pool", "act"):
            if self.dma_count[q]:
                dsems[q] = [es.enter_context(nc.semaphore(f"d_{q}{i}")) for i in range(NSEM_DMA)]
        engobj = {"pe": "tensor", "act": "scalar", "dve": "vector", "pool": "gpsimd", "sp": "sync"}

        def wait_for(eobj, d):
            if d.is_dma:
                q, n = d.dma_id
                eobj.wait_ge(dsems[q][n % NSEM_DMA], 16 * (n // NSEM_DMA + 1))
            else:
                eobj.wait_ge(sems[d.eng][d.sig // EPOCH], d.sig % EPOCH + 1)

        stats = {}
        with nc.Block() as block:
            for e in ("sp", "act", "pe", "dve", "pool"):
                st = self.streams[e]
                if not st and e != "sp":
                    continue

                def body(eobj, st=st, e=e):
                    nw = 0
                    for o in st:
                        for d in o.waits:
                            wait_for(eobj, d)
                            nw += 1
                        ins = o.fn(eobj)
                        if o.is_dma:
                            q, n = o.dma_id
                            ins.then_inc(dsems[q][n % NSEM_DMA], 16)
                        elif o.sig is not None:
                            ins.then_inc(sems[e][o.sig // EPOCH], 1)
                    if e == "sp":
                        for d in final_waits:
                            wait_for(eobj, d)
                    stats[e] = (len(st), nw)

                getattr(block, engobj[e])(body)
        self.stats = stats
        self.nsig = nsig
        print("signals", nsig, flush=True)
        return stats


class Arena:
    def __init__(self, P, name, nwords):
        self.t = P.sb(name, [128, nwords], F32)
        self.n = nwords
        self.off = 0
        self.floor = 0

    def mark(self):
        self.floor = self.off

    def reset(self):
        self.off = self.floor

    def alloc(self, shape, dtype, parts=128):
        per = 1
        for s_ in shape[1:]:
            per *= s_
        words = per if dtype in (F32, I32) else (per + 1) // 2
        words = (words + 7) // 8 * 8
        assert self.off + words <= self.n, ("arena overflow", self.off, words, self.n)
        v = self.t[0:shape[0], self.off:self.off + words]
        self.off += words
        if dtype not in (F32,):
            v = v.bitcast(dtype)
        v = v[:, 0:per]
        if len(shape) == 3:
            v = v.rearrange("p (a b) -> p a b", a=shape[1])
        elif len(shape) == 4:
            v = v.rearrange("p (a b c) -> p a b c", a=shape[1], b=shape[2])
        return v

import numpy as np
import ml_dtypes
from concourse.bass_utils import run_bass_kernel_spmd

D = 2048
EPS = 1e-6
NCORE = 8
ALPHA = (2.0 * 2) ** 0.25
BF = ml_dtypes.bfloat16


def new_nc():
    return bass.Bass("TRN2", target_bir_lowering=False)


def din(nc, name, shape, dt=F32):
    return nc.dram_tensor(name, list(shape), dt, kind="ExternalInput").ap()


def dout(nc, name, shape, dt=F32):
    return nc.dram_tensor(name, list(shape), dt, kind="ExternalOutput").ap()


def dscr(nc, name, shape, dt=F32):
    return nc.dram_tensor(name, list(shape), dt, kind="Internal").ap()


def run(nc, P, in_maps):
    with P.es:
        res = run_bass_kernel_spmd(nc, in_maps, core_ids=list(range(NCORE)))
    return res.results


class LN:
    def __init__(self, P, name, nb=2):
        self.P = P
        self.nb = nb
        self.st = [P.sb(f"{name}_st{i}", [128, 4, 6], F32) for i in range(nb)]
        self.mv = [P.sb(f"{name}_mv{i}", [128, 4], F32) for i in range(nb)]
        self.b = P.bufs(nb, f"{name}_st")
        self.k = 0

    def norm(self, out_ap, in_ap, b_in, b_out, rows=128):
        P = self.P
        i = self.k % self.nb
        self.k += 1
        st, mv, b = self.st[i], self.mv[i], self.b[i]
        for c in range(4):
            P.op("dve", lambda e, c=c: e.bn_stats(out=st[:rows, c, :], in_=in_ap[:, c * 512:(c + 1) * 512]),
                 reads=[b_in], writes=[b])
        P.op("dve", lambda e: e.bn_aggr(out=mv[:rows, 0:2], in_=st[:rows].rearrange("p a b -> p (a b)")), reads=[b], writes=[b])
        P.op("act", lambda e: e.activation(out=mv[:rows, 2:3], in_=mv[:rows, 1:2], func=ACTF.Sqrt, bias=EPS, scale=1.0),
             reads=[b], writes=[b])
        P.op("dve", lambda e: e.reciprocal(out=mv[:rows, 2:3], in_=mv[:rows, 2:3]), reads=[b], writes=[b])
        P.op("dve", lambda e: e.tensor_scalar(out=mv[:rows, 3:4], in0=mv[:rows, 0:1], scalar1=mv[:rows, 2:3], scalar2=-1.0,
                                              op0=ALU.mult, op1=ALU.mult), reads=[b], writes=[b])
        P.op("act", lambda e: e.activation(out=out_ap, in_=in_ap, func=ACTF.Identity, bias=mv[:rows, 3:4], scale=mv[:rows, 2:3]),
             reads=[b_in, b], writes=[b_out])


def load_bcast(P, name, src_row_ap, width, q="sp"):
    t = P.sb(name, [128, width], F32)
    b = P.buf(name)
    P.dma(q, t[:], src_row_ap.partition_broadcast(128), writes=[b])
    return t, b


def build_l0():
    nc = new_nc()
    CW = 12288 // NCORE
    cv = din(nc, "cv", [128, 16, 2])
    adaw = din(nc, "adaw", [2, D, CW])
    adab = din(nc, "adab", [2, CW])
    modv = dout(nc, "modv", [2, 2, CW])
    P = Prog(nc)
    cvs = P.sb("cvs", [128, 16, 2], F32)
    b_cv = P.buf()
    P.dma("sp", cvs[:], cv, writes=[b_cv])
    scv = P.sb("scv", [128, 16, 2], F32)
    P.op("act", lambda e: e.activation(out=scv[:], in_=cvs[:], func=ACTF.Silu), reads=[b_cv], writes=[b_cv])
    bias = P.sb("bias", [2, 2, CW], F32)
    b_bias = P.buf()
    for l in range(2):
        P.dma("sp", bias[:, l, :], adab[l:l + 1, :].partition_broadcast(2), writes=[b_bias])
    wt = [P.sb(f"wt{i}", [128, 16, 512], F32) for i in range(2)]
    b_wt = P.bufs(2)
    pm = [P.ps(f"pm{i}", [2, 512]) for i in range(2)]
    b_pm = P.bufs(2)
    res = P.sb("res", [2, 2, CW], F32)
    b_res = P.buf()
    k = 0
    for l in range(2):
        for cc in range(CW // 512):
            i = k % 2
            k += 1
            P.dma("sp", wt[i][:], adaw[l].rearrange("(kc p) n -> p kc n", p=128)[:, :, cc * 512:(cc + 1) * 512], writes=[b_wt[i]])
            for kc in range(16):
                P.op("pe", lambda e, i=i, kc=kc: e.matmul(pm[i][:], lhsT=scv[:, kc, :], rhs=wt[i][:, kc, :], start=(kc == 0), stop=(kc == 15)),
                     reads=[b_cv, b_wt[i]], writes=[b_pm[i]])
            P.op("dve", lambda e, i=i, l=l, cc=cc: e.tensor_tensor(out=res[:, l, cc * 512:(cc + 1) * 512], in0=pm[i][:],
                                                                  in1=bias[:, l, cc * 512:(cc + 1) * 512], op=ALU.add),
                 reads=[b_pm[i], b_bias], writes=[b_res])
    o = P.dma("sp", modv.rearrange("l r n -> r l n"), res[:], reads=[b_res])
    P.emit(final_waits=[o])
    return nc, P


def l0_inputs(inp):
    c = np.asarray(inp["c"], np.float32).reshape(D)
    cc = np.asarray(inp["c_ctx"], np.float32).reshape(D)
    cv = np.stack([c, cc], -1).reshape(16, 128, 2).transpose(1, 0, 2).copy()
    CW = 12288 // NCORE
    maps = []
    for j in range(NCORE):
        maps.append({"cv": cv, "adaw": np.ascontiguousarray(inp["ada_w"][:, :, j * CW:(j + 1) * CW]),
                     "adab": np.ascontiguousarray(inp["ada_b"][:, j * CW:(j + 1) * CW])})
    return maps


def l0_gather(results):
    return np.concatenate([np.asarray(r["modv"]) for r in results], axis=-1)


def load_mods(P, modl, modc, slots):
    out = {}
    for which, m in (("l", modl), ("c", modc)):
        for s in slots:
            t, b = load_bcast(P, f"mod_{which}{s}", m[0:1, s * D:(s + 1) * D], D)
            if s in (1, 4):
                P.op("pool", lambda e, t=t: e.tensor_scalar(out=t[:], in0=t[:], scalar1=1.0, scalar2=None, op0=ALU.add),
                     reads=[b], writes=[b])
            out[(s, which)] = (t, b)
    return out


def build_la(ntile=18, nctx_tile=2):
    nc = new_nc()
    NT = ntile * 128
    xin = din(nc, "xin", [NT, D])
    modl = din(nc, "modl", [1, 12288])
    modc = din(nc, "modc", [1, 12288])
    ident_d = din(nc, "ident", [128, 128])
    hT = dout(nc, "hT", [D, NT], BF16)
    P = Prog(nc)
    ident = P.sb("ident_s", [128, 128], F32)
    b_ident = P.buf("ident")
    P.dma("sp", ident[:], ident_d, writes=[b_ident])
    mods = load_mods(P, modl, modc, (0, 1))
    ln = LN(P, "ln")
    NB = 2
    xt = [P.sb(f"xt{i}", [128, D], F32) for i in range(NB)]
    b_xt = P.bufs(NB, "xt")
    ht = [P.sb(f"ht{i}", [128, D], F32) for i in range(NB)]
    b_ht = P.bufs(NB, "ht")
    pT = [P.ps(f"pT{i}", [128, 1024], F32) for i in range(2)]
    b_pT = P.bufs(2, "pT")
    hTs = [P.sb(f"hTs{i}", [128, 16, 512], BF16) for i in range(2)]
    b_hTs = P.bufs(2, "hTs")
    outs = []
    blocks = [(0, nctx_tile)] + [(t, min(4, ntile - t)) for t in range(nctx_tile, ntile, 4)]
    for bi, (t0, nt) in enumerate(blocks):
        hb = bi % 2
        for tb in range(nt):
            t = t0 + tb
            i = t % NB
            which = "c" if t < nctx_tile else "l"
            P.dma("sp", xt[i][:], xin[t * 128:(t + 1) * 128, :], writes=[b_xt[i]])
            ln.norm(ht[i][:], xt[i][:], b_xt[i], b_ht[i])
            sc, bsc = mods[(1, which)]
            sh, bsh = mods[(0, which)]
            P.op("dve", lambda e, i=i, sc=sc: e.tensor_tensor(out=ht[i][:], in0=ht[i][:], in1=sc[:], op=ALU.mult),
                 reads=[b_ht[i], bsc], writes=[b_ht[i]])
            P.op("pool", lambda e, i=i, sh=sh: e.tensor_tensor(out=ht[i][:], in0=ht[i][:], in1=sh[:], op=ALU.add),
                 reads=[b_ht[i], bsh], writes=[b_ht[i]])
            for half in range(2):
                for k in range(8):
                    kc = half * 8 + k
                    P.op("pe", lambda e, i=i, kc=kc, k=k, half=half: e.transpose(out=pT[half][:, k * 128:(k + 1) * 128],
                                                                                in_=ht[i][:, kc * 128:(kc + 1) * 128], identity=ident[:]),
                         reads=[b_ht[i], b_ident], writes=[b_pT[half]])
                P.op("act", lambda e, half=half, hb=hb, tb=tb: e.activation(
                    out=hTs[hb][:, half * 8:(half + 1) * 8, tb * 128:(tb + 1) * 128],
                    in_=pT[half][:].rearrange("p (k t) -> p k t", k=8), func=ACTF.Copy),
                    reads=[b_pT[half]], writes=[b_hTs[hb]])
        w = nt * 128
        o = P.dma("sp", hT.rearrange("(k p) n -> p k n", p=128)[:, :, t0 * 128:t0 * 128 + w], hTs[hb][:, :, 0:w],
                  reads=[b_hTs[hb]], writes=[])
        outs.append(o)
    P.emit(final_waits=outs)
    return nc, P


NCTX = 256


def lb_consts():
    ident_bf = np.eye(128, dtype=np.float32).astype(BF)
    rotT = np.zeros((128, 128), np.float32)
    for m in range(64):
        rotT[m + 64, m] = -1.0
    for m in range(64, 128):
        rotT[m - 64, m] = 1.0
    ones = np.ones((128, 128), np.float32)
    s = np.arange(64)[:, None]
    t = np.arange(64)[None, :]
    masks = np.stack([(s <= t), (s >= t)]).astype(np.float32)
    reset01 = np.ones((128, 512), np.float32)
    reset01[:, ::64] = 0.0
    return {"ident_bf": ident_bf, "rotT": rotT, "ones": ones, "ones_bf": ones.astype(BF), "masks": masks, "reset01": reset01}


def build_lb(nblk=32, upto=3, debug=False):
    nc = new_nc()
    NLAT = nblk * 512
    NTOK = NCTX + NLAT
    NT128 = NTOK // 128
    hT = din(nc, "hT", [D, NTOK], BF16)
    wfm = din(nc, "wfm", [6, D, 128])
    wtm = din(nc, "wtm", [D, 256])
    lbv = din(nc, "lbv", [128, 2, 3])
    gvec_d = din(nc, "gvec", [128, 3])
    cosT = din(nc, "cosT", [128, NLAT])
    sinT = din(nc, "sinT", [128, NLAT])
    c_ident = din(nc, "ident_bf", [128, 128], BF16)
    c_rotT = din(nc, "rotT", [128, 128])
    c_ones = din(nc, "ones", [128, 128])
    c_ones_bf = din(nc, "ones_bf", [128, 128], BF16)
    c_masks = din(nc, "masks", [2, 64, 64])
    c_reset = din(nc, "reset01", [128, 512])
    aT = dout(nc, "aT", [128, NTOK], BF16)
    bT = dout(nc, "bT", [128, NTOK], BF16)
    dscr_ = dout if debug else dscr
    s_q = dscr_(nc, "s_q", [128, NTOK], BF16)
    s_g = dscr_(nc, "s_g", [128, NTOK], BF16)
    s_lf = [dscr_(nc, f"s_lf{d}", [128, NTOK], F32) for d in range(2)]
    s_v = dscr_(nc, "s_v", [NTOK, 128], BF16)
    s_qb = dscr_(nc, "s_qb", [128, NTOK], BF16)
    s_of = dscr_(nc, "s_of", [128, NTOK], F32)
    P = Prog(nc)
    blocks = [(0, NCTX)] + [(NCTX + 512 * i, 512) for i in range(nblk)]
    NBLK = len(blocks)
    db = {n: P.bufs(NBLK, n) for n in ("q", "g", "lf0", "lf1", "v", "qb", "of")}

    def cload(name, src, shape, dt):
        t = P.sb(name, shape, dt)
        b = P.buf(name)
        P.dma("sp", t[:], src, writes=[b])
        return t, b

    ident, b_ident = cload("ident", c_ident, [128, 128], BF16)
    rotT, b_rot = cload("rotT_s", c_rotT, [128, 128], F32)
    ones, b_ones = cload("ones_s", c_ones, [128, 128], F32)
    ones_bf, b_onesbf = cload("onesbf_s", c_ones_bf, [128, 128], BF16)
    masks = P.sb("masks_s", [64, 2, 64], F32)
    b_masks = P.buf()
    P.dma("sp", masks[:], c_masks.rearrange("d s t -> s d t"), writes=[b_masks])
    reset01, b_reset = cload("reset_s", c_reset, [128, 512], F32)
    gvec, b_gvec = cload("gvec_s", gvec_d, [128, 3], F32)
    lbr = P.sb("lbr", [128, 2, 3], F32)
    b_lb = P.buf()
    P.dma("sp", lbr[:], lbv, writes=[b_lb])
    lbt = P.sb("lbt", [128, 8], F32)
    P.op("act", lambda e: e.activation(out=lbr[:], in_=lbr[:], func=ACTF.Exp), reads=[b_lb], writes=[b_lb])
    P.op("dve", lambda e: e.tensor_reduce(out=lbt[:, 0:2], in_=lbr[:], axis=AX.X, op=ALU.add), reads=[b_lb], writes=[b_lb])
    P.op("dve", lambda e: e.reciprocal(out=lbt[:, 0:2], in_=lbt[:, 0:2]), reads=[b_lb], writes=[b_lb])
    P.op("dve", lambda e: e.tensor_tensor(out=lbt[:, 2:4], in0=lbr[:, :, 0], in1=lbt[:, 0:2], op=ALU.mult), reads=[b_lb], writes=[b_lb])
    P.op("dve", lambda e: e.tensor_scalar(out=lbt[:, 4:6], in0=lbt[:, 2:4], scalar1=-1.0, scalar2=1.0, op0=ALU.mult, op1=ALU.add),
         reads=[b_lb], writes=[b_lb])

    KT = P.sb("KT", [128, NTOK], BF16)
    Vres = P.sb("Vres", [128, NT128, 128], BF16)
    b_KT = P.bufs(NBLK, "KT")
    b_V = P.bufs(NBLK, "V")
    pb = [P.ps(f"pb{i}", [128, 512], F32) for i in range(8)]
    b_pb = P.bufs(8, "pb", excl=True)
    pbf = [pb[6][:].bitcast(BF16), pb[7][:].bitcast(BF16)]
    b_pbf = [b_pb[6], b_pb[7]]
    A = Arena(P, "arena", 27 * 1024)

    wfm_bf = A.alloc([128, 6, 16, 128], BF16)
    wtm_bf = A.alloc([128, 16, 256], BF16)
    b_w = P.buf("w")
    wst = [A.alloc([128, 16, 128], F32) for _ in range(2)]
    b_wst = P.bufs(2)
    for idx in range(8):
        i = idx % 2
        if idx < 6:
            src = wfm[idx].rearrange("(kc p) n -> p kc n", p=128)
            dst = wfm_bf[:, idx]
        else:
            h = idx - 6
            src = wtm.rearrange("(kc p) n -> p kc n", p=128)[:, :, h * 128:(h + 1) * 128]
            dst = wtm_bf[:, :, h * 128:(h + 1) * 128]
        P.dma("sp", wst[i][:], src, writes=[b_wst[i]])
        if idx % 2:
            P.op("act", lambda e, i=i, dst=dst: e.activation(out=dst, in_=wst[i][:], func=ACTF.Copy), reads=[b_wst[i]], writes=[b_w])
        else:
            P.op("dve", lambda e, i=i, dst=dst: e.tensor_copy(out=dst, in_=wst[i][:]), reads=[b_wst[i]], writes=[b_w])
    hb = [A.alloc([128, 16, 512], BF16) for _ in range(2)]
    b_hb = P.bufs(2, "hb")
    NTMP = 2
    tmpA = [A.alloc([128, 512], F32) for _ in range(NTMP)]
    tmpB = [A.alloc([128, 512], F32) for _ in range(NTMP)]
    tmpC = [A.alloc([128, 512], F32) for _ in range(NTMP)]
    b_tA, b_tB, b_tC = P.bufs(NTMP), P.bufs(NTMP), P.bufs(NTMP)
    obf = [A.alloc([128, 512], BF16) for _ in range(4)]
    b_obf = P.bufs(4)
    cs = [A.alloc([128, 2, 512], F32) for _ in range(2)]
    b_cs = P.bufs(2)
    vo = [A.alloc([128, 128], BF16) for _ in range(2)]
    b_vo = P.bufs(2)
    kk = [0, 0, 0, 0]

    for bi, (s0, w) in enumerate(blocks):
        lat = bi > 0
        i = bi % 2
        P.dma("sp", hb[i][:, :, 0:w], hT.rearrange("(kc p) n -> p kc n", p=128)[:, :, s0:s0 + w], writes=[b_hb[i]])
        if lat:
            l0 = s0 - NCTX
            P.dma("sp", cs[i][:, 0, 0:w], cosT[:, l0:l0 + w], writes=[b_cs[i]])
            P.dma("sp", cs[i][:, 1, 0:w], sinT[:, l0:l0 + w], writes=[b_cs[i]])
        for idx in range(6):
            pf_i = kk[2] % 3
            kk[2] += 1
            pf, bpf = pb[pf_i], b_pb[pf_i]
            for kc in range(16):
                P.op("pe", lambda e, pf=pf, idx=idx, kc=kc, i=i, w=w: e.matmul(pf[:, 0:w], lhsT=wfm_bf[:, idx, kc, :], rhs=hb[i][:, kc, 0:w],
                                                                             start=(kc == 0), stop=(kc == 15)),
                     reads=[b_w, b_hb[i]], writes=[bpf])
            if idx == 0 or idx == 3:
                oi = kk[1] % 4
                kk[1] += 1
                fn = ACTF.Copy if idx == 0 else ACTF.Silu
                P.op("act", lambda e, oi=oi, pf=pf, w=w, fn=fn: e.activation(out=obf[oi][:, 0:w], in_=pf[:, 0:w], func=fn),
                     reads=[bpf], writes=[b_obf[oi]])
                dst, dbuf = (s_q, db["q"]) if idx == 0 else (s_g, db["g"])
                P.dma("sp", dst[:, s0:s0 + w], obf[oi][:, 0:w], reads=[b_obf[oi]], writes=[dbuf[bi]])
            elif idx in (1, 2):
                d = idx - 1
                ti = kk[0] % NTMP
                kk[0] += 1
                tA, bA = tmpA[ti], b_tA[ti]
                P.op("act", lambda e, tA=tA, pf=pf, w=w: e.activation(out=tA[:, 0:w], in_=pf[:, 0:w], func=ACTF.Sigmoid), reads=[bpf], writes=[bA])
                P.op("dve", lambda e, tA=tA, w=w, d=d: e.tensor_scalar(out=tA[:, 0:w], in0=tA[:, 0:w], scalar1=lbt[:, 4 + d:5 + d], scalar2=lbt[:, 2 + d:3 + d],
                                                                      op0=ALU.mult, op1=ALU.add), reads=[bA, b_lb], writes=[bA])
                P.op("act", lambda e, tA=tA, w=w: e.activation(out=tA[:, 0:w], in_=tA[:, 0:w], func=ACTF.Ln), reads=[bA], writes=[bA])
                P.dma("sp", s_lf[d][:, s0:s0 + w], tA[:, 0:w], reads=[bA], writes=[db[f"lf{d}"][bi]])
            else:
                ti = kk[0] % NTMP
                kk[0] += 1
                tA, bA, tB, bB, tC, bC = tmpA[ti], b_tA[ti], tmpB[ti], b_tB[ti], tmpC[ti], b_tC[ti]
                pn_i = 3 + kk[3] % 2
                kk[3] += 1
                pn, bpn = pb[pn_i], b_pb[pn_i]
                gcol = 1 if idx == 4 else 2
                P.op("act", lambda e, tA=tA, pf=pf, w=w: e.activation(out=tA[:, 0:w], in_=pf[:, 0:w], func=ACTF.Square), reads=[bpf], writes=[bA])
                P.op("pe", lambda e, pn=pn, tA=tA, w=w: e.matmul(pn[:, 0:w], lhsT=ones[:], rhs=tA[:, 0:w], start=True, stop=True),
                     reads=[b_ones, bA], writes=[bpn])
                P.op("act", lambda e, tB=tB, pn=pn, w=w: e.activation(out=tB[:, 0:w], in_=pn[:, 0:w], func=ACTF.Sqrt, bias=EPS, scale=1.0 / 128),
                     reads=[bpn], writes=[bB])
                P.op("dve", lambda e, tB=tB, w=w: e.reciprocal(out=tB[:, 0:w], in_=tB[:, 0:w]), reads=[bB], writes=[bB])
                P.op("dve", lambda e, tA=tA, tB=tB, pf=pf, w=w, gcol=gcol: e.scalar_tensor_tensor(out=tA[:, 0:w], in0=pf[:, 0:w], scalar=gvec[:, gcol:gcol + 1],
                                                                                                 in1=tB[:, 0:w], op0=ALU.mult, op1=ALU.mult),
                     reads=[bpf, bB, b_gvec], writes=[bA])
                if idx == 4:
                    oi = kk[1] % 4
                    kk[1] += 1
                    dest, bdest = obf[oi][:, 0:w], b_obf[oi]
                else:
                    dest, bdest = KT[:, s0:s0 + w], b_KT[bi]
                if lat:
                    pn2_i = 3 + kk[3] % 2
                    kk[3] += 1
                    pr, bpr = pb[pn2_i], b_pb[pn2_i]
                    P.op("pe", lambda e, pr=pr, tA=tA, w=w: e.matmul(pr[:, 0:w], lhsT=rotT[:], rhs=tA[:, 0:w], start=True, stop=True),
                         reads=[b_rot, bA], writes=[bpr])
                    P.op("dve", lambda e, tB=tB, pr=pr, w=w, i=i: e.tensor_tensor(out=tB[:, 0:w], in0=pr[:, 0:w], in1=cs[i][:, 1, 0:w], op=ALU.mult),
                         reads=[bpr, b_cs[i]], writes=[bB])
                    P.op("pool", lambda e, tC=tC, tA=tA, w=w, i=i: e.tensor_tensor(out=tC[:, 0:w], in0=tA[:, 0:w], in1=cs[i][:, 0, 0:w], op=ALU.mult),
                         reads=[bA, b_cs[i]], writes=[bC])
                    P.op("dve", lambda e, dest=dest, tB=tB, tC=tC, w=w: e.tensor_tensor(out=dest, in0=tB[:, 0:w], in1=tC[:, 0:w], op=ALU.add),
                         reads=[bB, bC], writes=[bdest])
                else:
                    P.op("act", lambda e, dest=dest, tA=tA, w=w: e.activation(out=dest, in_=tA[:, 0:w], func=ACTF.Copy), reads=[bA], writes=[bdest])
                if idx == 4:
                    P.dma("sp", s_qb[:, s0:s0 + w], dest, reads=[bdest], writes=[db["qb"][bi]])
        for tt in range(w // 128):
            pt_i = 5 + tt % 2
            pt, bpt = pb[pt_i], b_pb[pt_i]
            for kc in range(16):
                P.op("pe", lambda e, pt=pt, kc=kc, i=i, tt=tt: e.matmul(pt[:, 0:256], lhsT=hb[i][:, kc, tt * 128:(tt + 1) * 128], rhs=wtm_bf[:, kc, :],
                                                                       start=(kc == 0), stop=(kc == 15)),
                     reads=[b_w, b_hb[i]], writes=[bpt])
            vi = tt % 2
            P.op("act", lambda e, pt=pt, vi=vi: e.activation(out=vo[vi][:], in_=pt[:, 0:128], func=ACTF.Copy), reads=[bpt], writes=[b_vo[vi]])
            tok = s0 + tt * 128
            P.dma("sp", s_v[tok:tok + 128, :], vo[vi][:], reads=[b_vo[vi]], writes=[db["v"][bi]])
            P.op("dve", lambda e, pt=pt, tok=tok: e.tensor_copy(out=Vres[:, tok // 128, :], in_=pt[:, 128:256]), reads=[bpt], writes=[b_V[bi]])

    if upto == 1:
        kd_ = dout(nc, "KTo", [128, NTOK], BF16)
        vd_ = dout(nc, "Vo", [128, NT128, 128], BF16)
        o1 = P.dma("sp", kd_, KT[:], reads=b_KT)
        o2 = P.dma("sp", vd_, Vres[:], reads=b_V)
        P.barrier()
        o3 = P.dma("sp", kd_[:, 0:8], KT[:, 0:8])
        P.emit(final_waits=[o1, o2, o3])
        return nc, P
    P.barrier()
    A.reset()
    S_f = A.alloc([128, 128], F32)
    S_bf = [A.alloc([128, 128], BF16) for _ in range(2)]
    b_S = P.buf("S")
    b_Sbf = P.bufs(2, "Sbf")
    NS = 2
    qt = [A.alloc([128, 512], BF16) for _ in range(NS)]
    lf = [A.alloc([128, 512], F32) for _ in range(NS)]
    vch = [A.alloc([64, 8, 128], BF16) for _ in range(NS)]
    b_qt, b_lf, b_vch = P.bufs(NS), P.bufs(NS), P.bufs(NS)
    cum = [A.alloc([128, 512], F32) for _ in range(NS)]
    G = [A.alloc([128, 512], F32) for _ in range(NS)]
    Gr = [A.alloc([128, 512], F32) for _ in range(NS)]
    EE = [A.alloc([128, 512], F32) for _ in range(NS)]
    kkt = [A.alloc([128, 512], F32) for _ in range(NS)]
    tot = [A.alloc([128, 8], F32) for _ in range(NS)]
    etot = [A.alloc([128, 8], F32) for _ in range(NS)]
    qd = [A.alloc([128, 512], BF16) for _ in range(NS)]
    kd = [A.alloc([128, 512], BF16) for _ in range(NS)]
    qe = [A.alloc([128, 512], BF16) for _ in range(NS)]
    klT = [A.alloc([128, 512], BF16) for _ in range(NS)]
    b_el = P.bufs(NS, "el")
    b_qd, b_kd, b_qe, b_klT = P.bufs(NS), P.bufs(NS), P.bufs(NS), P.bufs(NS)
    kl_sb = [A.alloc([64, 128], BF16) for _ in range(2)]
    b_kl = P.bufs(2)
    sT_sb = [A.alloc([64, 64], BF16) for _ in range(2)]
    b_sT = P.bufs(2)
    ofl = [A.alloc([128, 512], F32) for _ in range(2)]
    b_ofl = P.bufs(2)
    gsl = [A.alloc([128, 512], BF16) for _ in range(2)]
    b_gsl = P.bufs(2)
    osb = [A.alloc([128, 512], F32) for _ in range(2)]
    b_osb = P.bufs(2)
    tsq = [A.alloc([128, 512], F32) for _ in range(2)]
    b_tsq = P.bufs(2)
    abf = [A.alloc([128, 512], BF16) for _ in range(2)]
    b_abf = P.bufs(2)
    out_dmas = []
    cnt = 0
    for d in range(2):
        P.op("dve", lambda e: e.memset(S_f[:], 0.0), writes=[b_S])
        P.op("dve", lambda e: e.memset(S_bf[0][:], 0.0), writes=[b_Sbf[0]])
        sbi = 0
        order = list(range(NBLK)) if d == 0 else [0] + list(range(NBLK - 1, 0, -1))
        for bi in order:
            s0, w = blocks[bi]
            nch = w // 64
            i = cnt % NS
            cnt += 1
            P.dma("sp", qt[i][:, 0:w], s_q[:, s0:s0 + w], reads=[db["q"][bi]], writes=[b_qt[i]])
            P.dma("sp", lf[i][:, 0:w], s_lf[d][:, s0:s0 + w], reads=[db[f"lf{d}"][bi]], writes=[b_lf[i]])
            P.dma("sp", vch[i][:, 0:nch, :], s_v[s0:s0 + w, :].rearrange("(n p) v -> p n v", p=64), reads=[db["v"][bi]], writes=[b_vch[i]])
            be = b_el[i]
            c3 = cum[i][:, 0:w].rearrange("p (n t) -> p n t", t=64)
            G3 = G[i][:, 0:w].rearrange("p (n t) -> p n t", t=64)
            P.op("dve", lambda e, i=i, w=w: e.tensor_tensor_scan(out=cum[i][:, 0:w], data0=reset01[:, 0:w], data1=lf[i][:, 0:w], initial=0.0,
                                                                op0=ALU.mult, op1=ALU.add), reads=[b_reset, b_lf[i]], writes=[be])
            P.op("dve", lambda e, i=i, c3=c3, nch=nch: e.tensor_copy(out=tot[i][:, 0:nch], in_=c3[:, :, 63]), reads=[be], writes=[be])
            tot_b = tot[i][:, 0:nch].unsqueeze(2).to_broadcast([128, nch, 64])
            if d == 0:
                P.op("pool", lambda e, i=i, w=w: e.tensor_copy(out=G[i][:, 0:w], in_=cum[i][:, 0:w]), reads=[be], writes=[be])
                ref = G3[:, :, 31:32]
            else:
                P.op("dve", lambda e, i=i, w=w: e.tensor_tensor(out=G[i][:, 0:w], in0=lf[i][:, 0:w], in1=cum[i][:, 0:w], op=ALU.subtract),
                     reads=[be, b_lf[i]], writes=[be])
                P.op("dve", lambda e, G3=G3, tot_b=tot_b: e.tensor_tensor(out=G3, in0=G3, in1=tot_b, op=ALU.add), reads=[be], writes=[be])
                ref = G3[:, :, 32:33]
            ref_b = ref.to_broadcast([128, nch, 64])
            Gr3 = Gr[i][:, 0:w].rearrange("p (n t) -> p n t", t=64)
            P.op("dve", lambda e, Gr3=Gr3, G3=G3, ref_b=ref_b: e.tensor_tensor(out=Gr3, in0=G3, in1=ref_b, op=ALU.subtract), reads=[be], writes=[be])
            P.op("act", lambda e, i=i, w=w: e.activation(out=kkt[i][:, 0:w], in_=lf[i][:, 0:w], func=ACTF.Exp), reads=[b_lf[i]], writes=[be])
            P.op("pool", lambda e, i=i, w=w: e.tensor_scalar(out=kkt[i][:, 0:w], in0=kkt[i][:, 0:w], scalar1=-1.0, scalar2=1.0, op0=ALU.mult, op1=ALU.add),
                 reads=[be], writes=[be])
            P.op("act", lambda e, i=i, w=w: e.activation(out=EE[i][:, 0:w], in_=Gr[i][:, 0:w], func=ACTF.Exp), reads=[be], writes=[be])
            P.op("dve", lambda e, i=i, w=w: e.tensor_tensor(out=qd[i][:, 0:w], in0=qt[i][:, 0:w], in1=EE[i][:, 0:w], op=ALU.mult),
                 reads=[be, b_qt[i]], writes=[b_qd[i]])
            P.op("act", lambda e, i=i, w=w: e.activation(out=EE[i][:, 0:w], in_=Gr[i][:, 0:w], func=ACTF.Exp, scale=-1.0), reads=[be], writes=[be])
            P.op("dve", lambda e, i=i, w=w: e.tensor_tensor(out=kd[i][:, 0:w], in0=kkt[i][:, 0:w], in1=EE[i][:, 0:w], op=ALU.mult),
                 reads=[be], writes=[b_kd[i]])
            P.op("act", lambda e, i=i, w=w: e.activation(out=EE[i][:, 0:w], in_=G[i][:, 0:w], func=ACTF.Exp), reads=[be], writes=[be])
            P.op("dve", lambda e, i=i, w=w: e.tensor_tensor(out=qe[i][:, 0:w], in0=qt[i][:, 0:w], in1=EE[i][:, 0:w], op=ALU.mult),
                 reads=[be, b_qt[i]], writes=[b_qe[i]])
            P.op("dve", lambda e, Gr3=Gr3, G3=G3, tot_b=tot_b: e.tensor_tensor(out=Gr3, in0=tot_b, in1=G3, op=ALU.subtract), reads=[be], writes=[be])
            P.op("act", lambda e, i=i, w=w: e.activation(out=EE[i][:, 0:w], in_=Gr[i][:, 0:w], func=ACTF.Exp), reads=[be], writes=[be])
            P.op("dve", lambda e, i=i, w=w: e.tensor_tensor(out=klT[i][:, 0:w], in0=kkt[i][:, 0:w], in1=EE[i][:, 0:w], op=ALU.mult),
                 reads=[be], writes=[b_klT[i]])
            P.op("act", lambda e, i=i, nch=nch: e.activation(out=etot[i][:, 0:nch], in_=tot[i][:, 0:nch], func=ACTF.Exp), reads=[be], writes=[be])
            po_i = cnt % 2
            po, bpo = pb[po_i], b_pb[po_i]
            chunks = list(range(nch)) if d == 0 else list(range(nch - 1, -1, -1))
            for ci, n in enumerate(chunks):
                c0, c1 = n * 64, (n + 1) * 64
                j = ci % 2
                P.op("pe", lambda e, j=j, i=i, c0=c0, c1=c1: e.transpose(out=pbf[j][0:64, 0:128], in_=klT[i][:, c0:c1], identity=ident[:]),
                     reads=[b_klT[i], b_ident], writes=[b_pbf[j]])
                P.op("act", lambda e, j=j: e.activation(out=kl_sb[j][:], in_=pbf[j][0:64, 0:128], func=ACTF.Copy), reads=[b_pbf[j]], writes=[b_kl[j]])
                ps_s, bps = pb[2 + j], b_pb[2 + j]
                P.op("pe", lambda e, ps_s=ps_s, i=i, c0=c0, c1=c1: e.matmul(ps_s[0:64, 0:64], lhsT=kd[i][:, c0:c1], rhs=qd[i][:, c0:c1], start=True, stop=True),
                     reads=[b_kd[i], b_qd[i]], writes=[bps])
                P.op("dve", lambda e, ps_s=ps_s, j=j, d=d: e.tensor_tensor(out=sT_sb[j][:], in0=ps_s[0:64, 0:64], in1=masks[:, d, :], op=ALU.mult),
                     reads=[bps, b_masks], writes=[b_sT[j]])
                P.op("pe", lambda e, po=po, i=i, n=n, j=j, c0=c0, c1=c1: e.matmul(po[:, c0:c1], lhsT=vch[i][:, n, :], rhs=sT_sb[j][:], start=True, stop=False),
                     reads=[b_vch[i], b_sT[j]], writes=[bpo])
                P.op("pe", lambda e, po=po, i=i, sbi=sbi, c0=c0, c1=c1: e.matmul(po[:, c0:c1], lhsT=S_bf[sbi][:], rhs=qe[i][:, c0:c1], start=False, stop=True),
                     reads=[b_Sbf[sbi], b_qe[i]], writes=[bpo])
                pst, bpst = pb[4], b_pb[4]
                P.op("pe", lambda e, pst=pst, j=j, i=i, n=n: e.matmul(pst[:, 0:128], lhsT=kl_sb[j][:], rhs=vch[i][:, n, :], start=True, stop=True),
                     reads=[b_kl[j], b_vch[i]], writes=[bpst])
                P.op("dve", lambda e, pst=pst, i=i, n=n: e.scalar_tensor_tensor(out=S_f[:], in0=S_f[:], scalar=etot[i][:, n:n + 1], in1=pst[:, 0:128],
                                                                               op0=ALU.mult, op1=ALU.add), reads=[b_S, be, bpst], writes=[b_S])
                sbi = 1 - sbi
                P.op("act", lambda e, sbi=sbi: e.activation(out=S_bf[sbi][:], in_=S_f[:], func=ACTF.Copy), reads=[b_S], writes=[b_Sbf[sbi]])
            if d == 0:
                oi = cnt % 2
                P.op("act", lambda e, oi=oi, po=po, w=w: e.activation(out=osb[oi][:, 0:w], in_=po[:, 0:w], func=ACTF.Copy), reads=[bpo], writes=[b_osb[oi]])
                P.dma("sp", s_of[:, s0:s0 + w], osb[oi][:, 0:w], reads=[b_osb[oi]], writes=[db["of"][bi]])
            else:
                oi = cnt % 2
                P.dma("sp", ofl[oi][:, 0:w], s_of[:, s0:s0 + w], reads=[db["of"][bi]], writes=[b_ofl[oi]])
                P.dma("sp", gsl[oi][:, 0:w], s_g[:, s0:s0 + w], reads=[db["g"][bi]], writes=[b_gsl[oi]])
                P.op("dve", lambda e, oi=oi, po=po, w=w: e.tensor_tensor(out=osb[oi][:, 0:w], in0=po[:, 0:w], in1=ofl[oi][:, 0:w], op=ALU.add),
                     reads=[bpo, b_ofl[oi]], writes=[b_osb[oi]])
                P.op("act", lambda e, oi=oi, w=w: e.activation(out=tsq[oi][:, 0:w], in_=osb[oi][:, 0:w], func=ACTF.Square), reads=[b_osb[oi]], writes=[b_tsq[oi]])
                pn, bpn = pb[5], b_pb[5]
                P.op("pe", lambda e, pn=pn, oi=oi, w=w: e.matmul(pn[:, 0:w], lhsT=ones[:], rhs=tsq[oi][:, 0:w], start=True, stop=True),
                     reads=[b_ones, b_tsq[oi]], writes=[bpn])
                P.op("act", lambda e, pn=pn, oi=oi, w=w: e.activation(out=tsq[oi][:, 0:w], in_=pn[:, 0:w], func=ACTF.Sqrt, bias=EPS, scale=1.0 / 128),
                     reads=[bpn], writes=[b_tsq[oi]])
                P.op("dve", lambda e, oi=oi, w=w: e.reciprocal(out=tsq[oi][:, 0:w], in_=tsq[oi][:, 0:w]), reads=[b_tsq[oi]], writes=[b_tsq[oi]])
                P.op("dve", lambda e, oi=oi, w=w: e.tensor_tensor(out=osb[oi][:, 0:w], in0=osb[oi][:, 0:w], in1=tsq[oi][:, 0:w], op=ALU.mult),
                     reads=[b_osb[oi], b_tsq[oi]], writes=[b_osb[oi]])
                P.op("dve", lambda e, oi=oi, w=w: e.scalar_tensor_tensor(out=abf[oi][:, 0:w], in0=osb[oi][:, 0:w], scalar=gvec[:, 0:1], in1=gsl[oi][:, 0:w],
                                                                        op0=ALU.mult, op1=ALU.mult), reads=[b_osb[oi], b_gsl[oi], b_gvec], writes=[b_abf[oi]])
                out_dmas.append(P.dma("sp", aT[:, s0:s0 + w], abf[oi][:, 0:w], reads=[b_abf[oi]]))

    if upto == 2:
        P.barrier()
        o3 = P.dma("sp", bT[:, 0:8], KT[:, 0:8])
        P.emit(final_waits=out_dmas + [o3])
        return nc, P
    P.barrier()
    A.reset()
    SCALE = 128 ** -0.5
    qb_s = [A.alloc([128, 512], BF16) for _ in range(2)]
    b_qbs = P.bufs(2)
    NPT = 4
    pT_sb = [A.alloc([128, 512], BF16) for _ in range(NPT)]
    b_pTs = P.bufs(NPT)
    rz = [A.alloc([128, 512], F32) for _ in range(2)]
    b_rz = P.bufs(2)
    ob = [A.alloc([128, 512], BF16) for _ in range(2)]
    b_ob = P.bufs(2)
    accD = [A.alloc([128, 512], F32) for _ in range(2)]
    accP = [A.alloc([128, 512], F32) for _ in range(2)]
    b_accD, b_accP = P.bufs(2), P.bufs(2)
    allKT = b_KT
    allV = b_V
    kcount = 0
    for bi, (s0, w) in enumerate(blocks):
        i = bi % 2
        P.dma("sp", qb_s[i][:, 0:w], s_qb[:, s0:s0 + w], reads=[db["qb"][bi]], writes=[b_qbs[i]])
        nkc = (NCTX // 128) if bi == 0 else NT128
        pO, bpO = pb[3 + i], b_pb[3 + i]
        pZ, bpZ = pb[5 + i], b_pb[5 + i]
        LOOK = 2
        slots = {}
        for step in range(nkc + LOOK):
            if step < nkc:
                kc = step
                kblk = 0 if kc < 2 else 1 + (kc * 128 - NCTX) // 512
                si = kcount % 3
                ti = kcount % NPT
                kcount += 1
                slots[kc] = ti
                pS, bpS = pb[si], b_pb[si]
                P.op("pe", lambda e, pS=pS, kc=kc, i=i, w=w: e.matmul(pS[:, 0:w], lhsT=KT[:, kc * 128:(kc + 1) * 128], rhs=qb_s[i][:, 0:w], start=True, stop=True),
                     reads=[allKT[kblk], b_qbs[i]], writes=[bpS])
                P.op("act", lambda e, pS=pS, ti=ti, w=w: e.activation(out=pT_sb[ti][:, 0:w], in_=pS[:, 0:w], func=ACTF.Exp, scale=SCALE),
                     reads=[bpS], writes=[b_pTs[ti]])
            if step >= LOOK:
                kc = step - LOOK
                kblk = 0 if kc < 2 else 1 + (kc * 128 - NCTX) // 512
                ti = slots[kc]
                P.op("pe", lambda e, pO=pO, kc=kc, ti=ti, w=w, nkc=nkc: e.matmul(pO[:, 0:w], lhsT=Vres[:, kc, :], rhs=pT_sb[ti][:, 0:w], start=(kc == 0), stop=(kc == nkc - 1)),
                     reads=[allV[kblk], b_pTs[ti]], writes=[bpO])
                eng = "dve"
                acc, bacc = accD[i], b_accD[i]
                if kc < 1:
                    P.op(eng, lambda e, acc=acc, ti=ti, w=w: e.tensor_copy(out=acc[:, 0:w], in_=pT_sb[ti][:, 0:w]), reads=[b_pTs[ti]], writes=[bacc])
                else:
                    P.op(eng, lambda e, acc=acc, ti=ti, w=w: e.tensor_tensor(out=acc[:, 0:w], in0=acc[:, 0:w], in1=pT_sb[ti][:, 0:w], op=ALU.add), reads=[b_pTs[ti], bacc], writes=[bacc])
        P.op("pe", lambda e, pZ=pZ, i=i, w=w: e.matmul(pZ[:, 0:w], lhsT=ones[:], rhs=accD[i][:, 0:w], start=True, stop=True), reads=[b_ones, b_accD[i]], writes=[bpZ])
        P.op("dve", lambda e, i=i, pZ=pZ, w=w: e.reciprocal(out=rz[i][:, 0:w], in_=pZ[:, 0:w]), reads=[bpZ], writes=[b_rz[i]])
        P.op("dve", lambda e, i=i, pO=pO, w=w: e.tensor_tensor(out=ob[i][:, 0:w], in0=pO[:, 0:w], in1=rz[i][:, 0:w], op=ALU.mult),
             reads=[bpO, b_rz[i]], writes=[b_ob[i]])
        out_dmas.append(P.dma("sp", bT[:, s0:s0 + w], ob[i][:, 0:w], reads=[b_ob[i]]))
    st = P.emit(final_waits=out_dmas)
    print("LB stats", st, flush=True)
    return nc, P


def lb_inputs(inp, hT_all, nblk=32):
    NLAT = nblk * 512
    w_in = np.asarray(inp["ev_w_in"][0])
    consts = lb_consts()
    n_tok = 16384
    rows = n_tok // 64
    row = np.repeat(np.arange(rows, dtype=np.float32), 64)
    col = np.tile(np.arange(64, dtype=np.float32), rows)
    n_freq = 32
    inv = (10000.0 ** (-np.arange(n_freq, dtype=np.float32) / n_freq)).astype(np.float32)
    ang = np.concatenate([row[:, None] * inv, col[:, None] * inv], -1)
    cos = np.cos(ang).astype(np.float32)
    sin = np.sin(ang).astype(np.float32)
    cosT = np.ascontiguousarray(np.concatenate([cos, cos], -1).T[:, :NLAT])
    sinT = np.ascontiguousarray(np.concatenate([sin, sin], -1).T[:, :NLAT])
    hTs = np.ascontiguousarray(hT_all[:, :NCTX + NLAT])
    maps = []
    for j in range(NCORE):
        c = lambda base, wd=128: w_in[:, base + j * wd: base + (j + 1) * wd]
        kvh = j // 4
        wq, wff, wfb, wi, wg = c(0), c(1024), c(2048), c(3072), c(4096)
        wqb = c(5120)
        wkb = w_in[:, 6144 + kvh * 128: 6144 + (kvh + 1) * 128]
        wvb = w_in[:, 6400 + kvh * 128: 6400 + (kvh + 1) * 128]
        wfm = np.ascontiguousarray(np.stack([wq, wff, wfb, wg, wqb, wkb]))
        wtm = np.ascontiguousarray(np.concatenate([wi, wvb], 1))
        lbv = np.ascontiguousarray(np.asarray(inp["hgrn_lb"])[:, :, j * 128:(j + 1) * 128].transpose(2, 0, 1))
        gvec = np.ascontiguousarray(np.stack([inp["hgrn_norm_g"][0], inp["gqa_q_norm_g"][0], inp["gqa_k_norm_g"][0]], -1))
        m = {"hT": hTs, "wfm": wfm, "wtm": wtm, "lbv": lbv, "gvec": gvec, "cosT": cosT, "sinT": sinT}
        m.update(consts)
        maps.append(m)
    return maps


def build_lc(ntile=18, nctx_tile=2):
    nc = new_nc()
    NT = ntile * 128
    oT = din(nc, "oT", [D, NT], BF16)
    wo = din(nc, "wo", [D, D])
    xin = din(nc, "xin", [NT, D])
    modl = din(nc, "modl", [1, 12288])
    modc = din(nc, "modc", [1, 12288])
    lng = din(nc, "lng", [1, D])
    lnb = din(nc, "lnb", [1, D])
    wr = din(nc, "wr", [D, 16])
    ident_d = din(nc, "ident", [128, 128])
    xmid = dout(nc, "xmid", [NT, D])
    h2o = dout(nc, "h2", [NT, D], BF16)
    affo = dout(nc, "aff", [NT, 16])
    P = Prog(nc)
    ident = P.sb("ident_s", [128, 128], F32)
    b_ident = P.buf()
    P.dma("sp", ident[:], ident_d, writes=[b_ident])
    wr_s = P.sb("wr_s", [128, 16, 16], F32)
    b_wr = P.buf()
    P.dma("sp", wr_s[:], wr.rearrange("(kc p) n -> p kc n", p=128), writes=[b_wr])
    lng_s, b_lng = load_bcast(P, "lng_s", lng, D)
    lnb_s, b_lnb = load_bcast(P, "lnb_s", lnb, D)
    wo_bf = P.sb("wo_bf", [128, 16, D], BF16)
    b_wo = P.buf()
    pb = [P.ps(f"pb{i}", [128, 512], F32) for i in range(8)]
    b_pb = P.bufs(8, "pb", excl=True)
    A = Arena(P, "arena", 22 * 1024)
    wst = [A.alloc([128, 16, 256], F32) for _ in range(2)]
    b_wst = P.bufs(2)
    for c in range(8):
        i = c % 2
        P.dma("sp", wst[i][:], wo.rearrange("(kc p) n -> p kc n", p=128)[:, :, c * 256:(c + 1) * 256], writes=[b_wst[i]])
        if c % 2:
            P.op("act", lambda e, i=i, c=c: e.activation(out=wo_bf[:, :, c * 256:(c + 1) * 256], in_=wst[i][:], func=ACTF.Copy), reads=[b_wst[i]], writes=[b_wo])
        else:
            P.op("dve", lambda e, i=i, c=c: e.tensor_copy(out=wo_bf[:, :, c * 256:(c + 1) * 256], in_=wst[i][:]), reads=[b_wst[i]], writes=[b_wo])
    P.barrier()
    A.reset()
    g1 = A.alloc([128, D], F32)
    sh2 = A.alloc([128, D], F32)
    sc2 = A.alloc([128, D], F32)
    b_g1, b_sh2, b_sc2 = P.buf(), P.buf(), P.buf()
    ob = A.alloc([128, 16, 512], BF16)
    b_ob = P.buf()
    xt = A.alloc([128, D], F32)
    zt = A.alloc([128, D], F32)
    xm = A.alloc([128, D], F32)
    h2 = A.alloc([128, D], F32)
    h2T = A.alloc([128, 16, 128], F32)
    h2b = A.alloc([128, D], BF16)
    b_xt, b_zt, b_xm, b_h2, b_h2T, b_h2b = (P.buf() for _ in range(6))
    sm = A.alloc([128, 64], F32)
    b_sm = P.buf()
    ln = LN(P, "ln")
    outs = []

    def load_modset(m):
        P.dma("sp", g1[:], m[0:1, 2 * D:3 * D].partition_broadcast(128), writes=[b_g1])
        P.dma("sp", sh2[:], m[0:1, 3 * D:4 * D].partition_broadcast(128), writes=[b_sh2])
        P.dma("sp", sc2[:], m[0:1, 4 * D:5 * D].partition_broadcast(128), writes=[b_sc2])
        P.op("pool", lambda e: e.tensor_scalar(out=sc2[:], in0=sc2[:], scalar1=1.0, scalar2=None, op0=ALU.add), reads=[b_sc2], writes=[b_sc2])

    blocks = ([(0, nctx_tile)] if nctx_tile else []) + [(t, min(4, ntile - t)) for t in range(nctx_tile, ntile, 4)]
    for bi, (t0, nt) in enumerate(blocks):
        if bi == 0:
            load_modset(modc if nctx_tile else modl)
        elif bi == 1 and nctx_tile:
            load_modset(modl)
        w = nt * 128
        P.dma("sp", ob[:, :, 0:w], oT.rearrange("(kc p) n -> p kc n", p=128)[:, :, t0 * 128:t0 * 128 + w], writes=[b_ob])
        for tb in range(nt):
            t = t0 + tb
            rows = slice(t * 128, (t + 1) * 128)
            P.dma("sp", xt[:], xin[rows, :], writes=[b_xt])
            for cc in range(4):
                for kc in range(16):
                    P.op("pe", lambda e, cc=cc, kc=kc, tb=tb: e.matmul(pb[cc][:], lhsT=ob[:, kc, tb * 128:(tb + 1) * 128], rhs=wo_bf[:, kc, cc * 512:(cc + 1) * 512],
                                                                      start=(kc == 0), stop=(kc == 15)), reads=[b_ob, b_wo], writes=[b_pb[cc]])
                P.op("dve", lambda e, cc=cc: e.tensor_tensor(out=zt[:, cc * 512:(cc + 1) * 512], in0=pb[cc][:], in1=g1[:, cc * 512:(cc + 1) * 512], op=ALU.mult),
                     reads=[b_pb[cc], b_g1], writes=[b_zt])
            P.op("dve", lambda e: e.scalar_tensor_tensor(out=zt[:], in0=xt[:], scalar=ALPHA, in1=zt[:], op0=ALU.mult, op1=ALU.add),
                 reads=[b_xt, b_zt], writes=[b_zt])
            ln.norm(xm[:], zt[:], b_zt, b_xm)
            P.op("dve", lambda e: e.tensor_tensor(out=xm[:], in0=xm[:], in1=lng_s[:], op=ALU.mult), reads=[b_xm, b_lng], writes=[b_xm])
            P.op("pool", lambda e: e.tensor_tensor(out=xm[:], in0=xm[:], in1=lnb_s[:], op=ALU.add), reads=[b_xm, b_lnb], writes=[b_xm])
            outs.append(P.dma("sp", xmid[rows, :], xm[:], reads=[b_xm]))
            ln.norm(h2[:], xm[:], b_xm, b_h2)
            P.op("dve", lambda e: e.tensor_tensor(out=h2[:], in0=h2[:], in1=sc2[:], op=ALU.mult), reads=[b_h2, b_sc2], writes=[b_h2])
            P.op("pool", lambda e: e.tensor_tensor(out=h2[:], in0=h2[:], in1=sh2[:], op=ALU.add), reads=[b_h2, b_sh2], writes=[b_h2])
            P.op("act", lambda e: e.activation(out=h2b[:], in_=h2[:], func=ACTF.Copy), reads=[b_h2], writes=[b_h2b])
            outs.append(P.dma("sp", h2o[rows, :], h2b[:], reads=[b_h2b]))
            for qi in range(4):
                pq, bpq = pb[4 + qi % 3], b_pb[4 + qi % 3]
                for k in range(4):
                    kc = qi * 4 + k
                    P.op("pe", lambda e, pq=pq, k=k, kc=kc: e.transpose(out=pq[:, k * 128:(k + 1) * 128], in_=h2[:, kc * 128:(kc + 1) * 128], identity=ident[:]),
                         reads=[b_h2, b_ident], writes=[bpq])
                P.op("act", lambda e, pq=pq, qi=qi: e.activation(out=h2T[:, qi * 4:(qi + 1) * 4, :], in_=pq[:].rearrange("p (k t) -> p k t", k=4), func=ACTF.Copy),
                     reads=[bpq], writes=[b_h2T])
            for kc in range(16):
                P.op("pe", lambda e, kc=kc: e.matmul(pb[7][:, 0:16], lhsT=h2T[:, kc, :], rhs=wr_s[:, kc, :], start=(kc == 0), stop=(kc == 15)),
                     reads=[b_h2T, b_wr], writes=[b_pb[7]])
            P.op("dve", lambda e: e.tensor_reduce(out=sm[:, 16:17], in_=pb[7][:, 0:16], axis=AX.X, op=ALU.max), reads=[b_pb[7]], writes=[b_sm])
            P.op("dve", lambda e: e.tensor_scalar(out=sm[:, 17:18], in0=sm[:, 16:17], scalar1=-1.0, scalar2=None, op0=ALU.mult), reads=[b_sm], writes=[b_sm])
            P.op("act", lambda e: e.activation(out=sm[:, 0:16], in_=pb[7][:, 0:16], func=ACTF.Exp, bias=sm[:, 17:18], scale=1.0, accum_out=sm[:, 18:19]),
                 reads=[b_pb[7], b_sm], writes=[b_sm])
            P.op("dve", lambda e: e.reciprocal(out=sm[:, 18:19], in_=sm[:, 18:19]), reads=[b_sm], writes=[b_sm])
            P.op("dve", lambda e: e.tensor_scalar(out=sm[:, 32:48], in0=sm[:, 0:16], scalar1=sm[:, 18:19], scalar2=None, op0=ALU.mult), reads=[b_sm], writes=[b_sm])
            outs.append(P.dma("sp", affo[rows, :], sm[:, 32:48], reads=[b_sm]))
    st = P.emit(final_waits=outs)
    print("LC stats", st, flush=True)
    return nc, P


NE = 16
FF = 1024


def ld_consts():
    p = np.arange(128)
    gmat = (p[:, None] // 8 == p[None, :] // 8).astype(np.float32)
    sel8 = np.zeros((128, 16), np.float32)
    sel8[np.arange(16) * 8, np.arange(16)] = 1.0
    tri = (p[:, None] < p[None, :]).astype(np.float32)
    iota = np.broadcast_to(np.arange(128, dtype=np.float32)[None, :], (128, 128)).copy()
    return {"gmat": gmat, "sel8": sel8, "tri": tri, "ones": np.ones((128, 128), np.float32), "iota3": iota,
            "ident_bf": np.eye(128, dtype=np.float32).astype(BF)}


class MoeCommon:
    def __init__(self, nc, P, nseg_lat, kcap_lat, has_ctx, arena_words):
        self.nc, self.P = nc, P
        self.has_ctx = has_ctx
        self.affT_lat = din(nc, "affT_lat", [16, nseg_lat * 8])
        self.affT_ctx = din(nc, "affT_ctx", [16, 256]) if has_ctx else None
        cd = {k: din(nc, k, list(v.shape), BF16 if v.dtype == BF else F32) for k, v in ld_consts().items()}

        def cload(name, shape, dt):
            t = P.sb(name + "_s", shape, dt)
            b = P.buf(name)
            P.dma("sp", t[:], cd[name], writes=[b])
            return t, b

        self.gmat, self.b_gmat = cload("gmat", [128, 128], F32)
        self.sel8, self.b_sel8 = cload("sel8", [128, 16], F32)
        self.tri, self.b_tri = cload("tri", [128, 128], F32)
        self.ones, self.b_ones = cload("ones", [128, 128], F32)
        self.ident, self.b_ident = cload("ident_bf", [128, 128], BF16)
        self.iota, self.b_iota = cload("iota3", [128, 128], F32)
        self.pb = [P.ps(f"pb{i}", [128, 512], F32) for i in range(8)]
        self.b_pb = P.bufs(8, "pb", excl=True)
        self.A = Arena(P, "arena", arena_words)
        self.nseg_lat, self.kcap_lat = nseg_lat, kcap_lat

    def thresholds(self):
        P, A, pb, b_pb = self.P, self.A, self.pb, self.b_pb
        thr = P.sb("thr", [128, 2, 16], F32)
        b_thr = P.buf()
        sets = [(self.affT_lat, self.nseg_lat, float(self.kcap_lat), 0)]
        if self.has_ctx:
            sets.append((self.affT_ctx, 32, 32.0, 1))
        for si_, (src, nseg, kcap, which) in enumerate(sets):
            if si_:
                P.barrier()
            A.reset()
            at = A.alloc([128, nseg], F32)
            junk = A.alloc([128, nseg], F32)
            bs = A.alloc([128, 8], F32)
            b_at, b_bs = P.buf(), P.buf()
            P.dma("sp", at[:], src.rearrange("e (s n) -> (e s) n", s=8), writes=[b_at])
            P.op("dve", lambda e, bs=bs: e.memset(bs[:], 0.0), writes=[b_bs])
            for it in range(32):
                dk = 2.0 ** -(it + 1)
                P.op("dve", lambda e, bs=bs, dk=dk: e.tensor_scalar(out=bs[:, 1:2], in0=bs[:, 0:1], scalar1=dk, scalar2=None, op0=ALU.add), reads=[b_bs], writes=[b_bs])
                P.op("dve", lambda e, bs=bs, at=at, junk=junk: e.tensor_scalar(out=junk[:], in0=at[:], scalar1=bs[:, 1:2], scalar2=None, op0=ALU.is_ge, op1=ALU.add,
                                                                             accum_out=bs[:, 2:3]), reads=[b_bs, b_at], writes=[b_bs])
                P.op("dve", lambda e, bs=bs: e.tensor_copy(out=bs[:, 4:5], in_=bs[:, 2:3]), reads=[b_bs], writes=[b_bs])
                P.op("pe", lambda e, bs=bs: e.matmul(pb[0][:, 0:1], lhsT=self.gmat[:], rhs=bs[:, 4:5], start=True, stop=True), reads=[self.b_gmat, b_bs], writes=[b_pb[0]])
                P.op("dve", lambda e, bs=bs, dk=dk, kcap=kcap: e.tensor_scalar(out=bs[:, 3:4], in0=pb[0][:, 0:1], scalar1=kcap - 0.5, scalar2=dk, op0=ALU.is_ge, op1=ALU.mult),
                     reads=[b_pb[0]], writes=[b_bs])
                P.op("dve", lambda e, bs=bs: e.tensor_tensor(out=bs[:, 0:1], in0=bs[:, 0:1], in1=bs[:, 3:4], op=ALU.add), reads=[b_bs], writes=[b_bs])
            tsel = A.alloc([128, 16], F32)
            P.op("dve", lambda e, bs=bs, tsel=tsel: e.tensor_scalar(out=tsel[:], in0=self.sel8[:], scalar1=bs[:, 0:1], scalar2=None, op0=ALU.mult), reads=[b_bs, self.b_sel8], writes=[b_at])
            P.op("pe", lambda e, tsel=tsel: e.matmul(pb[1][:, 0:16], lhsT=self.ones[:], rhs=tsel[:], start=True, stop=True), reads=[self.b_ones, b_at], writes=[b_pb[1]])
            P.op("dve", lambda e, which=which: e.tensor_copy(out=thr[:, which, :], in_=pb[1][:, 0:16]), reads=[b_pb[1]], writes=[b_thr])
        P.barrier()
        A.reset()
        self.thr, self.b_thr = thr, b_thr

    def select(self, aff_src, ntile, groups, want_hl):
        P, pb, b_pb = self.P, self.pb, self.b_pb
        aff = self.A.alloc([128, ntile, 16], F32)
        b_aff = P.buf()
        P.dma("sp", aff[:], aff_src.rearrange("(t p) e -> p t e", p=128), writes=[b_aff])
        mask = self.A.alloc([128, ntile, 16], F32)
        posm = P.sb("posm_s", [128, ntile, 16], F32)
        b_mask, b_posm = P.buf(), P.buf()
        is_ctx_group = [self.has_ctx and gi == 0 for gi in range(len(groups))]
        for gi, (tiles, ns) in enumerate(groups):
            which = 1 if is_ctx_group[gi] else 0
            for k, t in enumerate(tiles):
                P.op("dve", lambda e, t=t, which=which: e.tensor_tensor(out=mask[:, t, :], in0=aff[:, t, :], in1=self.thr[:, which, :], op=ALU.is_ge),
                     reads=[b_aff, self.b_thr], writes=[b_mask])
                P.op("pe", lambda e, t=t, k=k: e.matmul(pb[2][:, 0:16], lhsT=self.tri[:], rhs=mask[:, t, :], start=True, stop=(k == 0)),
                     reads=[self.b_tri, b_mask], writes=[b_pb[2]])
                for k2 in range(k):
                    P.op("pe", lambda e, t2=tiles[k2], k2=k2, k=k: e.matmul(pb[2][:, 0:16], lhsT=self.ones[:], rhs=mask[:, t2, :], start=False, stop=(k2 == k - 1)),
                         reads=[self.b_ones, b_mask], writes=[b_pb[2]])
                P.op("dve", lambda e, t=t: e.scalar_tensor_tensor(out=posm[:, t, :], in0=pb[2][:, 0:16], scalar=1.0, in1=mask[:, t, :], op0=ALU.add, op1=ALU.mult),
                     reads=[b_pb[2], b_mask], writes=[b_posm])
        P.op("dve", lambda e: e.tensor_scalar(out=posm[:], in0=posm[:], scalar1=-1.0, scalar2=None, op0=ALU.add), reads=[b_posm], writes=[b_posm])
        self.posm, self.b_posm, self.is_ctx_group = posm, b_posm, is_ctx_group
        if want_hl:
            affhl = P.sb("affhl", [128, ntile, 16, 2], BF16)
            b_affhl = P.buf()
            P.op("dve", lambda e: e.tensor_copy(out=affhl[:, :, :, 0], in_=aff[:]), reads=[b_aff], writes=[b_affhl])
            P.op("dve", lambda e: e.tensor_tensor(out=mask[:], in0=aff[:], in1=affhl[:, :, :, 0], op=ALU.subtract), reads=[b_aff, b_affhl, b_posm], writes=[b_mask])
            P.op("dve", lambda e: e.tensor_copy(out=affhl[:, :, :, 1], in_=mask[:]), reads=[b_mask], writes=[b_affhl])
            self.affhl, self.b_affhl = affhl, b_affhl


def build_ld1(groups, chunks, nseg_lat, kcap_lat, has_ctx):
    nc = new_nc()
    ntile = sum(len(g[0]) for g in groups)
    NT = ntile * 128
    P = Prog(nc)
    M = MoeCommon(nc, P, nseg_lat, kcap_lat, has_ctx, 39 * 1024)
    aff_all = din(nc, "aff_all", [NT, 16])
    h2 = din(nc, "h2", [NT, D], BF16)
    wg = din(nc, "wg", [2, D, FF])
    wu = din(nc, "wu", [2, D, FF])
    wd = din(nc, "wd", [2, FF, D])
    soff = []
    o = 0
    for g in groups:
        soff.append(o)
        o += g[1]
    NSLOT = o
    Yo = dout(nc, "Yo", [2, NSLOT, D], BF16)
    M.thresholds()
    M.select(aff_all, ntile, groups, True)
    A, pb, b_pb = M.A, M.pb, M.b_pb
    posm, b_posm, affhl, b_affhl, iota, b_iota = M.posm, M.b_posm, M.affhl, M.b_affhl, M.iota, M.b_iota
    outs = []
    P.barrier()
    A.reset()
    Wg_bf = A.alloc([128, 16, FF], BF16)
    Wu_bf = A.alloc([128, 16, FF], BF16)
    Wd_bf = A.alloc([128, 8, D], BF16)
    b_W = P.buf("W")
    wst = [A.alloc([128, 16, 128], F32) for _ in range(1)]
    b_wst = P.bufs(1)
    h2g = [A.alloc([128, 4, D], BF16) for _ in range(1)]
    b_h2g = P.bufs(1)
    sel = [A.alloc([128, 4, 128], BF16) for _ in range(2)]
    b_sel = P.bufs(2)
    Xg = A.alloc([128, 16, 512], BF16)
    b_Xg = P.buf()
    hidT = A.alloc([128, 8, 512], BF16)
    b_hid = P.buf()
    wsl = A.alloc([128, 4, 2], F32)
    wsum = A.alloc([128, 4], F32)
    b_wsl = P.buf()
    sg = [A.alloc([128, 512], F32) for _ in range(2)]
    b_sg = P.bufs(2)
    yst = [A.alloc([128, 512], BF16) for _ in range(2)]
    b_yst = P.bufs(2)
    P.op("dve", lambda e: e.memset(wsl[:], 0.0), writes=[b_wsl])
    cast_eng = ["dve", "act", "pool"]
    kw = [0, 0, 0]

    def cast(dst, src_t, bsrc):
        ce = cast_eng[kw[0] % 3]
        kw[0] += 1
        if ce == "act":
            P.op("act", lambda e: e.activation(out=dst, in_=src_t, func=ACTF.Copy), reads=[bsrc], writes=[b_W])
        else:
            P.op(ce, lambda e: e.tensor_copy(out=dst, in_=src_t), reads=[bsrc], writes=[b_W])

    for e_ in range(2):
        for (wsrc, wdst) in ((wg, Wg_bf), (wu, Wu_bf)):
            for c in range(8):
                i = 0
                P.dma("sp", wst[i][:], wsrc[e_].rearrange("(kc p) f -> p kc f", p=128)[:, :, c * 128:(c + 1) * 128], writes=[b_wst[i]])
                cast(wdst[:, :, c * 128:(c + 1) * 128], wst[i][:], b_wst[i])
        for c in range(8):
            i = 0
            wv = wst[i][:].rearrange("p a b -> p (a b)").rearrange("p (a b) -> p a b", a=8)
            P.dma("sp", wv, wd[e_].rearrange("(fc p) n -> p fc n", p=128)[:, :, c * 256:(c + 1) * 256], writes=[b_wst[i]])
            cast(Wd_bf[:, :, c * 256:(c + 1) * 256], wv, b_wst[i])
        for ch in chunks:
            c0 = soff[ch[0]]
            cw = sum(groups[g][1] for g in ch)
            for li, gi in enumerate(ch):
                tiles, ns = groups[gi]
                hi = kw[2] % 2
                kw[2] += 1
                lo = soff[gi] - c0
                for k, t in enumerate(tiles):
                    P.dma("sp", h2g[0][:, k, :], h2[t * 128:(t + 1) * 128, :], writes=[b_h2g[0]])
                    P.op("dve", lambda e, hi=hi, k=k, t=t, e_=e_: e.tensor_scalar(out=sel[hi][:, k, :], in0=iota[:], scalar1=posm[:, t, e_:e_ + 1], scalar2=None, op0=ALU.is_equal),
                         reads=[b_posm, b_iota], writes=[b_sel[hi]])
                for fc in range(16):
                    pg, bpg = pb[fc % 2], b_pb[fc % 2]
                    for k, t in enumerate(tiles):
                        P.op("pe", lambda e, pg=pg, hi=hi, k=k, ns=ns, fc=fc, nt_=len(tiles): e.matmul(pg[:, 0:ns], lhsT=h2g[0][:, k, fc * 128:(fc + 1) * 128], rhs=sel[hi][:, k, 0:ns],
                                                                                                  start=(k == 0), stop=(k == nt_ - 1)), reads=[b_h2g[0], b_sel[hi]], writes=[bpg])
                    if fc % 2:
                        P.op("act", lambda e, pg=pg, ns=ns, fc=fc, lo=lo: e.activation(out=Xg[:, fc, lo:lo + ns], in_=pg[:, 0:ns], func=ACTF.Copy), reads=[bpg], writes=[b_Xg])
                    else:
                        P.op("dve", lambda e, pg=pg, ns=ns, fc=fc, lo=lo: e.tensor_copy(out=Xg[:, fc, lo:lo + ns], in_=pg[:, 0:ns]), reads=[bpg], writes=[b_Xg])
                for k, t in enumerate(tiles):
                    P.op("pe", lambda e, hi=hi, k=k, t=t, ns=ns, e_=e_, nt_=len(tiles): e.matmul(pb[6][0:ns, 0:2], lhsT=sel[hi][:, k, 0:ns], rhs=affhl[:, t, e_, :],
                                                                                            start=(k == 0), stop=(k == nt_ - 1)), reads=[b_sel[hi], b_affhl], writes=[b_pb[6]])
                P.op("dve", lambda e, li=li, ns=ns: e.tensor_copy(out=wsl[0:ns, li, :], in_=pb[6][0:ns, 0:2]), reads=[b_pb[6]], writes=[b_wsl])
            P.op("dve", lambda e: e.tensor_tensor(out=wsum[:], in0=wsl[:, :, 0], in1=wsl[:, :, 1], op=ALU.add), reads=[b_wsl], writes=[b_wsl])
            for fc in range(8):
                fl = fc % 2
                pgt, bpgt = pb[2 + fl], b_pb[2 + fl]
                pup, bpup = pb[4 + fl], b_pb[4 + fl]
                for (pp, bpp, W_) in ((pgt, bpgt, Wg_bf), (pup, bpup, Wu_bf)):
                    for kc in range(16):
                        P.op("pe", lambda e, pp=pp, W_=W_, kc=kc, fc=fc, cw=cw: e.matmul(pp[:, 0:cw], lhsT=W_[:, kc, fc * 128:(fc + 1) * 128], rhs=Xg[:, kc, 0:cw],
                                                                                     start=(kc == 0), stop=(kc == 15)), reads=[b_W, b_Xg], writes=[bpp])
                P.op("act", lambda e, pgt=pgt, fl=fl, cw=cw: e.activation(out=sg[fl][:, 0:cw], in_=pgt[:, 0:cw], func=ACTF.Silu), reads=[bpgt], writes=[b_sg[fl]])
                P.op("dve", lambda e, pup=pup, fl=fl, fc=fc, cw=cw: e.tensor_tensor(out=hidT[:, fc, 0:cw], in0=pup[:, 0:cw], in1=sg[fl][:, 0:cw], op=ALU.mult),
                     reads=[bpup, b_sg[fl]], writes=[b_hid])
            cnt = 0
            for cc in range(4):
                for li, gi in enumerate(ch):
                    tiles, ns = groups[gi]
                    lo = soff[gi] - c0
                    yi = cnt % 2
                    cnt += 1
                    py, bpy = pb[6 + yi], b_pb[6 + yi]
                    for fc in range(8):
                        P.op("pe", lambda e, py=py, ns=ns, fc=fc, lo=lo, cc=cc: e.matmul(py[0:ns, :], lhsT=hidT[:, fc, lo:lo + ns], rhs=Wd_bf[:, fc, cc * 512:(cc + 1) * 512],
                                                                                     start=(fc == 0), stop=(fc == 7)), reads=[b_hid, b_W], writes=[bpy])
                    P.op("act", lambda e, py=py, yi=yi, li=li, ns=ns: e.activation(out=yst[yi][0:ns, :], in_=py[0:ns, :], func=ACTF.Identity, bias=0.0, scale=wsum[0:ns, li:li + 1]),
                         reads=[bpy, b_wsl], writes=[b_yst[yi]])
                    outs.append(P.dma("sp", Yo[e_, soff[gi]:soff[gi] + ns, cc * 512:(cc + 1) * 512], yst[yi][0:ns, :], reads=[b_yst[yi]]))
    st = P.emit(final_waits=outs)
    print("LD1 stats", st, flush=True)
    return nc, P


def build_ld2(groups, nseg_lat, kcap_lat, has_ctx):
    nc = new_nc()
    ntile = sum(len(g[0]) for g in groups)
    NT = ntile * 128
    P = Prog(nc)
    M = MoeCommon(nc, P, nseg_lat, kcap_lat, has_ctx, 32 * 1024)
    aff_own = din(nc, "aff_own", [NT, 16])
    xmid = din(nc, "xmid", [NT, D])
    soff = []
    o = 0
    for g in groups:
        soff.append(o)
        o += g[1]
    NSLOT = o
    Yin = din(nc, "Yin", [NE, NSLOT, D], BF16)
    modl = din(nc, "modl", [1, 12288])
    modc = din(nc, "modc", [1, 12288])
    lng = din(nc, "lng", [1, D])
    lnb = din(nc, "lnb", [1, D])
    xout = dout(nc, "xout", [NT, D])
    M.thresholds()
    M.select(aff_own, ntile, groups, False)
    A, pb, b_pb = M.A, M.pb, M.b_pb
    posm, b_posm, iota, b_iota, ident, b_ident = M.posm, M.b_posm, M.iota, M.b_iota, M.ident, M.b_ident
    g2 = A.alloc([128, D], F32)
    b_g2 = P.buf()
    lng_s = A.alloc([128, D], F32)
    lnb_s = A.alloc([128, D], F32)
    b_ln = P.buf()
    P.dma("sp", lng_s[:], lng.partition_broadcast(128), writes=[b_ln])
    P.dma("sp", lnb_s[:], lnb.partition_broadcast(128), writes=[b_ln])
    Yq = A.alloc([128, NE, D], BF16)
    b_Yq = P.buf()
    sel3 = A.alloc([128, 16, 128], BF16)
    b_sel3 = P.buf()
    selT = A.alloc([128, 16, 128], BF16)
    b_selT = P.buf()
    xt = A.alloc([128, D], F32)
    zt = A.alloc([128, D], F32)
    xo = A.alloc([128, D], F32)
    b_xt, b_zt, b_xo = P.buf(), P.buf(), P.buf()
    ln = LN(P, "ln")
    pbf = [pb[4][:].bitcast(BF16), pb[5][:].bitcast(BF16)]
    outs = []
    for gi, (tiles, ns) in enumerate(groups):
        m = modc if M.is_ctx_group[gi] else modl
        if gi == 0 or (gi == 1 and M.is_ctx_group[0]):
            P.dma("sp", g2[:], m[0:1, 5 * D:6 * D].partition_broadcast(128), writes=[b_g2])
        P.dma("sp", Yq[0:ns], Yin[:, soff[gi]:soff[gi] + ns, :].rearrange("e s n -> s e n"), writes=[b_Yq])
        for t in tiles:
            rows = slice(t * 128, (t + 1) * 128)
            P.dma("sp", xt[:], xmid[rows, :], writes=[b_xt])
            posm_b = posm[:, t, :].unsqueeze(2).to_broadcast([128, 16, 128])
            P.op("dve", lambda e, posm_b=posm_b: e.tensor_tensor(out=sel3[:], in0=iota[:].unsqueeze(1).to_broadcast([128, 16, 128]), in1=posm_b, op=ALU.is_equal),
                 reads=[b_posm, b_iota], writes=[b_sel3])
            for half in range(2):
                for k in range(8):
                    e_ = half * 8 + k
                    P.op("pe", lambda e, half=half, k=k, e_=e_: e.transpose(out=pbf[half][:, k * 128:(k + 1) * 128], in_=sel3[:, e_, :], identity=ident[:]),
                         reads=[b_sel3, b_ident], writes=[b_pb[4 + half]])
                if half:
                    P.op("act", lambda e, half=half: e.activation(out=selT[:, half * 8:(half + 1) * 8, :], in_=pbf[half][:].rearrange("p (k n) -> p k n", k=8), func=ACTF.Copy),
                         reads=[b_pb[4 + half]], writes=[b_selT])
                else:
                    P.op("dve", lambda e, half=half: e.tensor_copy(out=selT[:, half * 8:(half + 1) * 8, :], in_=pbf[half][:].rearrange("p (k n) -> p k n", k=8)),
                         reads=[b_pb[4 + half]], writes=[b_selT])
            for cc in range(4):
                for e_ in range(NE):
                    P.op("pe", lambda e, cc=cc, e_=e_, ns=ns: e.matmul(pb[cc][:], lhsT=selT[0:ns, e_, :], rhs=Yq[0:ns, e_, cc * 512:(cc + 1) * 512], start=(e_ == 0), stop=(e_ == NE - 1)),
                         reads=[b_selT, b_Yq], writes=[b_pb[cc]])
                P.op("dve", lambda e, cc=cc: e.tensor_tensor(out=zt[:, cc * 512:(cc + 1) * 512], in0=pb[cc][:], in1=g2[:, cc * 512:(cc + 1) * 512], op=ALU.mult),
                     reads=[b_pb[cc], b_g2], writes=[b_zt])
            P.op("dve", lambda e: e.scalar_tensor_tensor(out=zt[:], in0=xt[:], scalar=ALPHA, in1=zt[:], op0=ALU.mult, op1=ALU.add), reads=[b_xt, b_zt], writes=[b_zt])
            ln.norm(xo[:], zt[:], b_zt, b_xo)
            P.op("dve", lambda e: e.tensor_tensor(out=xo[:], in0=xo[:], in1=lng_s[:], op=ALU.mult), reads=[b_xo, b_ln], writes=[b_xo])
            P.op("pool", lambda e: e.tensor_tensor(out=xo[:], in0=xo[:], in1=lnb_s[:], op=ALU.add), reads=[b_xo, b_ln], writes=[b_xo])
            outs.append(P.dma("sp", xout[rows, :], xo[:], reads=[b_xo]))
    st = P.emit(final_waits=outs)
    print("LD2 stats", st, flush=True)
    return nc, P


NCTX = 256


def le_consts():
    rot = np.zeros((128, 128), np.float32)
    for m in range(32):
        rot[m + 32, m] = -1.0
    for m in range(32, 64):
        rot[m - 32, m] = 1.0
    return {"rot64": rot, "ones": np.ones((128, 128), np.float32), "ones_bf": np.ones((128, 128), np.float32).astype(BF)}


def build_le(nblk=32, debug=False):
    nc = new_nc()
    NLAT = nblk * 512
    NTOK = NCTX + NLAT
    NT128 = NTOK // 128
    hT = din(nc, "hT", [D, NTOK], BF16)
    wdn = din(nc, "wdn", [D, 1088])
    gqk = din(nc, "gqk", [128, 8])
    wuq = din(nc, "wuq", [2, 512, 192])
    wukv = din(nc, "wukv", [2, 512, 256])
    cosT = din(nc, "cos64", [64, NLAT])
    sinT = din(nc, "sin64", [64, NLAT])
    c_rot = din(nc, "rot64", [128, 128])
    c_ones = din(nc, "ones", [128, 128])
    c_ones_bf = din(nc, "ones_bf", [128, 128], BF16)
    oT = dout(nc, "oT", [256, NLAT], BF16)
    dscr_ = dout if debug else dscr
    s_cq = dscr_(nc, "s_cq", [4, 128, NTOK], BF16)
    s_ckv = dscr_(nc, "s_ckv", [4, 128, NTOK], BF16)
    s_qn = dscr_(nc, "s_qn", [2, 128, NTOK], BF16)
    s_qr = dscr_(nc, "s_qr", [2, 64, NTOK], BF16)
    P = Prog(nc)
    blocks = [(0, NCTX)] + [(NCTX + 512 * i, 512) for i in range(nblk)]
    NBLK = len(blocks)
    db = {n: P.bufs(NBLK, n) for n in ("cq", "ckv", "qn0", "qn1", "qr0", "qr1")}

    def cload(name, src, shape, dt):
        t = P.sb(name, shape, dt)
        b = P.buf(name)
        P.dma("sp", t[:], src, writes=[b])
        return t, b

    rot, b_rot = cload("rot_s", c_rot, [128, 128], F32)
    ones, b_ones = cload("ones_s", c_ones, [128, 128], F32)
    ones_bf, b_onesbf = cload("onesbf_s", c_ones_bf, [128, 128], BF16)
    gq, b_gq = cload("gq_s", gqk, [128, 8], F32)
    KR = P.sb("KR", [64, NTOK], BF16)
    b_KR = P.bufs(NBLK, "KR")
    b_KN = P.bufs(NBLK, "KN")
    b_V = P.bufs(NBLK, "V")
    pb = [P.ps(f"pb{i}", [128, 512], F32) for i in range(8)]
    b_pb = P.bufs(8, "pb", excl=True)
    A = Arena(P, "arena", 31 * 1024)

    wdn_bf = A.alloc([128, 16, 1088], BF16)
    b_w = P.buf("w")
    wst = [A.alloc([128, 16, 136], F32) for _ in range(2)]
    b_wst = P.bufs(2)
    for c in range(8):
        i = c % 2
        P.dma("sp", wst[i][:], wdn.rearrange("(kc p) n -> p kc n", p=128)[:, :, c * 136:(c + 1) * 136], writes=[b_wst[i]])
        if c % 2:
            P.op("act", lambda e, i=i, c=c: e.activation(out=wdn_bf[:, :, c * 136:(c + 1) * 136], in_=wst[i][:], func=ACTF.Copy), reads=[b_wst[i]], writes=[b_w])
        else:
            P.op("dve", lambda e, i=i, c=c: e.tensor_copy(out=wdn_bf[:, :, c * 136:(c + 1) * 136], in_=wst[i][:]), reads=[b_wst[i]], writes=[b_w])
    hb = [A.alloc([128, 16, 512], BF16) for _ in range(2)]
    b_hb = P.bufs(2, "hb")
    cf = A.alloc([128, 4, 512], F32)
    b_cf = P.buf()
    sq = [A.alloc([128, 512], F32) for _ in range(2)]
    b_sq = P.bufs(2)
    rs = A.alloc([128, 512], F32)
    b_rs = P.buf()
    cn = [A.alloc([128, 4, 512], BF16) for _ in range(2)]
    b_cn = P.bufs(2)
    cs = [A.alloc([64, 2, 512], F32) for _ in range(2)]
    b_cs = P.bufs(2)
    t64 = [A.alloc([128, 512], F32) for _ in range(3)]
    b_t64 = P.bufs(3)
    P.op("dve", lambda e, t=t64[0]: e.memset(t[:], 0.0), writes=[b_t64[0]])
    kk = [0, 0]

    def rope64(src_ps, bsrc, dest, bdest, w, cs_t, bcs, tt, btt, lat):
        if not lat:
            P.op("act", lambda e: e.activation(out=dest, in_=src_ps, func=ACTF.Copy), reads=[bsrc], writes=[bdest])
            return
        a, ba = tt[0], btt[0]
        b_, bb = tt[1], btt[1]
        c_, bc = tt[2], btt[2]
        P.op("act", lambda e: e.activation(out=a[0:64, 0:w], in_=src_ps, func=ACTF.Copy), reads=[bsrc], writes=[ba])
        P.op("pe", lambda e: e.matmul(pb[7][:, 0:w], lhsT=rot[:], rhs=a[:, 0:w], start=True, stop=True), reads=[b_rot, ba], writes=[b_pb[7]])
        P.op("dve", lambda e: e.tensor_tensor(out=b_[0:64, 0:w], in0=pb[7][0:64, 0:w], in1=cs_t[:, 1, 0:w], op=ALU.mult), reads=[b_pb[7], bcs], writes=[bb])
        P.op("pool", lambda e: e.tensor_tensor(out=c_[0:64, 0:w], in0=a[0:64, 0:w], in1=cs_t[:, 0, 0:w], op=ALU.mult), reads=[ba, bcs], writes=[bc])
        P.op("dve", lambda e: e.tensor_tensor(out=dest, in0=b_[0:64, 0:w], in1=c_[0:64, 0:w], op=ALU.add), reads=[bb, bc], writes=[bdest])

    for bi, (s0, w) in enumerate(blocks):
        lat = bi > 0
        i = bi % 2
        P.dma("sp", hb[i][:, :, 0:w], hT.rearrange("(kc p) n -> p kc n", p=128)[:, :, s0:s0 + w], writes=[b_hb[i]])
        if lat:
            l0 = s0 - NCTX
            P.dma("sp", cs[i][:, 0, 0:w], cosT[:, l0:l0 + w], writes=[b_cs[i]])
            P.dma("sp", cs[i][:, 1, 0:w], sinT[:, l0:l0 + w], writes=[b_cs[i]])
        for which in ((0, 1) if lat else (1,)):
            for c in range(4):
                col = which * 512 + c * 128
                pf, bpf = pb[c % 3], b_pb[c % 3]
                for kc in range(16):
                    P.op("pe", lambda e, pf=pf, kc=kc, col=col, i=i, w=w: e.matmul(pf[:, 0:w], lhsT=wdn_bf[:, kc, col:col + 128], rhs=hb[i][:, kc, 0:w],
                                                                                start=(kc == 0), stop=(kc == 15)), reads=[b_w, b_hb[i]], writes=[bpf])
                P.op("act", lambda e, pf=pf, c=c, w=w: e.activation(out=cf[:, c, 0:w], in_=pf[:, 0:w], func=ACTF.Copy), reads=[bpf], writes=[b_cf])
                si = kk[0] % 2
                kk[0] += 1
                P.op("act", lambda e, pf=pf, si=si, w=w: e.activation(out=sq[si][:, 0:w], in_=pf[:, 0:w], func=ACTF.Square), reads=[bpf], writes=[b_sq[si]])
                P.op("pe", lambda e, si=si, c=c, w=w: e.matmul(pb[3][:, 0:w], lhsT=ones[:], rhs=sq[si][:, 0:w], start=(c == 0), stop=(c == 3)),
                     reads=[b_ones, b_sq[si]], writes=[b_pb[3]])
            P.op("act", lambda e, w=w: e.activation(out=rs[:, 0:w], in_=pb[3][:, 0:w], func=ACTF.Sqrt, bias=EPS, scale=1.0 / 512), reads=[b_pb[3]], writes=[b_rs])
            P.op("dve", lambda e, w=w: e.reciprocal(out=rs[:, 0:w], in_=rs[:, 0:w]), reads=[b_rs], writes=[b_rs])
            ci = kk[1] % 2
            kk[1] += 1
            for c in range(4):
                P.op("dve", lambda e, c=c, w=w, ci=ci, which=which: e.scalar_tensor_tensor(out=cn[ci][:, c, 0:w], in0=cf[:, c, 0:w], scalar=gq[:, which * 4 + c:which * 4 + c + 1],
                                                                                          in1=rs[:, 0:w], op0=ALU.mult, op1=ALU.mult), reads=[b_cf, b_rs, b_gq], writes=[b_cn[ci]])
            dst, dbuf = (s_cq, db["cq"]) if which == 0 else (s_ckv, db["ckv"])
            P.dma("sp", dst[:, :, s0:s0 + w].rearrange("c p n -> p c n"), cn[ci][:, :, 0:w], reads=[b_cn[ci]], writes=[dbuf[bi]])
        for kc in range(16):
            P.op("pe", lambda e, kc=kc, i=i, w=w: e.matmul(pb[4][0:64, 0:w], lhsT=wdn_bf[:, kc, 1024:1088], rhs=hb[i][:, kc, 0:w], start=(kc == 0), stop=(kc == 15)),
                 reads=[b_w, b_hb[i]], writes=[b_pb[4]])
        rope64(pb[4][0:64, 0:w], b_pb[4], KR[:, s0:s0 + w], b_KR[bi], w, cs[i], b_cs[i], t64, b_t64, lat)

    SCALE = 192 ** -0.5
    out_dmas = []
    P.barrier()
    A.reset()
    KN = A.alloc([128, NTOK], BF16)
    Vres = A.alloc([128, NT128, 128], BF16)
    A.mark()
    for hh in range(2):
        P.barrier()
        A.reset()
        wuq_bf = A.alloc([128, 4, 192], BF16)
        wukv_bf = A.alloc([128, 4, 256], BF16)
        wst2 = A.alloc([128, 4, 256], F32)
        b_w2, b_wst2 = P.buf(), P.buf()
        P.dma("sp", wst2[:, :, 0:192], wuq[hh].rearrange("(c p) n -> p c n", p=128), writes=[b_wst2])
        P.op("dve", lambda e: e.tensor_copy(out=wuq_bf[:], in_=wst2[:, :, 0:192]), reads=[b_wst2], writes=[b_w2])
        P.dma("sp", wst2[:], wukv[hh].rearrange("(c p) n -> p c n", p=128), writes=[b_wst2])
        P.op("dve", lambda e: e.tensor_copy(out=wukv_bf[:], in_=wst2[:]), reads=[b_wst2], writes=[b_w2])
        cqb = [A.alloc([128, 4, 512], BF16) for _ in range(2)]
        ckb = [A.alloc([128, 4, 512], BF16) for _ in range(2)]
        b_cqb, b_ckb = P.bufs(2), P.bufs(2)
        cs2 = [A.alloc([64, 2, 512], F32) for _ in range(2)]
        b_cs2 = P.bufs(2)
        qno = [A.alloc([128, 512], BF16) for _ in range(2)]
        qro = [A.alloc([64, 512], BF16) for _ in range(2)]
        b_qno, b_qro = P.bufs(2), P.bufs(2)
        t64b = [A.alloc([128, 512], F32) for _ in range(3)]
        b_t64b = P.bufs(3)
        P.op("dve", lambda e, t=t64b[0]: e.memset(t[:], 0.0), writes=[b_t64b[0]])
        for bi, (s0, w) in enumerate(blocks):
            lat = bi > 0
            i = bi % 2
            P.dma("sp", ckb[i][:, :, 0:w], s_ckv[:, :, s0:s0 + w].rearrange("c p n -> p c n"), reads=[db["ckv"][bi]], writes=[b_ckb[i]])
            if lat:
                l0 = s0 - NCTX
                P.dma("sp", cqb[i][:, :, 0:w], s_cq[:, :, s0:s0 + w].rearrange("c p n -> p c n"), reads=[db["cq"][bi]], writes=[b_cqb[i]])
                P.dma("sp", cs2[i][:, 0, 0:w], cosT[:, l0:l0 + w], writes=[b_cs2[i]])
                P.dma("sp", cs2[i][:, 1, 0:w], sinT[:, l0:l0 + w], writes=[b_cs2[i]])
                for c in range(4):
                    P.op("pe", lambda e, c=c, i=i, w=w, wuq_bf=wuq_bf, cqb=cqb: e.matmul(pb[0][:, 0:w], lhsT=wuq_bf[:, c, 0:128], rhs=cqb[i][:, c, 0:w], start=(c == 0), stop=(c == 3)),
                         reads=[b_w2, b_cqb[i]], writes=[b_pb[0]])
                P.op("act", lambda e, i=i, w=w: e.activation(out=qno[i][:, 0:w], in_=pb[0][:, 0:w], func=ACTF.Copy), reads=[b_pb[0]], writes=[b_qno[i]])
                P.dma("sp", s_qn[hh, :, s0:s0 + w], qno[i][:, 0:w], reads=[b_qno[i]], writes=[db[f"qn{hh}"][bi]])
                for c in range(4):
                    P.op("pe", lambda e, c=c, i=i, w=w: e.matmul(pb[1][0:64, 0:w], lhsT=wuq_bf[:, c, 128:192], rhs=cqb[i][:, c, 0:w], start=(c == 0), stop=(c == 3)),
                         reads=[b_w2, b_cqb[i]], writes=[b_pb[1]])
                rope64(pb[1][0:64, 0:w], b_pb[1], qro[i][:, 0:w], b_qro[i], w, cs2[i], b_cs2[i], t64b, b_t64b, True)
                P.dma("sp", s_qr[hh, :, s0:s0 + w], qro[i][:, 0:w], reads=[b_qro[i]], writes=[db[f"qr{hh}"][bi]])
            for c in range(4):
                P.op("pe", lambda e, c=c, i=i, w=w: e.matmul(pb[2][:, 0:w], lhsT=wukv_bf[:, c, 0:128], rhs=ckb[i][:, c, 0:w], start=(c == 0), stop=(c == 3)),
                     reads=[b_w2, b_ckb[i]], writes=[b_pb[2]])
            P.op("act", lambda e, s0=s0, w=w: e.activation(out=KN[:, s0:s0 + w], in_=pb[2][:, 0:w], func=ACTF.Copy), reads=[b_pb[2]], writes=[b_KN[bi]])
            for tt in range(w // 128):
                pt, bpt = pb[3 + tt % 2], b_pb[3 + tt % 2]
                for c in range(4):
                    P.op("pe", lambda e, pt=pt, c=c, i=i, tt=tt: e.matmul(pt[:, 0:128], lhsT=ckb[i][:, c, tt * 128:(tt + 1) * 128], rhs=wukv_bf[:, c, 128:256], start=(c == 0), stop=(c == 3)),
                         reads=[b_w2, b_ckb[i]], writes=[bpt])
                tok = s0 + tt * 128
                P.op("dve", lambda e, pt=pt, tok=tok: e.tensor_copy(out=Vres[:, tok // 128, :], in_=pt[:, 0:128]), reads=[bpt], writes=[b_V[bi]])
        P.barrier()
        A.reset()
        qn_s = [A.alloc([128, 512], BF16) for _ in range(2)]
        qr_s = [A.alloc([64, 512], BF16) for _ in range(2)]
        b_qns, b_qrs = P.bufs(2), P.bufs(2)
        NPT = 4
        pT_sb = [A.alloc([128, 512], BF16) for _ in range(NPT)]
        b_pTs = P.bufs(NPT)
        rz = [A.alloc([128, 512], F32) for _ in range(2)]
        b_rz = P.bufs(2)
        ob = [A.alloc([128, 512], BF16) for _ in range(2)]
        b_ob = P.bufs(2)
        accD = [A.alloc([128, 512], F32) for _ in range(2)]
        accP = [A.alloc([128, 512], F32) for _ in range(2)]
        b_accD, b_accP = P.bufs(2), P.bufs(2)
        kcount = 0
        for bi, (s0, w) in enumerate(blocks):
            if bi == 0:
                continue
            i = bi % 2
            P.dma("sp", qn_s[i][:, 0:w], s_qn[hh, :, s0:s0 + w], reads=[db[f"qn{hh}"][bi]], writes=[b_qns[i]])
            P.dma("sp", qr_s[i][:, 0:w], s_qr[hh, :, s0:s0 + w], reads=[db[f"qr{hh}"][bi]], writes=[b_qrs[i]])
            pO, bpO = pb[3 + i], b_pb[3 + i]
            pZ, bpZ = pb[5 + i], b_pb[5 + i]
            nkc = NT128
            LOOK = 2
            slots = {}
            for step in range(nkc + LOOK):
                if step < nkc:
                    kc = step
                    kblk = 0 if kc < 2 else 1 + (kc * 128 - NCTX) // 512
                    si = kcount % 3
                    ti = kcount % NPT
                    kcount += 1
                    slots[kc] = ti
                    pS, bpS = pb[si], b_pb[si]
                    P.op("pe", lambda e, pS=pS, kc=kc, i=i, w=w: e.matmul(pS[:, 0:w], lhsT=KN[:, kc * 128:(kc + 1) * 128], rhs=qn_s[i][:, 0:w], start=True, stop=False),
                         reads=[b_KN[kblk], b_qns[i]], writes=[bpS])
                    P.op("pe", lambda e, pS=pS, kc=kc, i=i, w=w: e.matmul(pS[:, 0:w], lhsT=KR[:, kc * 128:(kc + 1) * 128], rhs=qr_s[i][:, 0:w], start=False, stop=True),
                         reads=[b_KR[kblk], b_qrs[i]], writes=[bpS])
                    P.op("act", lambda e, pS=pS, ti=ti, w=w: e.activation(out=pT_sb[ti][:, 0:w], in_=pS[:, 0:w], func=ACTF.Exp, scale=SCALE), reads=[bpS], writes=[b_pTs[ti]])
                if step >= LOOK:
                    kc = step - LOOK
                    kblk = 0 if kc < 2 else 1 + (kc * 128 - NCTX) // 512
                    ti = slots[kc]
                    P.op("pe", lambda e, pO=pO, kc=kc, ti=ti, w=w, nkc=nkc: e.matmul(pO[:, 0:w], lhsT=Vres[:, kc, :], rhs=pT_sb[ti][:, 0:w], start=(kc == 0), stop=(kc == nkc - 1)),
                         reads=[b_V[kblk], b_pTs[ti]], writes=[bpO])
                    eng = "dve"
                    acc, bacc = accD[i], b_accD[i]
                    if kc < 1:
                        P.op(eng, lambda e, acc=acc, ti=ti, w=w: e.tensor_copy(out=acc[:, 0:w], in_=pT_sb[ti][:, 0:w]), reads=[b_pTs[ti]], writes=[bacc])
                    else:
                        P.op(eng, lambda e, acc=acc, ti=ti, w=w: e.tensor_tensor(out=acc[:, 0:w], in0=acc[:, 0:w], in1=pT_sb[ti][:, 0:w], op=ALU.add), reads=[b_pTs[ti], bacc], writes=[bacc])
            P.op("pe", lambda e, pZ=pZ, i=i, w=w, accD=accD: e.matmul(pZ[:, 0:w], lhsT=ones[:], rhs=accD[i][:, 0:w], start=True, stop=True), reads=[b_ones, b_accD[i]], writes=[bpZ])
            P.op("dve", lambda e, i=i, pZ=pZ, w=w: e.reciprocal(out=rz[i][:, 0:w], in_=pZ[:, 0:w]), reads=[bpZ], writes=[b_rz[i]])
            P.op("dve", lambda e, i=i, pO=pO, w=w: e.tensor_tensor(out=ob[i][:, 0:w], in0=pO[:, 0:w], in1=rz[i][:, 0:w], op=ALU.mult), reads=[bpO, b_rz[i]], writes=[b_ob[i]])
            l0 = s0 - NCTX
            out_dmas.append(P.dma("sp", oT[hh * 128:(hh + 1) * 128, l0:l0 + w], ob[i][:, 0:w], reads=[b_ob[i]]))
    st = P.emit(final_waits=out_dmas)
    print("LE stats", st, flush=True)
    return nc, P


def le_inputs(inp, hT_all, nblk=32):
    NLAT = nblk * 512
    consts = le_consts()
    n_tok = 16384
    rows = n_tok // 64
    row = np.repeat(np.arange(rows, dtype=np.float32), 64)
    col = np.tile(np.arange(64, dtype=np.float32), rows)
    n_freq = 16
    inv = (10000.0 ** (-np.arange(n_freq, dtype=np.float32) / n_freq)).astype(np.float32)
    ang = np.concatenate([row[:, None] * inv, col[:, None] * inv], -1)
    cos = np.cos(ang).astype(np.float32)
    sin = np.sin(ang).astype(np.float32)
    cos64 = np.ascontiguousarray(np.concatenate([cos, cos], -1).T[:, :NLAT])
    sin64 = np.ascontiguousarray(np.concatenate([sin, sin], -1).T[:, :NLAT])
    hTs = np.ascontiguousarray(hT_all[:, :NCTX + NLAT])
    wdn = np.asarray(inp["mla_w_down"][0])
    gqk = np.ascontiguousarray(np.concatenate([np.asarray(inp["mla_q_norm_g"][0]).reshape(4, 128).T, np.asarray(inp["mla_kv_norm_g"][0]).reshape(4, 128).T], 1))
    wuq_all = np.asarray(inp["mla_w_uq"][0]).reshape(512, 16, 192)
    wukv_all = np.asarray(inp["mla_w_ukv"][0]).reshape(512, 16, 256)
    maps = []
    for j in range(NCORE):
        m = {"hT": hTs, "wdn": wdn, "gqk": gqk,
             "wuq": np.ascontiguousarray(wuq_all[:, 2 * j:2 * j + 2].transpose(1, 0, 2)),
             "wukv": np.ascontiguousarray(wukv_all[:, 2 * j:2 * j + 2].transpose(1, 0, 2)),
             "cos64": cos64, "sin64": sin64}
        m.update(consts)
        maps.append(m)
    return maps

import time

NLAT_CORE = 2048
NCTX = 256
VERBOSE = True


def _run(nc, P, maps, tag):
    t0 = time.time()
    res = run_bass_kernel_spmd(nc, maps, core_ids=list(range(NCORE)))
    if VERBOSE:
        print(f"[kernel] {tag}: {time.time() - t0:.1f}s dev_ns={getattr(res, 'exec_time_ns', None)}", flush=True)
    return res.results


def run_moe(run_fn, l, inp, modv, aff_list, h2_list, xmid_list, has_ctx):
    cD = ld_consts()
    o = NCTX if has_ctx else 0
    aff_lat = np.concatenate([np.asarray(a)[o:] for a in aff_list], 0)
    h2_lat = np.concatenate([np.asarray(h)[o:] for h in h2_list], 0)
    if has_ctx:
        aff_c = np.asarray(aff_list[0])[:NCTX]
        aff_all = np.concatenate([aff_c, aff_lat], 0)
        h2_all = np.concatenate([np.asarray(h2_list[0])[:NCTX], h2_lat], 0)
        groups_all = [([0, 1], 32)] + [([2 + 4 * q + k for k in range(4)], 128) for q in range(32)]
        chunks = [[0]] + [[1 + 4 * c + k for k in range(4)] for c in range(8)]
        groups_own = [([0, 1], 32)] + [([2 + 4 * q + k for k in range(4)], 128) for q in range(4)]
        cs = 32
    else:
        aff_all, h2_all = aff_lat, h2_lat
        groups_all = [([4 * q + k for k in range(4)], 128) for q in range(32)]
        chunks = [[4 * c + k for k in range(4)] for c in range(8)]
        groups_own = [([4 * q + k for k in range(4)], 128) for q in range(4)]
        cs = 0
    h2_all = np.ascontiguousarray(h2_all)
    nc, P = build_ld1(groups_all, chunks, 2048, 2048, has_ctx)
    maps = []
    for j in range(NCORE):
        perm = [2 * j, 2 * j + 1] + [e for e in range(16) if e not in (2 * j, 2 * j + 1)]
        m = {"affT_lat": np.ascontiguousarray(aff_lat[:, perm].T), "aff_all": np.ascontiguousarray(aff_all[:, perm]), "h2": h2_all,
             "wg": np.ascontiguousarray(inp["moe_w_gate"][l][2 * j:2 * j + 2]), "wu": np.ascontiguousarray(inp["moe_w_up"][l][2 * j:2 * j + 2]),
             "wd": np.ascontiguousarray(inp["moe_w_down"][l][2 * j:2 * j + 2])}
        if has_ctx:
            m["affT_ctx"] = np.ascontiguousarray(aff_c[:, perm].T)
        m.update(cD)
        maps.append(m)
    res1 = run_fn(nc, P, maps, f"LD1_{l}")
    Yo = [np.asarray(r["Yo"]) for r in res1]
    nc, P = build_ld2(groups_own, 2048, 2048, has_ctx)
    affT_lat = np.ascontiguousarray(aff_lat.T)
    maps = []
    for j in range(NCORE):
        lo = cs + 512 * j
        parts = []
        for e in range(16):
            y = Yo[e // 2][e % 2]
            parts.append(np.concatenate([y[:cs], y[lo:lo + 512]], 0)[None])
        m = {"affT_lat": affT_lat, "aff_own": np.asarray(aff_list[j]), "xmid": np.asarray(xmid_list[j]), "Yin": np.ascontiguousarray(np.concatenate(parts, 0)),
             "modl": np.ascontiguousarray(modv[l, 0][None]), "modc": np.ascontiguousarray(modv[l, 1][None]),
             "lng": inp["ln_g"][l, 1][None], "lnb": inp["ln_b"][l, 1][None]}
        if has_ctx:
            m["affT_ctx"] = np.ascontiguousarray(aff_c.T)
        m.update(cD)
        maps.append(m)
    res2 = run_fn(nc, P, maps, f"LD2_{l}")
    return [np.asarray(r["xout"]) for r in res2]


def _f32(a):
    return np.ascontiguousarray(np.asarray(a, dtype=np.float32))


def kernel(x, c, ctx, c_ctx, ada_w, ada_b, ln_g, ln_b, ev_w_in, ev_w_out, hgrn_lb, hgrn_norm_g,
           gqa_q_norm_g, gqa_k_norm_g, mla_w_down, mla_q_norm_g, mla_kv_norm_g, mla_w_uq, mla_w_ukv,
           mla_w_o, moe_router, moe_w_gate, moe_w_up, moe_w_down):
    inp = dict(x=x, c=c, ctx=ctx, c_ctx=c_ctx, ada_w=ada_w, ada_b=ada_b, ln_g=ln_g, ln_b=ln_b, ev_w_in=ev_w_in,
               ev_w_out=ev_w_out, hgrn_lb=hgrn_lb, hgrn_norm_g=hgrn_norm_g, gqa_q_norm_g=gqa_q_norm_g,
               gqa_k_norm_g=gqa_k_norm_g, mla_w_down=mla_w_down, mla_q_norm_g=mla_q_norm_g, mla_kv_norm_g=mla_kv_norm_g,
               mla_w_uq=mla_w_uq, mla_w_ukv=mla_w_ukv, mla_w_o=mla_w_o, moe_router=moe_router, moe_w_gate=moe_w_gate,
               moe_w_up=moe_w_up, moe_w_down=moe_w_down)
    inp = {k: _f32(v) for k, v in inp.items()}
    xs = inp["x"][0]
    ctx0 = inp["ctx"][0]
    ident = np.eye(128, dtype=np.float32)

    nc, P = build_l0()
    modv = l0_gather(_run(nc, P, l0_inputs(inp), "L0"))

    def modmaps(l):
        return {"modl": np.ascontiguousarray(modv[l, 0][None]), "modc": np.ascontiguousarray(modv[l, 1][None])}

    def run_la(l, x_lat, x_ctx):
        nc_la, P_la = build_la()
        maps = []
        for j in range(NCORE):
            m = {"xin": np.concatenate([x_ctx, x_lat[j * NLAT_CORE:(j + 1) * NLAT_CORE]], 0), "ident": ident}
            m.update(modmaps(l))
            maps.append(m)
        res = _run(nc_la, P_la, maps, f"LA{l}")
        return np.concatenate([np.asarray(res[0]["hT"])[:, :NCTX]] + [np.asarray(r["hT"])[:, NCTX:] for r in res], axis=1)

    def run_ld(l, res_c, has_ctx):
        return run_moe(_run, l, inp, modv, [r["aff"] for r in res_c], [r["h2"] for r in res_c], [r["xmid"] for r in res_c], has_ctx)

    hT_all = run_la(0, xs, ctx0)
    nc, P = build_lb()
    res_b = _run(nc, P, lb_inputs(inp, hT_all), "LB")
    oT_full = np.concatenate([np.asarray(r["aT"]) for r in res_b] + [np.asarray(r["bT"]) for r in res_b], axis=0)
    nc, P = build_lc(18, 2)
    maps = []
    for j in range(NCORE):
        lo = NCTX + j * NLAT_CORE
        m = {"oT": np.ascontiguousarray(np.concatenate([oT_full[:, :NCTX], oT_full[:, lo:lo + NLAT_CORE]], 1)), "wo": inp["ev_w_out"][0],
             "xin": np.concatenate([ctx0, xs[j * NLAT_CORE:(j + 1) * NLAT_CORE]], 0),
             "lng": inp["ln_g"][0, 0][None], "lnb": inp["ln_b"][0, 0][None], "wr": inp["moe_router"][0], "ident": ident}
        m.update(modmaps(0))
        maps.append(m)
    res_c = _run(nc, P, maps, "LC0")
    res_d = run_ld(0, res_c, True)
    x1 = np.concatenate([r[NCTX:] for r in res_d], 0)
    ctx1 = res_d[0][:NCTX]

    hT_all1 = run_la(1, x1, ctx1)
    nc, P = build_le()
    res_e = _run(nc, P, le_inputs(inp, hT_all1), "LE")
    oT1 = np.concatenate([np.asarray(r["oT"]) for r in res_e], axis=0)
    nc, P = build_lc(16, 0)
    maps = []
    for j in range(NCORE):
        m = {"oT": np.ascontiguousarray(oT1[:, j * NLAT_CORE:(j + 1) * NLAT_CORE]), "wo": inp["mla_w_o"][0],
             "xin": np.ascontiguousarray(x1[j * NLAT_CORE:(j + 1) * NLAT_CORE]),
             "lng": inp["ln_g"][1, 0][None], "lnb": inp["ln_b"][1, 0][None], "wr": inp["moe_router"][1], "ident": ident}
        m.update(modmaps(1))
        maps.append(m)
    res_c1 = _run(nc, P, maps, "LC1")
    res_d1 = run_ld(1, res_c1, False)
    out = np.concatenate(res_d1, 0)
    return out[None].astype(np.float32)
```
